# Optimizing a Trainium2 kernel written in Bass

```python
import jax, jax.numpy as jnp
from jax import lax
import numpy as np

D_MODEL = 2048
BATCH = 2
SEQ = 8192
DEPTH = 2

HEAD_DIM = 128
BLOCK_Q = 128
ROPE_THETA = 10000.0
LN_EPS = 1e-5
RMS_EPS = 1e-6

MLA_HEADS = 10
MLA_Q_RANK = 448
MLA_KV_RANK = 128
MLA_NOPE_DIM = 128
MLA_ROPE_DIM = 64
MLA_V_DIM = 128
MLA_QK_DIM = MLA_NOPE_DIM + MLA_ROPE_DIM

DIL_PATTERNS = ((128, 1), (512, 4), (2048, 16))
DIL_GROUPS = len(DIL_PATTERNS)
DIL_HEADS = 6

OFF_CQ = 0
OFF_CKV = OFF_CQ + MLA_Q_RANK
OFF_KROPE = OFF_CKV + MLA_KV_RANK
OFF_DQ = OFF_KROPE + MLA_ROPE_DIM
OFF_DK = OFF_DQ + DIL_GROUPS * DIL_HEADS * HEAD_DIM
OFF_DV = OFF_DK + DIL_HEADS * HEAD_DIM
W_IN_COLS = OFF_DV + DIL_HEADS * HEAD_DIM
MIX_WIDTH = MLA_HEADS * MLA_V_DIM + DIL_HEADS * HEAD_DIM

FOX_HEADS = 16
FOX_WIDTH = FOX_HEADS * HEAD_DIM

D_FF = 5632
N_EXPERTS = 8
TOP_K = 2

ALPHA = (2.0 * DEPTH) ** 0.25
BETA = (8.0 * DEPTH) ** -0.25
N_EVEN_LAYERS = (DEPTH + 1) // 2
N_ODD_LAYERS = DEPTH // 2

kernel_name = "hybrid_mla_dilated_fox_moe_deepnorm"

F32 = jnp.float32


def layer_norm(x, g, b):
    xf = x.astype(F32)
    mu = jnp.mean(xf, axis=-1, keepdims=True)
    var = jnp.mean(jnp.square(xf - mu), axis=-1, keepdims=True)
    return ((xf - mu) * lax.rsqrt(var + LN_EPS) * g.astype(F32) + b.astype(F32)).astype(x.dtype)


def rms_norm(x, g):
    xf = x.astype(F32)
    y = xf * lax.rsqrt(jnp.mean(jnp.square(xf), axis=-1, keepdims=True) + RMS_EPS)
    return (y * g.astype(F32)).astype(x.dtype)


def rope_tables(seq, dim):
    inv_freq = 1.0 / (ROPE_THETA ** (jnp.arange(0, dim, 2, dtype=F32) / dim))
    ang = jnp.arange(seq, dtype=F32)[:, None] * inv_freq[None, :]
    return jnp.cos(ang), jnp.sin(ang)


def apply_rope(x, cos, sin):
    half = x.shape[-1] // 2
    xf = x.astype(F32)
    x1, x2 = xf[..., :half], xf[..., half:]
    c = cos[None, :, None, :]
    s = sin[None, :, None, :]
    return jnp.concatenate([x1 * c - x2 * s, x2 * c + x1 * s], axis=-1).astype(x.dtype)


def _to_blocks(a):
    b, s = a.shape[0], a.shape[1]
    a = a.reshape((b, s // BLOCK_Q, BLOCK_Q) + a.shape[2:])
    return jnp.moveaxis(a, 1, 0)


def _from_blocks(a):
    a = jnp.moveaxis(a, 0, 1)
    return a.reshape((a.shape[0], a.shape[1] * a.shape[2]) + a.shape[3:])


def causal_block_attention(q, k, v, scale, log_f_cum=None):
    seq = q.shape[1]
    kpos = jnp.arange(seq)
    qb = _to_blocks(q)
    blk_idx = jnp.arange(qb.shape[0])
    if log_f_cum is None:
        xs = (blk_idx, qb)
    else:
        c_keys = jnp.transpose(log_f_cum, (0, 2, 1))[:, :, None, :]
        xs = (blk_idx, qb, _to_blocks(log_f_cum))

    def one_block(args):
        i, q_blk = args[0], args[1]
        qpos = i * BLOCK_Q + jnp.arange(BLOCK_Q)
        s = jnp.einsum('bqhd,bkhd->bhqk', q_blk, k, preferred_element_type=F32) * scale
        if log_f_cum is not None:
            c_q = jnp.transpose(args[2], (0, 2, 1))[..., None]
            s = s + (c_q - c_keys)
        s = jnp.where((kpos[None, :] <= qpos[:, None])[None, None], s, -jnp.inf)
        p = jax.nn.softmax(s, axis=-1)
        return jnp.einsum('bhqk,bkhd->bqhd', p.astype(v.dtype), v)

    return _from_blocks(lax.map(one_block, xs))


def dilated_group_attention(q, k, v, window, dilation, scale):
    offs = jnp.arange(window // dilation + 1) * dilation
    qb = _to_blocks(q)

    def one_block(args):
        i, q_blk = args
        qpos = i * BLOCK_Q + jnp.arange(BLOCK_Q)
        kidx = qpos[:, None] - offs[None, :]
        valid = kidx >= 0
        kidx = jnp.maximum(kidx, 0)
        k_g = jnp.take(k, kidx, axis=1)
        v_g = jnp.take(v, kidx, axis=1)
        s = jnp.einsum('bqhd,bqjhd->bhqj', q_blk, k_g, preferred_element_type=F32) * scale
        s = jnp.where(valid[None, None], s, -jnp.inf)
        lse = jax.nn.logsumexp(s, axis=-1, keepdims=True)
        p = jnp.exp(s - lse)
        o = jnp.einsum('bhqj,bqjhd->bqhd', p.astype(v.dtype), v_g)
        return o, jnp.transpose(lse[..., 0], (0, 2, 1))

    o, lse = lax.map(one_block, (jnp.arange(qb.shape[0]), qb))
    return _from_blocks(o), _from_blocks(lse)


def mla_dilated_mixer(x, w_in, q_norm, w_q_b, kv_norm, w_kv_b, w_out, cos64, sin64, cos128, sin128):
    b, s, _ = x.shape
    h = x @ w_in
    c_q = rms_norm(h[..., OFF_CQ:OFF_CKV], q_norm)
    q = (c_q @ w_q_b).reshape(b, s, MLA_HEADS, MLA_QK_DIM)
    q = jnp.concatenate([q[..., :MLA_NOPE_DIM],
                         apply_rope(q[..., MLA_NOPE_DIM:], cos64, sin64)], axis=-1)
    c_kv = rms_norm(h[..., OFF_CKV:OFF_KROPE], kv_norm)
    kv = (c_kv @ w_kv_b).reshape(b, s, MLA_HEADS, MLA_NOPE_DIM + MLA_V_DIM)
    k_rope = apply_rope(h[..., OFF_KROPE:OFF_DQ].reshape(b, s, 1, MLA_ROPE_DIM), cos64, sin64)
    k = jnp.concatenate([kv[..., :MLA_NOPE_DIM],
                         jnp.broadcast_to(k_rope, (b, s, MLA_HEADS, MLA_ROPE_DIM))], axis=-1)
    v = kv[..., MLA_NOPE_DIM:]
    o_mla = causal_block_attention(q, k, v, MLA_QK_DIM ** -0.5)
    dq = apply_rope(h[..., OFF_DQ:OFF_DK].reshape(b, s, DIL_GROUPS * DIL_HEADS, HEAD_DIM), cos128, sin128)
    dq = dq.reshape(b, s, DIL_GROUPS, DIL_HEADS, HEAD_DIM)
    dk = apply_rope(h[..., OFF_DK:OFF_DV].reshape(b, s, DIL_HEADS, HEAD_DIM), cos128, sin128)
    dv = h[..., OFF_DV:W_IN_COLS].reshape(b, s, DIL_HEADS, HEAD_DIM)
    outs, lses = [], []
    for g, (window, dilation) in enumerate(DIL_PATTERNS):
        o_g, lse_g = dilated_group_attention(dq[:, :, g], dk, dv, window, dilation, HEAD_DIM ** -0.5)
        outs.append(o_g)
        lses.append(lse_g)
    wts = jax.nn.softmax(jnp.stack(lses, axis=0), axis=0)
    o_dil = jnp.sum(wts[..., None] * jnp.stack(outs, axis=0).astype(F32), axis=0).astype(x.dtype)
    mixed = jnp.concatenate([o_mla.reshape(b, s, MLA_HEADS * MLA_V_DIM),
                             o_dil.reshape(b, s, DIL_HEADS * HEAD_DIM)], axis=-1)
    return mixed @ w_out


def forgetting_attention_mixer(x, w_qkv, w_f, b_f, w_out):
    b, s, _ = x.shape
    qkv = (x @ w_qkv).reshape(b, s, 3, FOX_HEADS, HEAD_DIM)
    log_f = jax.nn.log_sigmoid((x @ w_f + b_f).astype(F32))
    c = jnp.cumsum(log_f, axis=1)
    o = causal_block_attention(qkv[:, :, 0], qkv[:, :, 1], qkv[:, :, 2], HEAD_DIM ** -0.5, c)
    return o.reshape(b, s, FOX_WIDTH) @ w_out


def swiglu(x, w_gate, w_up, w_down):
    return (jax.nn.silu(x @ w_gate) * (x @ w_up)) @ w_down


def top2_moe(x, router_w, router_b, w_gate, w_up, w_down):
    logits = (x @ router_w + router_b).astype(F32)
    top_logits, top_idx = lax.top_k(logits, TOP_K)
    top_w = jax.nn.softmax(top_logits, axis=-1)
    gate = jnp.sum(jax.nn.one_hot(top_idx, N_EXPERTS, dtype=F32) * top_w[..., None], axis=-2).astype(x.dtype)
    out = jnp.zeros_like(x)
    for e in range(N_EXPERTS):
        out = out + gate[..., e:e + 1] * swiglu(x, w_gate[e], w_up[e], w_down[e])
    return out


def setup_inputs(seed: int = 0) -> dict:
    key = jax.random.key(seed)
    ks = jax.random.split(key, 32)

    def w(k, shape, fan_in, scale=1.0):
        return jax.random.normal(k, shape, F32) * (scale * fan_in ** -0.5)

    def gain(k, shape):
        return 1.0 + 0.02 * jax.random.normal(k, shape, F32)

    def bias(k, shape, scale=0.02):
        return scale * jax.random.normal(k, shape, F32)

    ne, no = N_EVEN_LAYERS, N_ODD_LAYERS
    in_col_scale = jnp.ones((W_IN_COLS,), F32).at[OFF_DV:].set(BETA)
    kv_b_col_scale = jnp.tile(jnp.concatenate([jnp.ones((MLA_NOPE_DIM,), F32),
                                               jnp.full((MLA_V_DIM,), BETA, F32)]), MLA_HEADS)
    qkv_col_scale = jnp.ones((3 * FOX_WIDTH,), F32).at[2 * FOX_WIDTH:].set(BETA)
    return {
        "x": jax.random.normal(ks[0], (BATCH, SEQ, D_MODEL), F32),
        "ev_w_in": w(ks[1], (ne, D_MODEL, W_IN_COLS), D_MODEL) * in_col_scale,
        "ev_q_norm": gain(ks[2], (ne, MLA_Q_RANK)),
        "ev_w_q_b": w(ks[3], (ne, MLA_Q_RANK, MLA_HEADS * MLA_QK_DIM), MLA_Q_RANK),
        "ev_kv_norm": gain(ks[4], (ne, MLA_KV_RANK)),
        "ev_w_kv_b": w(ks[5], (ne, MLA_KV_RANK, MLA_HEADS * (MLA_NOPE_DIM + MLA_V_DIM)), MLA_KV_RANK) * kv_b_col_scale,
        "ev_w_out": w(ks[6], (ne, MIX_WIDTH, D_MODEL), MIX_WIDTH, BETA),
        "ev_ln1_g": gain(ks[7], (ne, D_MODEL)),
        "ev_ln1_b": bias(ks[8], (ne, D_MODEL)),
        "ev_ffn_w_gate": w(ks[9], (ne, D_MODEL, D_FF), D_MODEL),
        "ev_ffn_w_up": w(ks[10], (ne, D_MODEL, D_FF), D_MODEL, BETA),
        "ev_ffn_w_down": w(ks[11], (ne, D_FF, D_MODEL), D_FF, BETA),
        "ev_ln2_g": gain(ks[12], (ne, D_MODEL)),
        "ev_ln2_b": bias(ks[13], (ne, D_MODEL)),
        "od_w_qkv": w(ks[14], (no, D_MODEL, 3 * FOX_WIDTH), D_MODEL) * qkv_col_scale,
        "od_w_f": w(ks[15], (no, D_MODEL, FOX_HEADS), D_MODEL),
        "od_b_f": bias(ks[16], (no, FOX_HEADS), 0.1),
        "od_w_out": w(ks[17], (no, FOX_WIDTH, D_MODEL), FOX_WIDTH, BETA),
        "od_ln1_g": gain(ks[18], (no, D_MODEL)),
        "od_ln1_b": bias(ks[19], (no, D_MODEL)),
        "od_router_w": w(ks[20], (no, D_MODEL, N_EXPERTS), D_MODEL),
        "od_router_b": bias(ks[21], (no, N_EXPERTS), 0.01),
        "od_exp_w_gate": w(ks[22], (no, N_EXPERTS, D_MODEL, D_FF), D_MODEL),
        "od_exp_w_up": w(ks[23], (no, N_EXPERTS, D_MODEL, D_FF), D_MODEL, BETA),
        "od_exp_w_down": w(ks[24], (no, N_EXPERTS, D_FF, D_MODEL), D_FF, BETA),
        "od_ln2_g": gain(ks[25], (no, D_MODEL)),
        "od_ln2_b": bias(ks[26], (no, D_MODEL)),
    }


def reference(x, ev_w_in, ev_q_norm, ev_w_q_b, ev_kv_norm, ev_w_kv_b, ev_w_out, ev_ln1_g, ev_ln1_b,
              ev_ffn_w_gate, ev_ffn_w_up, ev_ffn_w_down, ev_ln2_g, ev_ln2_b,
              od_w_qkv, od_w_f, od_b_f, od_w_out, od_ln1_g, od_ln1_b,
              od_router_w, od_router_b, od_exp_w_gate, od_exp_w_up, od_exp_w_down, od_ln2_g, od_ln2_b):
    seq = x.shape[1]
    cos64, sin64 = rope_tables(seq, MLA_ROPE_DIM)
    cos128, sin128 = rope_tables(seq, HEAD_DIM)
    for layer in range(DEPTH):
        i = layer // 2
        if layer % 2 == 0:
            mix = mla_dilated_mixer(x, ev_w_in[i], ev_q_norm[i], ev_w_q_b[i], ev_kv_norm[i], ev_w_kv_b[i],
                                    ev_w_out[i], cos64, sin64, cos128, sin128)
            x = layer_norm(ALPHA * x + mix, ev_ln1_g[i], ev_ln1_b[i])
            ffn = swiglu(x, ev_ffn_w_gate[i], ev_ffn_w_up[i], ev_ffn_w_down[i])
            x = layer_norm(ALPHA * x + ffn, ev_ln2_g[i], ev_ln2_b[i])
        else:
            mix = forgetting_attention_mixer(x, od_w_qkv[i], od_w_f[i], od_b_f[i], od_w_out[i])
            x = layer_norm(ALPHA * x + mix, od_ln1_g[i], od_ln1_b[i])
            ffn = top2_moe(x, od_router_w[i], od_router_b[i], od_exp_w_gate[i], od_exp_w_up[i], od_exp_w_down[i])
            x = layer_norm(ALPHA * x + ffn, od_ln2_g[i], od_ln2_b[i])
    return x
```

```python
import numpy as np
import concourse.bass as bass
import concourse.mybir as mybir
from concourse.bass_utils import run_bass_kernel_spmd

F32 = mybir.dt.float32
BF16 = mybir.dt.bfloat16
I32 = mybir.dt.int32
AF = mybir.ActivationFunctionType
ALU = mybir.AluOpType
AX = mybir.AxisListType

ENGS = ['pe', 'act', 'dve', 'pool', 'sp']
SEM_CAP = 30000
DMA_POOL = 12


class Op:
    __slots__ = ('eng', 'fn', 'reads', 'writes', 'dma', 'idx', 'pos', 'waits', 'snap',
                 'milestone', 'ms', 'dsem', 'dval', 'dprev')

    def __init__(self, eng, fn, reads, writes, dma):
        self.eng = eng
        self.fn = fn
        self.reads = reads
        self.writes = writes
        self.dma = dma
        self.milestone = False
        self.waits = ()
        self.ms = -1


class Prog:
    def __init__(self, nc, same_sync=True):
        self.nc = nc
        self.ops = []
        self.same_sync = same_sync
        self.final_dmas = []

    def op(self, eng, fn, reads=(), writes=(), dma=False):
        o = Op(eng, fn, tuple(reads), tuple(writes), dma)
        self.ops.append(o)
        return o

    def pe(self, fn, r=(), w=()):
        return self.op('pe', fn, r, w)

    def act(self, fn, r=(), w=()):
        return self.op('act', fn, r, w)

    def dve(self, fn, r=(), w=()):
        return self.op('dve', fn, r, w)

    def pool(self, fn, r=(), w=()):
        return self.op('pool', fn, r, w)

    def dma(self, q, out, in_, r=(), w=(), final=False, **kw):
        o = self.op(q, lambda e: e.dma_start(out=out, in_=in_, **kw), r, w, dma=True)
        if final:
            self.final_dmas.append(o)
        return o

    def matmul(self, out, lhsT, rhs, start=True, stop=True, r=(), w=()):
        return self.op('pe', lambda e: e.matmul(out, lhsT, rhs, start=start, stop=stop), r, w)

    def transpose(self, out, in_, ident, r=(), w=()):
        return self.op('pe', lambda e: e.transpose(out, in_, ident), r, w)

    def activation(self, out, in_, func, r=(), w=(), **kw):
        return self.op('act', lambda e: e.activation(out=out, in_=in_, func=func, **kw), r, w)

    def tt(self, eng, out, in0, in1, op, r=(), w=()):
        return self.op(eng, lambda e: e.tensor_tensor(out=out, in0=in0, in1=in1, op=op), r, w)

    def ts(self, eng, out, in0, s1, s2, op0, op1=None, r=(), w=(), **kw):
        if op1 is None:
            return self.op(eng, lambda e: e.tensor_scalar(out=out, in0=in0, scalar1=s1, scalar2=s2, op0=op0, **kw), r, w)
        return self.op(eng, lambda e: e.tensor_scalar(out=out, in0=in0, scalar1=s1, scalar2=s2, op0=op0, op1=op1, **kw), r, w)

    def stt(self, eng, out, in0, scalar, in1, op0, op1, r=(), w=()):
        return self.op(eng, lambda e: e.scalar_tensor_tensor(out=out, in0=in0, scalar=scalar, in1=in1, op0=op0, op1=op1), r, w)

    def copy(self, eng, out, in_, r=(), w=()):
        if eng == 'act':
            return self.op(eng, lambda e: e.copy(out=out, in_=in_), r, w)
        return self.op(eng, lambda e: e.tensor_copy(out=out, in_=in_), r, w)

    def memset(self, eng, ap, val, w=()):
        return self.op(eng, lambda e: e.memset(ap, val), (), w)

    def recip(self, out, in_, r=(), w=()):
        return self.op('dve', lambda e: e.reciprocal(out=out, in_=in_), r, w)

    def analyze(self):
        per = {e: [] for e in ENGS}
        for i, o in enumerate(self.ops):
            o.idx = i
            o.pos = len(per[o.eng])
            per[o.eng].append(o)
        self.per = per
        last_w = {}
        readers = {}
        known = {e: {f: -1 for f in ENGS} for e in ENGS}
        known_dma = {e: set() for e in ENGS}
        for o in self.ops:
            deps = {}
            for r in o.reads:
                w = last_w.get(r)
                if w is not None:
                    deps[w.idx] = w
            for r in o.writes:
                w = last_w.get(r)
                if w is not None:
                    deps[w.idx] = w
                rd = readers.get(r)
                if rd:
                    for x in rd.values():
                        deps[x.idx] = x
            deps.pop(o.idx, None)
            kn = known[o.eng]
            kd = known_dma[o.eng]
            waits = []
            for d in deps.values():
                if d.dma:
                    if d.idx in kd:
                        continue
                    waits.append(d)
                else:
                    if d.eng == o.eng and not o.dma:
                        if d.eng == 'pe' or not self.same_sync:
                            continue
                    if kn[d.eng] >= d.pos:
                        continue
                    waits.append(d)
            for d in waits:
                d.milestone = True
                if d.dma:
                    kd.add(d.idx)
                else:
                    if kn[d.eng] < d.pos:
                        kn[d.eng] = d.pos
                for f, p in d.snap.items():
                    if kn[f] < p:
                        kn[f] = p
            o.waits = waits
            o.snap = dict(kn)
            for r in o.writes:
                last_w[r] = o
                readers[r] = {}
            for r in o.reads:
                if r.startswith('const'):
                    continue
                rd = readers.setdefault(r, {})
                key = o.idx if o.dma else o.eng
                rd[key] = o
        for o in self.final_dmas:
            o.milestone = True

    def emit(self):
        nc = self.nc
        self.analyze()
        per = self.per
        import contextlib
        with contextlib.ExitStack() as st:
            sems = {}
            for e in ENGS:
                n = 0
                for o in per[e]:
                    if o.dma:
                        continue
                    if o.milestone:
                        o.ms = n
                        n += 1
                nsem = n // SEM_CAP + 1
                sems[e] = [st.enter_context(nc.semaphore(f"ms_{e}_{k}")) for k in range(nsem)]
            dpool = {}
            for e in ENGS:
                dm = [o for o in per[e] if o.dma]
                if not dm:
                    continue
                pool = [st.enter_context(nc.semaphore(f"dq_{e}_{k}")) for k in range(DMA_POOL)]
                vals = [0] * DMA_POOL
                k = 0
                for o in dm:
                    o.dsem = pool[k]
                    o.dprev = vals[k]
                    vals[k] += 16
                    o.dval = vals[k]
                    k = (k + 1) % DMA_POOL
            block = st.enter_context(nc.Block())

            def run(e, eng):
                for o in per[e]:
                    for d in o.waits:
                        if d.dma:
                            eng.wait_ge(d.dsem, d.dval)
                        else:
                            eng.wait_ge(sems[d.eng][d.ms // SEM_CAP], d.ms % SEM_CAP + 1)
                    if o.dma:
                        if o.dprev > 0:
                            eng.wait_ge(o.dsem, o.dprev)
                        ins = o.fn(eng)
                        ins.then_inc(o.dsem, 16)
                    else:
                        ins = o.fn(eng)
                        if o.milestone:
                            ins.then_inc(sems[e][o.ms // SEM_CAP], 1)
                if e == 'sp':
                    for o in self.final_dmas:
                        eng.wait_ge(o.dsem, o.dval)

            if per['pe']:
                block.tensor(lambda eng: run('pe', eng))
            if per['act']:
                block.scalar(lambda eng: run('act', eng))
            if per['dve']:
                block.vector(lambda eng: run('dve', eng))
            if per['pool']:
                block.gpsimd(lambda eng: run('pool', eng))
            block.sync(lambda eng: run('sp', eng))


D = 2048
NTOK = 2048
TS = 512
NT = NTOK // TS
NDC = D // 128
DFF = 5632
NFC = DFF // 128
NEXP = 8
ALPHA_C = 4.0 ** 0.25
LN_EPS = 1e-5
RMS_EPS = 1e-6
NEG = -30000.0
OFF_CQ, OFF_CKV, OFF_KR, OFF_DQ, OFF_DK, OFF_DV = 0, 448, 576, 640, 2944, 3712


class Ctx:
    def __init__(self):
        self.nc = bass.Bass("TRN2", target_bir_lowering=False)
        self.P = Prog(self.nc)
        self.in_names = []
        self.out_names = []
        self._n = 0

    def inp(self, name, shape, dt=F32):
        self.in_names.append(name)
        return self.nc.dram_tensor(name, list(shape), dt, kind="ExternalInput").ap()

    def out(self, name, shape, dt=F32):
        self.out_names.append(name)
        return self.nc.dram_tensor(name, list(shape), dt, kind="ExternalOutput").ap()

    def sb(self, name, shape, dt=F32):
        return self.nc.alloc_sbuf_tensor(name, list(shape), dt)

    def ps(self, name):
        return self.nc.alloc_psum_tensor(name, [128, 512], F32)

    def ring(self, name, n, shape, dt):
        return Ring([(self.sb(f"{name}{i}", shape, dt), f"{name}{i}") for i in range(n)])

    def psring(self, name, n):
        return Ring([(self.ps(f"{name}{i}"), f"{name}{i}") for i in range(n)])

    def run(self, in_maps):
        self.P.emit()
        res = run_bass_kernel_spmd(self.nc, in_maps, core_ids=list(range(len(in_maps))))
        return res.results


class Ring:
    def __init__(self, items):
        self.items = items
        self.i = 0

    def next(self):
        it = self.items[self.i % len(self.items)]
        self.i += 1
        return it


def cast_load(P, dst, src, n, res, q='pool', maxcols=1024, per_chunk=False):
    cols = dst.shape[-1]
    for i in range(n):
        for c0 in range(0, cols, maxcols):
            c1 = min(cols, c0 + maxcols)
            P.dma(q, dst[:, i, c0:c1], src[:, i, c0:c1], w=[f"{res}_{i}" if per_chunk else res])


def load_consts(C, need_rot=False):
    P = C.P
    k = {}
    k['ones32'] = C.sb('ones32', [128, 128], F32)
    k['ones16'] = C.sb('ones16', [128, 128], BF16)
    P.memset('dve', k['ones32'][:, :], 1.0, w=['const_ones32'])
    P.memset('dve', k['ones16'][:, :], 1.0, w=['const_ones16'])
    return k


def layernorm_fm(C, K, z, zres, g_sb, b_sb, gres, xo, xo_res, x16, x16_res, ps_s1, ps_s2, scr):
    P = C.P
    (s1, s1r), (s2, s2r) = ps_s1, ps_s2
    sq = scr['sq']
    zl = list(zres) if isinstance(zres, (list, tuple)) else [zres]
    for oc in range(NDC):
        P.matmul(s1[:, :], K['ones32'][:, :], z[:, oc, :], start=(oc == 0), stop=(oc == NDC - 1),
                 r=zl + ['const_ones32'], w=[s1r])
    for oc in range(NDC):
        t, tr = sq.next()
        P.activation(t[:, :], z[:, oc, :], AF.Square, r=zl, w=[tr])
        P.matmul(s2[:, :], K['ones32'][:, :], t[:, :], start=(oc == 0), stop=(oc == NDC - 1),
                 r=[tr, 'const_ones32'], w=[s2r])
    mean, m2, rstd = scr['mean'], scr['m2'], scr['rstd']
    P.ts('dve', mean[:, :], s1[:, :], 1.0 / D, None, ALU.mult, r=[s1r], w=['ln_mean'])
    P.stt('dve', m2[:, :], mean[:, :], 1.0, mean[:, :], ALU.mult, ALU.mult, r=['ln_mean'], w=['ln_m2'])
    P.stt('dve', m2[:, :], s2[:, :], 1.0 / D, m2[:, :], ALU.mult, ALU.subtract, r=[s2r, 'ln_m2'], w=['ln_m2'])
    P.ts('dve', m2[:, :], m2[:, :], LN_EPS, None, ALU.add, r=['ln_m2'], w=['ln_m2'])
    P.activation(rstd[:, :], m2[:, :], AF.Sqrt, r=['ln_m2'], w=['ln_rstd'])
    P.recip(rstd[:, :], rstd[:, :], r=['ln_rstd'], w=['ln_rstd'])
    for oc in range(NDC):
        t, tr = sq.next()
        P.stt('dve', t[:, :], z[:, oc, :], 1.0, mean[:, :], ALU.mult, ALU.subtract, r=zl + ['ln_mean'], w=[tr])
        P.stt('dve', t[:, :], t[:, :], 1.0, rstd[:, :], ALU.mult, ALU.mult, r=[tr, 'ln_rstd'], w=[tr])
        P.ts('dve', xo[:, oc, :], t[:, :], g_sb[:, oc:oc + 1], b_sb[:, oc:oc + 1], ALU.mult, ALU.add,
             r=[tr, gres], w=[xo_res] + (zl if xo is z else []))
        P.copy('act', x16[:, oc, :], xo[:, oc, :], r=[xo_res], w=[x16_res(oc) if callable(x16_res) else x16_res])


def ln_scratch(C):
    return {'sq': C.ring('lnsq', 3, [128, 512], F32), 'mean': C.sb('ln_mean', [128, 512]),
            'm2': C.sb('ln_m2', [128, 512]), 'rstd': C.sb('ln_rstd', [128, 512])}


def rope_fm(C, n, acc, accr, cs_sb, col0, Rm, rings, out_ap, sfx):
    P = C.P
    t16, t16r = rings['t16'].next()
    rot, rotr = rings['rot'].next()
    a32, a32r = rings['a32'].next()
    b32, b32r = rings['b32'].next()
    o16, o16r = rings['o16'].next()
    P.copy('act', t16[0:n, :], acc[0:n, :], r=[accr], w=[t16r])
    P.matmul(rot[0:n, :], Rm[0:n, 0:n], t16[0:n, :], r=[t16r, 'const_R' + sfx], w=[rotr])
    P.copy('act', a32[0:n, :], acc[0:n, :], r=[accr], w=[a32r])
    P.copy('act', b32[0:n, :], rot[0:n, :], r=[rotr], w=[b32r])
    P.tt('pool', a32[0:n, :], a32[0:n, :], cs_sb[0:n, 0, col0:col0 + TS], ALU.mult, r=[a32r, 'const_cs' + sfx], w=[a32r])
    P.tt('pool', b32[0:n, :], b32[0:n, :], cs_sb[0:n, 1, col0:col0 + TS], ALU.mult, r=[b32r, 'const_cs' + sfx], w=[b32r])
    P.tt('pool', o16[0:n, :], a32[0:n, :], b32[0:n, :], ALU.add, r=[a32r, b32r], w=[o16r])
    P.dma('sp', out_ap, o16[0:n, :], r=[o16r], final=True)


def build_l1():
    C = Ctx()
    P = C.P
    xT = C.inp('xT', [D, NTOK])
    w_in = C.inp('w_in', [D, 4480])
    qn = C.inp('qn', [128, 4])
    w_qb = C.inp('w_qb', [512, 1920])
    kvn = C.inp('kvn', [128, 1])
    w_kvb = C.inp('w_kvb', [128, 2560])
    cs64 = C.inp('cs64', [64, 2, NTOK])
    cs128 = C.inp('cs128', [128, 2, NTOK])
    r64 = C.inp('r64', [64, 64])
    r128 = C.inp('r128', [128, 128])
    qm = C.out('qm', [10, 192, NTOK], BF16)
    km = C.out('km', [10, 128, NTOK], BF16)
    kr = C.out('kr', [64, NTOK], BF16)
    vm = C.out('vm', [10, 128, 16, 128], BF16)
    dq = C.out('dq', [18, 128, NTOK], BF16)
    dk = C.out('dk', [6, 128, NTOK], BF16)
    dv = C.out('dv', [6, 128, 16, 128], BF16)
    K = load_consts(C)
    qn_sb = C.sb('qn_sb', [128, 4])
    kvn_sb = C.sb('kvn_sb', [128, 1])
    cs64_sb = C.sb('cs64_sb', [64, 2, NTOK])
    cs128_sb = C.sb('cs128_sb', [128, 2, NTOK])
    R64 = C.sb('R64', [64, 64], BF16)
    R128 = C.sb('R128', [128, 128], BF16)
    wqb16 = C.sb('wqb16', [128, 4, 1920], BF16)
    wk16 = C.sb('wk16', [128, 10, 128], BF16)
    wv16 = C.sb('wv16', [128, 10, 128], BF16)
    P.dma('sp', qn_sb[:, :], qn, w=['const_qn'])
    P.dma('sp', kvn_sb[:, :], kvn, w=['const_kvn'])
    cast_load(P, cs64_sb, cs64, 2, 'const_cs64', q='sp', maxcols=2048)
    cast_load(P, cs128_sb, cs128, 2, 'const_cs128', q='sp', maxcols=2048)
    P.dma('pool', R64[:, :], r64, w=['const_R64'])
    P.dma('pool', R128[:, :], r128, w=['const_R128'])
    cast_load(P, wqb16, w_qb.rearrange("(kc p) n -> p kc n", p=128), 4, 'const_wqb', maxcols=480)
    wkv_v = w_kvb.rearrange("r (h two c) -> r two h c", two=2, c=128)
    cast_load(P, wk16, wkv_v[:, 0, :, :], 10, 'const_wk')
    cast_load(P, wv16, wkv_v[:, 1, :, :], 10, 'const_wv')

    x16r = C.ring('x16_', 2, [128, NDC, TS], BF16)
    wgr = C.ring('wg_', 2, [128, NDC, 640], BF16)
    accs = C.psring('acc', 3)
    rings = {'t16': C.ring('t16_', 2, [128, TS], BF16), 'rot': C.psring('rot', 2),
             'a32': C.ring('a32_', 2, [128, TS], F32), 'b32': C.ring('b32_', 2, [128, TS], F32),
             'o16': C.ring('o16_', 4, [128, TS], BF16)}
    ssq = (C.ps('ssq'), 'ssq')
    sskv = (C.ps('sskv'), 'sskv')
    sqr = C.ring('sq_', 2, [128, TS], F32)
    cq32 = C.sb('cq32', [128, 4, TS])
    cqn16 = C.sb('cqn16', [128, 4, TS], BF16)
    ckv32 = C.sb('ckv32', [128, TS])
    ckvn16 = C.sb('ckvn16', [128, TS], BF16)
    rstdq = C.sb('rstdq', [128, TS])
    rstdkv = C.sb('rstdkv', [128, TS])
    vst = C.ring('vst_', 2, [128, 10, 128], BF16)
    xT_v = xT.rearrange("(dc p) t -> p dc t", p=128)
    w_in_v = w_in.rearrange("(dc p) n -> p dc n", p=128)

    def proj(acc, accr, x16, x16res, wg, wgres, c0, n):
        for dc in range(NDC):
            P.matmul(acc[0:n, :], wg[:, dc, c0:c0 + n], x16[:, dc, :], start=(dc == 0), stop=(dc == NDC - 1),
                     r=[x16res, wgres], w=[accr])

    def rms(ss, ssr, n_feat, rstd, rstdres):
        P.ts('dve', rstd[:, :], ss[:, :], 1.0 / n_feat, RMS_EPS, ALU.mult, ALU.add, r=[ssr], w=[rstdres])
        P.activation(rstd[:, :], rstd[:, :], AF.Sqrt, r=[rstdres], w=[rstdres])
        P.recip(rstd[:, :], rstd[:, :], r=[rstdres], w=[rstdres])

    for g in range(NT):
        t0 = g * TS
        x16, x16res = x16r.next()
        cast_load(P, x16, xT_v[:, :, t0:t0 + TS], NDC, x16res)
        wg, wgres = wgr.next()
        cast_load(P, wg[:, :, 0:640], w_in_v[:, :, 0:640], NDC, wgres)
        cq_sizes = [128, 128, 128, 64]
        for cc, n in enumerate(cq_sizes):
            acc, accr = accs.next()
            proj(acc, accr, x16, x16res, wg, wgres, cc * 128, n)
            P.copy('act', cq32[0:n, cc, :], acc[0:n, :], r=[accr], w=['cq32'])
            sq, sqres = sqr.next()
            P.activation(sq[0:n, :], acc[0:n, :], AF.Square, r=[accr], w=[sqres])
            P.matmul(ssq[0][:, :], K['ones32'][0:n, :], sq[0:n, :], start=(cc == 0), stop=(cc == 3),
                     r=[sqres, 'const_ones32'], w=['ssq'])
        rms(ssq[0], 'ssq', 448.0, rstdq, 'rstdq')
        for cc, n in enumerate(cq_sizes):
            P.stt('dve', cqn16[0:n, cc, :], cq32[0:n, cc, :], qn_sb[0:n, cc:cc + 1], rstdq[0:n, :], ALU.mult, ALU.mult,
                  r=['cq32', 'rstdq', 'const_qn'], w=['cqn16'])
        acc, accr = accs.next()
        proj(acc, accr, x16, x16res, wg, wgres, OFF_CKV, 128)
        P.copy('act', ckv32[:, :], acc[:, :], r=[accr], w=['ckv32'])
        sq, sqres = sqr.next()
        P.activation(sq[:, :], acc[:, :], AF.Square, r=[accr], w=[sqres])
        P.matmul(sskv[0][:, :], K['ones32'][:, :], sq[:, :], r=[sqres, 'const_ones32'], w=['sskv'])
        rms(sskv[0], 'sskv', 128.0, rstdkv, 'rstdkv')
        P.stt('dve', ckvn16[:, :], ckv32[:, :], kvn_sb[:, 0:1], rstdkv[:, :], ALU.mult, ALU.mult,
              r=['ckv32', 'rstdkv', 'const_kvn'], w=['ckvn16'])
        acc, accr = accs.next()
        proj(acc, accr, x16, x16res, wg, wgres, OFF_KR, 64)
        rope_fm(C, 64, acc, accr, cs64_sb, t0, R64, rings, kr[:, t0:t0 + TS], '64')
        for h in range(10):
            acc, accr = accs.next()
            for kc, kn in enumerate(cq_sizes):
                P.matmul(acc[:, :], wqb16[0:kn, kc, h * 192:h * 192 + 128], cqn16[0:kn, kc, :], start=(kc == 0), stop=(kc == 3),
                         r=['cqn16', 'const_wqb'], w=[accr])
            o16, o16r = rings['o16'].next()
            P.copy('act', o16[:, :], acc[:, :], r=[accr], w=[o16r])
            P.dma('sp', qm[h, 0:128, t0:t0 + TS], o16[:, :], r=[o16r], final=True)
            acc, accr = accs.next()
            for kc, kn in enumerate(cq_sizes):
                P.matmul(acc[0:64, :], wqb16[0:kn, kc, h * 192 + 128:h * 192 + 192], cqn16[0:kn, kc, :], start=(kc == 0),
                         stop=(kc == 3), r=['cqn16', 'const_wqb'], w=[accr])
            rope_fm(C, 64, acc, accr, cs64_sb, t0, R64, rings, qm[h, 128:192, t0:t0 + TS], '64')
        for h in range(10):
            acc, accr = accs.next()
            P.matmul(acc[:, :], wk16[:, h, :], ckvn16[:, :], r=['ckvn16', 'const_wk'], w=[accr])
            o16, o16r = rings['o16'].next()
            P.copy('act', o16[:, :], acc[:, :], r=[accr], w=[o16r])
            P.dma('sp', km[h, :, t0:t0 + TS], o16[:, :], r=[o16r], final=True)
        for tb in range(4):
            vs, vsr = vst.next()
            for (h0, h1) in [(0, 4), (4, 8), (8, 10)]:
                acc, accr = accs.next()
                nn = (h1 - h0) * 128
                P.matmul(acc[:, 0:nn], ckvn16[:, tb * 128:(tb + 1) * 128], wv16[:, h0:h1, :], r=['ckvn16', 'const_wv'], w=[accr])
                P.copy('act', vs[:, h0:h1, :], acc[:, 0:nn].rearrange("p (h c) -> p h c", c=128), r=[accr], w=[vsr])
            P.dma('sp', vm[:, :, g * 4 + tb, :].rearrange("h p c -> p h c"), vs[:, :, :], r=[vsr], final=True)
        for gi in range(6):
            wg, wgres = wgr.next()
            c_lo = OFF_DQ + gi * 512
            cast_load(P, wg[:, :, 0:512], w_in_v[:, :, c_lo:c_lo + 512], NDC, wgres)
            for j in range(4):
                ci = gi * 4 + j
                acc, accr = accs.next()
                proj(acc, accr, x16, x16res, wg, wgres, j * 128, 128)
                dst = dq[ci, :, t0:t0 + TS] if ci < 18 else dk[ci - 18, :, t0:t0 + TS]
                rope_fm(C, 128, acc, accr, cs128_sb, t0, R128, rings, dst, '128')
        for (c_lo, ncol, h0) in [(OFF_DV, 512, 0), (OFF_DV + 512, 256, 4)]:
            wg, wgres = wgr.next()
            cast_load(P, wg[:, :, 0:ncol], w_in_v[:, :, c_lo:c_lo + ncol], NDC, wgres)
            nh = ncol // 128
            for tb in range(4):
                acc, accr = accs.next()
                for dc in range(NDC):
                    P.matmul(acc[:, 0:ncol], x16[:, dc, tb * 128:(tb + 1) * 128], wg[:, dc, 0:ncol], start=(dc == 0),
                             stop=(dc == NDC - 1), r=[x16res, wgres], w=[accr])
                vs, vsr = vst.next()
                P.copy('act', vs[:, 0:nh, :], acc[:, 0:ncol].rearrange("p (h c) -> p h c", c=128), r=[accr], w=[vsr])
                P.dma('sp', dv[h0:h0 + nh, :, g * 4 + tb, :].rearrange("h p c -> p h c"), vs[:, 0:nh, :], r=[vsr], final=True)
    return C


def core_positions(c):
    r = c % 4
    t = np.arange(NTOK)
    return 512 * (4 * (t // 512) + r) + (t % 512)


def rope_table_fm(pos, dim):
    half = dim // 2
    inv_freq = (1.0 / (np.float32(10000.0) ** (np.arange(0, dim, 2, dtype=np.float32) / np.float32(dim)))).astype(np.float32)
    ang = pos.astype(np.float32)[None, :] * inv_freq[:, None]
    cos = np.cos(ang).astype(np.float32)
    sin = np.sin(ang).astype(np.float32)
    out = np.empty((dim, 2, pos.shape[0]), np.float32)
    out[:half, 0] = cos
    out[half:, 0] = cos
    out[:half, 1] = sin
    out[half:, 1] = sin
    return out


def rot_lhsT(dim):
    half = dim // 2
    m = np.zeros((dim, dim), np.float32)
    for j in range(half):
        m[j + half, j] = -1.0
        m[j, j + half] = 1.0
    return m


def fm_vec(v, n_chunks):
    o = np.zeros((n_chunks * 128,), np.float32)
    o[:v.shape[0]] = v
    return np.ascontiguousarray(o.reshape(n_chunks, 128).T)


def l1_inputs(inp):
    maps = []
    wqb = np.zeros((512, 1920), np.float32)
    wqb[:448] = inp['ev_w_q_b'][0]
    common = {
        'w_in': np.ascontiguousarray(inp['ev_w_in'][0]), 'qn': fm_vec(inp['ev_q_norm'][0], 4), 'w_qb': wqb,
        'kvn': fm_vec(inp['ev_kv_norm'][0], 1), 'w_kvb': np.ascontiguousarray(inp['ev_w_kv_b'][0]),
        'r64': rot_lhsT(64), 'r128': rot_lhsT(128),
    }
    for c in range(8):
        pos = core_positions(c)
        m = dict(common)
        m['xT'] = np.ascontiguousarray(inp['x'][c // 4][pos].T)
        m['cs64'] = rope_table_fm(pos, 64)
        m['cs128'] = rope_table_fm(pos, 128)
        maps.append(m)
    return maps


def build_l2_mla():
    C = Ctx()
    P = C.P
    qm = C.inp('qm', [10, 192, NTOK], BF16)
    kmf = C.inp('kmf', [4, 10, 128, NTOK], BF16)
    krf = C.inp('krf', [4, 64, NTOK], BF16)
    vmf = C.inp('vmf', [4, 10, 128, 16, 128], BF16)
    cmask = C.inp('cmask', [128, 16, TS], BF16)
    mixT = C.out('mixT', [10, 128, NTOK], BF16)
    K = load_consts(C)
    scale = 192.0 ** -0.5
    cm = C.sb('cm', [128, 16, TS], BF16)
    cast_load(P, cm, cmask, 16, 'const_cm', q='sp', maxcols=TS)
    krT = C.sb('krT', [64, 4, NTOK], BF16)
    cast_load(P, krT, krf.rearrange("r p t -> p r t"), 4, 'const_kr', q='sp', maxcols=NTOK)
    kTr = C.ring('kT_', 2, [128, 4, NTOK], BF16)
    vr = C.ring('v_', 2, [128, 4, 16 * 128], BF16)
    qnr = C.ring('qn_', 2, [128, NTOK], BF16)
    qrr = C.ring('qr_', 2, [64, NTOK], BF16)
    Sr = C.psring('S', 3)
    Or = C.psring('O', 2)
    Dr = C.psring('Dn', 2)
    pTr = C.ring('pT_', 3, [128, TS], BF16)
    tmpr = C.ring('tmp_', 2, [128, TS], F32)
    rec = C.sb('rec', [128, TS])
    o32 = C.sb('o32', [128, TS])
    o16r = C.ring('ao16_', 2, [128, TS], BF16)
    for h in range(10):
        kT, kTres = kTr.next()
        v, vres = vr.next()
        qn, qnres = qnr.next()
        qr, qrres = qrr.next()
        for r2 in range(4):
            P.dma('sp', kT[:, r2, :], kmf[r2, h, :, :], w=[kTres + f'_{r2}'])
            P.dma('sp', v[:, r2, :], vmf[r2, h, :, :, :].rearrange("p b c -> p (b c)"), w=[vres + f'_{r2}'])
        P.dma('sp', qn[:, :], qm[h, 0:128, :], w=[qnres])
        P.dma('sp', qr[:, :], qm[h, 128:192, :], w=[qrres])
        for g in range(NT):
            O, Ores = Or.next()
            Dn, Dres = Dr.next()
            nblk = 16 * (g + 1)
            idx = 0
            for g2 in range(g + 1):
                for r2 in range(4):
                    for kb in range(4):
                        S, Sres = Sr.next()
                        pT, pTres = pTr.next()
                        c0 = g2 * TS + kb * 128
                        P.matmul(S[:, :], kT[:, r2, c0:c0 + 128], qn[:, g * TS:(g + 1) * TS], start=True, stop=False,
                                 r=[kTres + f'_{r2}', qnres], w=[Sres])
                        P.matmul(S[:, :], krT[0:64, r2, c0:c0 + 128], qr[0:64, g * TS:(g + 1) * TS], start=False, stop=True,
                                 r=['const_kr', qrres], w=[Sres])
                        if g2 == g:
                            tmp, tmpres = tmpr.next()
                            P.stt('dve', tmp[:, :], S[:, :], scale, cm[:, r2 * 4 + kb, :], ALU.mult, ALU.add,
                                  r=[Sres, 'const_cm'], w=[tmpres])
                            P.activation(pT[:, :], tmp[:, :], AF.Exp, r=[tmpres], w=[pTres])
                        else:
                            P.activation(pT[:, :], S[:, :], AF.Exp, r=[Sres], w=[pTres], scale=scale)
                        blk = g2 * 4 + kb
                        P.matmul(O[:, :], v[:, r2, blk * 128:(blk + 1) * 128], pT[:, :], start=(idx == 0), stop=(idx == nblk - 1),
                                 r=[vres + f'_{r2}', pTres], w=[Ores])
                        P.matmul(Dn[:, :], K['ones16'][:, :], pT[:, :], start=(idx == 0), stop=(idx == nblk - 1),
                                 r=['const_ones16', pTres], w=[Dres])
                        idx += 1
            P.recip(rec[:, :], Dn[:, :], r=[Dres], w=['rec'])
            P.copy('act', o32[:, :], O[:, :], r=[Ores], w=['o32'])
            o16, o16res = o16r.next()
            P.tt('pool', o16[:, :], o32[:, :], rec[:, :], ALU.mult, r=['o32', 'rec'], w=[o16res])
            P.dma('sp', mixT[h, :, g * TS:(g + 1) * TS], o16[:, :], r=[o16res], final=True)
    return C


def causal_mask_tiles(r):
    m = np.zeros((128, 16, TS), np.float32)
    ki = np.arange(128)[:, None]
    qi = np.arange(TS)[None, :]
    for r2 in range(4):
        for kb in range(4):
            if r2 > r:
                m[:, r2 * 4 + kb, :] = NEG
            elif r2 == r:
                m[:, r2 * 4 + kb, :] = np.where(kb * 128 + ki <= qi, 0.0, NEG)
    import ml_dtypes
    return m.astype(ml_dtypes.bfloat16)


DIL_PATTERNS = ((128, 1), (512, 4), (2048, 16))


def _dil_slots(grp):
    if grp < 2:
        return [(3, 1), (0, 0), (1, 0), (2, 0), (3, 0)]
    return [(r2, 1) for r2 in range(4)] + [(r2, 0) for r2 in range(4)]


def _dil_mask_bool(r, grp, r2, dg, kb):
    W, d = DIL_PATTERNS[grp]
    ki = np.arange(128)[:, None]
    qi = np.arange(TS)[None, :]
    delta = 512 * (4 * dg + r - r2) + qi - (128 * kb + ki)
    return (delta >= 0) & (delta <= W) & (delta % d == 0)


def dil_block_list():
    blocks = []
    for grp in range(3):
        for (r2, dg) in _dil_slots(grp):
            for kb in range(4):
                if any(_dil_mask_bool(r, grp, r2, dg, kb).any() for r in range(4)):
                    blocks.append((grp, r2, dg, kb))
    return blocks


def dil_mask_tiles(r):
    import ml_dtypes
    bl = dil_block_list()
    m = np.full((128, len(bl), TS), NEG, np.float32)
    for i, (grp, r2, dg, kb) in enumerate(bl):
        m[:, i, :] = np.where(_dil_mask_bool(r, grp, r2, dg, kb), 0.0, NEG)
    return m.astype(ml_dtypes.bfloat16)


def build_l2_dil():
    C = Ctx()
    P = C.P
    bl = dil_block_list()
    nb = len(bl)
    dq = C.inp('dq', [18, 128, NTOK], BF16)
    dkf = C.inp('dkf', [4, 6, 128, NTOK], BF16)
    dvf = C.inp('dvf', [4, 6, 128, 16, 128], BF16)
    dmask = C.inp('dmask', [128, nb, TS], BF16)
    mixT = C.out('mixD', [6, 128, NTOK], BF16)
    K = load_consts(C)
    scale = 128.0 ** -0.5
    dm = C.sb('dm', [128, nb, TS], BF16)
    for i in range(nb):
        P.dma('sp', dm[:, i, :], dmask[:, i, :], w=[f'const_dm{i}'])
    kT = C.sb('dkT', [128, 4, NTOK], BF16)
    v = C.sb('dv', [128, 4, 16 * 128], BF16)
    q3 = C.sb('dq3', [128, 3, NTOK], BF16)
    Sr = C.psring('S', 3)
    Or = C.psring('O', 2)
    Dr = C.psring('Dn', 2)
    pTr = C.ring('pT_', 3, [128, TS], BF16)
    tmpr = C.ring('tmp_', 3, [128, TS], F32)
    rec = C.sb('rec', [128, TS])
    o32 = C.sb('o32', [128, TS])
    o16r = C.ring('ao16_', 2, [128, TS], BF16)
    for hd in range(6):
        for r2 in range(4):
            P.dma('sp', kT[:, r2, :], dkf[r2, hd, :, :], w=[f'dkT_{r2}'])
            P.dma('sp', v[:, r2, :], dvf[r2, hd, :, :, :].rearrange("p b c -> p (b c)"), w=[f'dv_{r2}'])
        for grp in range(3):
            P.dma('sp', q3[:, grp, :], dq[grp * 6 + hd, :, :], w=[f'dq3_{grp}'])
        for g in range(NT):
            todo = [(i, b) for i, b in enumerate(bl) if g - b[2] >= 0]
            O, Ores = Or.next()
            Dn, Dres = Dr.next()
            for idx, (i, (grp, r2, dg, kb)) in enumerate(todo):
                g2 = g - dg
                S, Sres = Sr.next()
                pT, pTres = pTr.next()
                tmp, tmpres = tmpr.next()
                c0 = g2 * TS + kb * 128
                P.matmul(S[:, :], kT[:, r2, c0:c0 + 128], q3[:, grp, g * TS:(g + 1) * TS], r=[f'dkT_{r2}', f'dq3_{grp}'], w=[Sres])
                P.stt('dve', tmp[:, :], S[:, :], scale, dm[:, i, :], ALU.mult, ALU.add, r=[Sres, f'const_dm{i}'], w=[tmpres])
                P.activation(pT[:, :], tmp[:, :], AF.Exp, r=[tmpres], w=[pTres])
                blk = g2 * 4 + kb
                first, last = idx == 0, idx == len(todo) - 1
                P.matmul(O[:, :], v[:, r2, blk * 128:(blk + 1) * 128], pT[:, :], start=first, stop=last, r=[f'dv_{r2}', pTres], w=[Ores])
                P.matmul(Dn[:, :], K['ones16'][:, :], pT[:, :], start=first, stop=last, r=['const_ones16', pTres], w=[Dres])
            P.recip(rec[:, :], Dn[:, :], r=[Dres], w=['rec'])
            P.copy('act', o32[:, :], O[:, :], r=[Ores], w=['o32'])
            o16, o16res = o16r.next()
            P.tt('pool', o16[:, :], o32[:, :], rec[:, :], ALU.mult, r=['o32', 'rec'], w=[o16res])
            P.dma('sp', mixT[hd, :, g * TS:(g + 1) * TS], o16[:, :], r=[o16res], final=True)
    return C


def build_l3(debug_xa=False):
    C = Ctx()
    P = C.P
    mixT = C.inp('mixT', [16, 128, NTOK], BF16)
    xT = C.inp('xT', [D, NTOK])
    w_out = C.inp('w_out', [D, D])
    ln1g = C.inp('ln1g', [128, NDC]); ln1b = C.inp('ln1b', [128, NDC])
    ln2g = C.inp('ln2g', [128, NDC]); ln2b = C.inp('ln2b', [128, NDC])
    wg_d = C.inp('wg', [D, DFF]); wu_d = C.inp('wu', [D, DFF]); wd_d = C.inp('wd', [DFF, D])
    w_qkv = C.inp('w_qkv', [D, 6144])
    w_f = C.inp('w_f', [D, 16]); b_f = C.inp('b_f', [16, 1])
    x1T = C.out('x1T', [D, NTOK])
    fq = C.out('fq', [16, 128, NTOK], BF16)
    fk = C.out('fk', [16, 128, NTOK], BF16)
    fv = C.out('fv', [16, 128, 16, 128], BF16)
    logf = C.out('logf', [16, NTOK])
    xaT = C.out('xaT', [D, NTOK]) if debug_xa else None
    K = load_consts(C)
    g1 = C.sb('g1', [128, NDC]); b1 = C.sb('b1', [128, NDC]); g2 = C.sb('g2', [128, NDC]); b2 = C.sb('b2', [128, NDC])
    P.dma('sp', g1[:, :], ln1g, w=['const_g1']); P.dma('sp', b1[:, :], ln1b, w=['const_b1'])
    P.dma('sp', g2[:, :], ln2g, w=['const_g2']); P.dma('sp', b2[:, :], ln2b, w=['const_b2'])
    wf32 = C.sb('wf32', [128, NDC, 16])
    P.dma('sp', wf32[:, :, :], w_f.rearrange("(dc p) n -> p dc n", p=128), w=['const_wf'])
    nbf = C.sb('nbf', [16, 1])
    P.dma('sp', nbf[:, :], b_f, w=['const_nbf'])
    P.op('act', lambda e: e.mul(out=nbf[:, :], in_=nbf[:, :], mul=-1.0), ['const_nbf'], ['const_nbf2'])

    mbuf = C.sb('mbuf', [128, NDC, TS], BF16)
    z = C.sb('z', [128, NDC, TS])
    xa16 = C.sb('xa16', [128, NDC, TS], BF16)
    h16 = C.sb('h16', [128, NFC, TS], BF16)
    wr = C.ring('w_', 4, [128, NDC, 256], BF16)
    wdr = C.ring('wd_', 3, [128, 4, TS], BF16)
    xcr = C.ring('xc_', 3, [128, TS], F32)
    scr = ln_scratch(C)
    sgr = C.ring('sg_', 2, [128, TS], F32)
    o16r = C.ring('o16_', 4, [128, TS], BF16)
    vsr_ = C.ring('vs_', 2, [128, 4, 128], BF16)
    lf = C.ring('lf_', 2, [16, TS], F32)
    accs = C.psring('acc', 2)
    gu = C.psring('gu', 4)
    s1 = (C.ps('s1'), 's1'); s2 = (C.ps('s2'), 's2')
    mixT_v = mixT.rearrange("c p t -> p c t")
    xT_v = xT.rearrange("(dc p) t -> p dc t", p=128)
    x1T_v = x1T.rearrange("(dc p) t -> p dc t", p=128)

    def wload(src_v, c_lo):
        w, wres = wr.next()
        cast_load(P, w, src_v[:, :, c_lo:c_lo + 256], NDC, wres, per_chunk=True)
        return w, wres

    for g in range(NT):
        t0 = g * TS
        for c in range(NDC):
            P.dma('sp', mbuf[:, c, :], mixT_v[:, c, t0:t0 + TS], w=[f'mbuf_{c}'])
        w_out_v = w_out.rearrange("(ic p) n -> p ic n", p=128)
        for og in range(8):
            w, wres = wload(w_out_v, og * 256)
            for o in range(2):
                oc = og * 2 + o
                acc, accr = accs.next()
                for ic in range(NDC):
                    P.matmul(acc[:, :], w[:, ic, o * 128:(o + 1) * 128], mbuf[:, ic, :], start=(ic == 0), stop=(ic == NDC - 1),
                             r=[f'{wres}_{ic}', f'mbuf_{ic}'], w=[accr])
                xc, xcres = xcr.next()
                P.dma('sp', xc[:, :], xT_v[:, oc, t0:t0 + TS], w=[xcres])
                P.op('act', lambda e, xc=xc: e.mul(out=xc[:, :], in_=xc[:, :], mul=ALPHA_C), [xcres], [xcres])
                P.stt('dve', z[:, oc, :], acc[:, :], 1.0, xc[:, :], ALU.mult, ALU.add, r=[accr, xcres], w=['z'])
        layernorm_fm(C, K, z, 'z', g1, b1, 'const_g1', z, 'z', xa16, 'xa16', s1, s2, scr)
        if debug_xa:
            for oc in range(NDC):
                P.dma('sp', xaT.rearrange("(dc p) t -> p dc t", p=128)[:, oc, t0:t0 + TS], z[:, oc, :], r=['z'], final=True)
        for oc in range(NDC):
            P.op('act', lambda e, oc=oc: e.mul(out=z[:, oc, :], in_=z[:, oc, :], mul=ALPHA_C), ['z'], ['z'])
        wg_v = wg_d.rearrange("(dc p) n -> p dc n", p=128)
        wu_v = wu_d.rearrange("(dc p) n -> p dc n", p=128)
        for fg in range(DFF // 256):
            wgt, wgres = wload(wg_v, fg * 256)
            wut, wures = wload(wu_v, fg * 256)
            for fc in range(2):
                ffc = fg * 2 + fc
                G, Gres = gu.next()
                U, Ures = gu.next()
                for dc in range(NDC):
                    P.matmul(G[:, :], wgt[:, dc, fc * 128:(fc + 1) * 128], xa16[:, dc, :], start=(dc == 0), stop=(dc == NDC - 1),
                             r=[f'{wgres}_{dc}', 'xa16'], w=[Gres])
                for dc in range(NDC):
                    P.matmul(U[:, :], wut[:, dc, fc * 128:(fc + 1) * 128], xa16[:, dc, :], start=(dc == 0), stop=(dc == NDC - 1),
                             r=[f'{wures}_{dc}', 'xa16'], w=[Ures])
                sg, sgres = sgr.next()
                P.activation(sg[:, :], G[:, :], AF.Silu, r=[Gres], w=[sgres])
                P.stt('dve', h16[:, ffc, :], U[:, :], 1.0, sg[:, :], ALU.mult, ALU.mult, r=[Ures, sgres], w=[f'h16_{ffc}'])
        wd_v = wd_d.rearrange("(f p) n -> p f n", p=128)
        for ocg in range(4):
            Dacc = [gu.next() for _ in range(4)]
            for fq4 in range(NFC // 4):
                wd, wdres = wdr.next()
                cast_load(P, wd, wd_v[:, fq4 * 4:(fq4 + 1) * 4, ocg * TS:(ocg + 1) * TS], 4, wdres, per_chunk=True)
                for j in range(4):
                    ffc = fq4 * 4 + j
                    for o in range(4):
                        P.matmul(Dacc[o][0][:, :], wd[:, j, o * 128:(o + 1) * 128], h16[:, ffc, :], start=(ffc == 0), stop=(ffc == NFC - 1),
                                 r=[f'{wdres}_{j}', f'h16_{ffc}'], w=[Dacc[o][1]])
            for o in range(4):
                oc = ocg * 4 + o
                P.stt('dve', z[:, oc, :], Dacc[o][0][:, :], 1.0, z[:, oc, :], ALU.mult, ALU.add, r=[Dacc[o][1], 'z'], w=['z'])
        layernorm_fm(C, K, z, 'z', g2, b2, 'const_g2', z, 'z', mbuf, lambda oc: f'mbuf_{oc}', s1, s2, scr)
        for oc in range(NDC):
            P.dma('sp', x1T_v[:, oc, t0:t0 + TS], z[:, oc, :], r=['z'], final=True)
        w_qkv_v = w_qkv.rearrange("(dc p) n -> p dc n", p=128)
        for qg in range(16):
            w, wres = wload(w_qkv_v, qg * 256)
            for j in range(2):
                ch = qg * 2 + j
                acc, accr = accs.next()
                for dc in range(NDC):
                    P.matmul(acc[:, :], w[:, dc, j * 128:(j + 1) * 128], mbuf[:, dc, :], start=(dc == 0), stop=(dc == NDC - 1),
                             r=[f'{wres}_{dc}', f'mbuf_{dc}'], w=[accr])
                o16, o16res = o16r.next()
                P.copy('act', o16[:, :], acc[:, :], r=[accr], w=[o16res])
                dst = fq[ch, :, t0:t0 + TS] if ch < 16 else fk[ch - 16, :, t0:t0 + TS]
                P.dma('sp', dst, o16[:, :], r=[o16res], final=True)
        for vg in range(8):
            w, wres = wload(w_qkv_v, 4096 + vg * 256)
            for tb in range(4):
                acc, accr = accs.next()
                for dc in range(NDC):
                    P.matmul(acc[:, 0:256], mbuf[:, dc, tb * 128:(tb + 1) * 128], w[:, dc, :], start=(dc == 0), stop=(dc == NDC - 1),
                             r=[f'{wres}_{dc}', f'mbuf_{dc}'], w=[accr])
                vs, vsres = vsr_.next()
                P.copy('act', vs[:, 0:2, :], acc[:, 0:256].rearrange("p (h c) -> p h c", c=128), r=[accr], w=[vsres])
                P.dma('sp', fv[vg * 2:vg * 2 + 2, :, g * 4 + tb, :].rearrange("h p c -> p h c"), vs[:, 0:2, :], r=[vsres], final=True)
        acc, accr = accs.next()
        for dc in range(NDC):
            P.matmul(acc[0:16, :], wf32[:, dc, :], z[:, dc, :], start=(dc == 0), stop=(dc == NDC - 1), r=['const_wf', 'z'], w=[accr])
        l, lres = lf.next()
        P.activation(l[:, :], acc[0:16, :], AF.Exp, r=[accr, 'const_nbf2'], w=[lres], scale=-1.0, bias=nbf[:, 0:1])
        P.activation(l[:, :], l[:, :], AF.Ln, r=[lres], w=[lres], bias=1.0)
        P.op('act', lambda e, l=l: e.mul(out=l[:, :], in_=l[:, :], mul=-1.0), [lres], [lres])
        P.dma('sp', logf[:, t0:t0 + TS], l[:, :], r=[lres], final=True)
    return C


def l3_inputs(inp, mixT_list):
    common = {
        'w_out': np.ascontiguousarray(inp['ev_w_out'][0]),
        'ln1g': fm_vec(inp['ev_ln1_g'][0], NDC), 'ln1b': fm_vec(inp['ev_ln1_b'][0], NDC),
        'ln2g': fm_vec(inp['ev_ln2_g'][0], NDC), 'ln2b': fm_vec(inp['ev_ln2_b'][0], NDC),
        'wg': np.ascontiguousarray(inp['ev_ffn_w_gate'][0]), 'wu': np.ascontiguousarray(inp['ev_ffn_w_up'][0]),
        'wd': np.ascontiguousarray(inp['ev_ffn_w_down'][0]), 'w_qkv': np.ascontiguousarray(inp['od_w_qkv'][0]),
        'w_f': np.ascontiguousarray(inp['od_w_f'][0]), 'b_f': np.ascontiguousarray(inp['od_b_f'][0].reshape(16, 1)),
    }
    maps = []
    for c in range(8):
        m = dict(common)
        m['mixT'] = mixT_list[c]
        m['xT'] = np.ascontiguousarray(inp['x'][c // 4][core_positions(c)].T)
        maps.append(m)
    return maps


def build_l4():
    C = Ctx()
    P = C.P
    fq = C.inp('fq', [16, 128, NTOK], BF16)
    fkf = C.inp('fkf', [4, 16, 128, NTOK], BF16)
    fvf = C.inp('fvf', [4, 16, 128, 16, 128], BF16)
    logff = C.inp('logff', [4, 16, NTOK])
    cmask = C.inp('cmask', [128, 16, TS], BF16)
    onehot = C.inp('onehot', [16, 4])
    sel_d = C.inp('sel', [16, 16, 128])
    id_d = C.inp('ident16', [16, 16])
    mixF = C.out('mixF', [16, 128, NTOK], BF16)
    K = load_consts(C)
    scale = 128.0 ** -0.5
    cm = C.sb('cm_sb', [128, 16, TS], BF16)
    cast_load(P, cm, cmask, 16, 'const_cm', q='sp', maxcols=TS, per_chunk=True)
    lf_sb = C.sb('lf_sb', [16, 4, NTOK])
    cT = C.sb('cT', [16, 4, NTOK])
    for r2 in range(4):
        P.dma('sp', lf_sb[:, r2, :], logff[r2, :, :], w=[f'lf_{r2}'])
    oh = C.sb('oh_sb', [16, 4]); P.dma('sp', oh[:, :], onehot, w=['const_oh'])
    sel = C.sb('sel_sb', [16, 16, 128]); P.dma('sp', sel[:, :, :], sel_d, w=['const_sel'])
    id16 = C.sb('id16', [16, 16]); P.dma('sp', id16[:, :], id_d, w=['const_id'])
    ones_s = C.sb('ones_s', [16, TS]); P.memset('dve', ones_s[:, :], 1.0, w=['const_ones_s'])
    prev = None
    for j in range(16):
        g2, r2 = j // 4, j % 4
        seg = lf_sb[:, r2, g2 * TS:(g2 + 1) * TS]
        out = cT[:, r2, g2 * TS:(g2 + 1) * TS]
        init = 0.0 if prev is None else prev
        rd = [f'lf_{r2}', 'const_ones_s'] + ([] if prev is None else [f'cT_{j - 1}'])
        P.op('dve', lambda e, out=out, seg=seg, init=init: e.tensor_tensor_scan(out=out, data0=ones_s[:, :], data1=seg, initial=init,
                                                                                 op0=ALU.mult, op1=ALU.add), rd, [f'cT_{j}'])
        prev = cT[:, r2, (g2 + 1) * TS - 1:(g2 + 1) * TS]
    allc = [f'cT_{j}' for j in range(16)]
    c_own = C.sb('c_own', [16, NT, TS])
    for g in range(NT):
        P.ts('dve', c_own[:, g, :], cT[:, 0, g * TS:(g + 1) * TS], oh[:, 0:1], None, ALU.mult, r=allc + ['const_oh'], w=[f'cown_{g}'])
        for r2 in range(1, 4):
            P.stt('dve', c_own[:, g, :], cT[:, r2, g * TS:(g + 1) * TS], oh[:, r2:r2 + 1], c_own[:, g, :], ALU.mult, ALU.add,
                  r=allc + ['const_oh', f'cown_{g}'], w=[f'cown_{g}'])
    c_tok = C.sb('c_tok', [128, 64, 16])
    ctp = C.psring('ctp', 2)
    for half in range(2):
        ps, psr = ctp.next()
        for i in range(32):
            blk = half * 32 + i
            r2, lb = blk // 16, blk % 16
            P.matmul(ps[:, i * 16:(i + 1) * 16], cT[:, r2, lb * 128:(lb + 1) * 128], id16[:, :], r=allc + ['const_id'], w=[psr])
        P.copy('act', c_tok[:, half * 32:(half + 1) * 32, :], ps[:, :].rearrange("p (b h) -> p b h", h=16), r=[psr], w=['c_tok'])
    kT = C.sb('kT', [128, 4, NTOK], BF16)
    v = C.sb('v', [128, 4, 16 * 128], BF16)
    qr_ = C.ring('q_', 2, [128, NTOK], BF16)
    Sr = C.psring('S', 2)
    Or = C.psring('O', 2)
    Dr = C.psring('Dn', 2)
    pTr = C.ring('pT_', 3, [128, TS], BF16)
    tmpr = C.ring('tmp_', 3, [128, TS], F32)
    cqr = C.sb('cqr', [128, TS])
    cref = C.sb('cref', [128, 1])
    bias_all = C.sb('bias_all', [128, 64])
    rec = C.sb('rec', [128, TS])
    o32 = C.sb('o32', [128, TS])
    o16r = C.ring('ao16_', 2, [128, TS], BF16)
    for h in range(16):
        q, qres = qr_.next()
        for r2 in range(4):
            P.dma('sp', kT[:, r2, :], fkf[r2, h, :, :], w=[f'kT_{r2}'])
            P.dma('sp', v[:, r2, :], fvf[r2, h, :, :, :].rearrange("p b c -> p (b c)"), w=[f'v_{r2}'])
        P.dma('sp', q[:, :], fq[h, :, :], w=[qres])
        for g in range(NT):
            cqb, cqbr = ctp.next()
            P.matmul(cqb[:, :], sel[:, h, :], c_own[:, g, :], r=['const_sel', f'cown_{g}'], w=[cqbr])
            P.copy('act', cref[:, :], cqb[:, 0:1], r=[cqbr], w=['cref'])
            P.ts('dve', cqr[:, :], cqb[:, :], cref[:, 0:1], None, ALU.subtract, r=[cqbr, 'cref'], w=['cqr'])
            P.ts('dve', bias_all[:, :], c_tok[:, :, h], cref[:, 0:1], -1.0, ALU.subtract, ALU.mult, r=['c_tok', 'cref'], w=['bias_all'])
            O, Ores = Or.next()
            Dn, Dres = Dr.next()
            nblk = 16 * (g + 1)
            idx = 0
            for g2 in range(g + 1):
                for r2 in range(4):
                    for kb in range(4):
                        S, Sres = Sr.next()
                        pT, pTres = pTr.next()
                        tmp, tmpres = tmpr.next()
                        c0 = g2 * TS + kb * 128
                        lb = g2 * 4 + kb
                        P.matmul(S[:, :], kT[:, r2, c0:c0 + 128], q[:, g * TS:(g + 1) * TS], r=[f'kT_{r2}', qres], w=[Sres])
                        P.stt('dve', tmp[:, :], S[:, :], scale, cqr[:, :], ALU.mult, ALU.add, r=[Sres, 'cqr'], w=[tmpres])
                        if g2 == g:
                            P.stt('dve', tmp[:, :], tmp[:, :], 1.0, cm[:, r2 * 4 + kb, :], ALU.mult, ALU.add,
                                  r=[tmpres, f'const_cm_{r2 * 4 + kb}'], w=[tmpres])
                        bcol = r2 * 16 + lb
                        P.activation(pT[:, :], tmp[:, :], AF.Exp, r=[tmpres, 'bias_all'], w=[pTres], bias=bias_all[:, bcol:bcol + 1])
                        P.matmul(O[:, :], v[:, r2, lb * 128:(lb + 1) * 128], pT[:, :], start=(idx == 0), stop=(idx == nblk - 1),
                                 r=[f'v_{r2}', pTres], w=[Ores])
                        P.matmul(Dn[:, :], K['ones16'][:, :], pT[:, :], start=(idx == 0), stop=(idx == nblk - 1),
                                 r=['const_ones16', pTres], w=[Dres])
                        idx += 1
            P.recip(rec[:, :], Dn[:, :], r=[Dres], w=['rec'])
            P.copy('act', o32[:, :], O[:, :], r=[Ores], w=['o32'])
            o16, o16res = o16r.next()
            P.tt('pool', o16[:, :], o32[:, :], rec[:, :], ALU.mult, r=['o32', 'rec'], w=[o16res])
            P.dma('sp', mixF[h, :, g * TS:(g + 1) * TS], o16[:, :], r=[o16res], final=True)
    return C


def l4_inputs(r3):
    sel = np.zeros((16, 16, 128), np.float32)
    for h in range(16):
        sel[h, h, :] = 1.0
    maps = []
    for c in range(8):
        b, r = c // 4, c % 4
        grp = [r3[4 * b + r2] for r2 in range(4)]
        oh = np.zeros((16, 4), np.float32)
        oh[:, r] = 1.0
        maps.append({'fq': r3[c]['fq'], 'fkf': np.stack([g['fk'] for g in grp]), 'fvf': np.stack([g['fv'] for g in grp]),
                     'logff': np.stack([g['logf'] for g in grp]), 'cmask': causal_mask_tiles(r), 'onehot': oh, 'sel': sel,
                     'ident16': np.eye(16, dtype=np.float32)})
    return maps


def build_l5a():
    C = Ctx()
    P = C.P
    mixT = C.inp('mixT', [16, 128, NTOK], BF16)
    xT = C.inp('xT', [D, NTOK])
    w_out = C.inp('w_out', [D, D])
    ln1g = C.inp('ln1g', [128, NDC]); ln1b = C.inp('ln1b', [128, NDC])
    rw = C.inp('rw', [D, NEXP]); rb = C.inp('rb', [1, NEXP])
    xbT = C.out('xbT', [D, NTOK])
    xb16T = C.out('xb16T', [D, NTOK], BF16)
    gates = C.out('gates', [NTOK, NEXP])
    K = load_consts(C)
    g1 = C.sb('g1', [128, NDC]); b1 = C.sb('b1', [128, NDC])
    P.dma('sp', g1[:, :], ln1g, w=['const_g1']); P.dma('sp', b1[:, :], ln1b, w=['const_b1'])
    rw32 = C.sb('rw32', [128, NDC, NEXP])
    P.dma('sp', rw32[:, :, :], rw.rearrange("(dc p) n -> p dc n", p=128), w=['const_rw'])
    rb32 = C.sb('rb32', [1, NEXP]); P.dma('sp', rb32[:, :], rb, w=['const_rb'])
    mbuf = C.sb('mbuf', [128, NDC, TS], BF16)
    xb16 = C.sb('xb16', [128, NDC, TS], BF16)
    z = C.sb('z', [128, NDC, TS])
    wr = C.ring('w_', 4, [128, NDC, 256], BF16)
    xcr = C.ring('xc_', 3, [128, TS], F32)
    scr = ln_scratch(C)
    accs = C.psring('acc', 2)
    s1 = (C.ps('s1'), 's1'); s2 = (C.ps('s2'), 's2')
    lgp = C.psring('lgp', 2)
    sm = {n: C.ring(n + '_', 2, [128, NEXP], F32) for n in ('l', 'mk', 'l2', 'sel', 'e', 'gt')}
    s1c = {n: C.ring(n + '_', 2, [128, 1], F32) for n in ('m1', 'm2', 'nm1', 'den')}
    mixT_v = mixT.rearrange("c p t -> p c t")
    xT_v = xT.rearrange("(dc p) t -> p dc t", p=128)
    xbT_v = xbT.rearrange("(dc p) t -> p dc t", p=128)
    xb16T_v = xb16T.rearrange("(dc p) t -> p dc t", p=128)
    w_out_v = w_out.rearrange("(ic p) n -> p ic n", p=128)
    for g in range(NT):
        t0 = g * TS
        for c in range(NDC):
            P.dma('sp', mbuf[:, c, :], mixT_v[:, c, t0:t0 + TS], w=[f'mbuf_{c}'])
        for og in range(8):
            w, wres = wr.next()
            cast_load(P, w, w_out_v[:, :, og * 256:(og + 1) * 256], NDC, wres, per_chunk=True)
            for o in range(2):
                oc = og * 2 + o
                acc, accr = accs.next()
                for ic in range(NDC):
                    P.matmul(acc[:, :], w[:, ic, o * 128:(o + 1) * 128], mbuf[:, ic, :], start=(ic == 0), stop=(ic == NDC - 1),
                             r=[f'{wres}_{ic}', f'mbuf_{ic}'], w=[accr])
                xc, xcres = xcr.next()
                P.dma('sp', xc[:, :], xT_v[:, oc, t0:t0 + TS], w=[xcres])
                P.op('act', lambda e, xc=xc: e.mul(out=xc[:, :], in_=xc[:, :], mul=ALPHA_C), [xcres], [xcres])
                P.stt('dve', z[:, oc, :], acc[:, :], 1.0, xc[:, :], ALU.mult, ALU.add, r=[accr, xcres], w=['z'])
        layernorm_fm(C, K, z, 'z', g1, b1, 'const_g1', z, 'z', xb16, 'xb16', s1, s2, scr)
        for oc in range(NDC):
            P.dma('sp', xbT_v[:, oc, t0:t0 + TS], z[:, oc, :], r=['z'], final=True)
            P.dma('sp', xb16T_v[:, oc, t0:t0 + TS], xb16[:, oc, :], r=['xb16'], final=True)
        for tb in range(4):
            lg, lgr = lgp.next()
            for dc in range(NDC):
                P.matmul(lg[:, 0:NEXP], z[:, dc, tb * 128:(tb + 1) * 128], rw32[:, dc, :], start=(dc == 0), stop=False,
                         r=['z', 'const_rw'], w=[lgr])
            P.matmul(lg[:, 0:NEXP], K['ones32'][0:1, :], rb32[0:1, :], start=False, stop=True, r=['const_ones32', 'const_rb'], w=[lgr])
            l, lr_ = sm['l'].next(); mk, mkr = sm['mk'].next(); l2, l2r = sm['l2'].next()
            sl, slr = sm['sel'].next(); ee, eer = sm['e'].next(); gt, gtr = sm['gt'].next()
            m1, m1r = s1c['m1'].next(); m2, m2r = s1c['m2'].next(); nm1, nm1r = s1c['nm1'].next(); den, denr = s1c['den'].next()
            P.copy('act', l[:, :], lg[:, 0:NEXP], r=[lgr], w=[lr_])
            P.op('dve', lambda e, m1=m1, l=l: e.tensor_reduce(out=m1[:, :], in_=l[:, :], axis=AX.X, op=ALU.max), [lr_], [m1r])
            P.ts('dve', mk[:, :], l[:, :], m1[:, 0:1], None, ALU.is_equal, r=[lr_, m1r], w=[mkr])
            P.stt('dve', l2[:, :], mk[:, :], -1.0e30, l[:, :], ALU.mult, ALU.add, r=[mkr, lr_], w=[l2r])
            P.op('dve', lambda e, m2=m2, l2=l2: e.tensor_reduce(out=m2[:, :], in_=l2[:, :], axis=AX.X, op=ALU.max), [l2r], [m2r])
            P.ts('dve', sl[:, :], l[:, :], m2[:, 0:1], None, ALU.is_ge, r=[lr_, m2r], w=[slr])
            P.ts('dve', nm1[:, :], m1[:, :], -1.0, None, ALU.mult, r=[m1r], w=[nm1r])
            P.activation(ee[:, :], l[:, :], AF.Exp, r=[lr_, nm1r], w=[eer], bias=nm1[:, 0:1])
            P.stt('dve', ee[:, :], ee[:, :], 1.0, sl[:, :], ALU.mult, ALU.mult, r=[eer, slr], w=[eer])
            P.op('dve', lambda e, den=den, ee=ee: e.tensor_reduce(out=den[:, :], in_=ee[:, :], axis=AX.X, op=ALU.add), [eer], [denr])
            P.recip(den[:, :], den[:, :], r=[denr], w=[denr])
            P.ts('dve', gt[:, :], ee[:, :], den[:, 0:1], None, ALU.mult, r=[eer, denr], w=[gtr])
            P.dma('sp', gates[t0 + tb * 128:t0 + (tb + 1) * 128, :], gt[:, :], r=[gtr], final=True)
    return C


NALL = 8 * NTOK


def build_l5b():
    C = Ctx()
    P = C.P
    xall = C.inp('xall', [D, NALL], BF16)
    grow = C.inp('grow', [1, NALL])
    wg_d = C.inp('wg', [D, DFF]); wu_d = C.inp('wu', [D, DFF]); wd_d = C.inp('wd', [DFF, D])
    yT = C.out('yT', [D, NALL], BF16)
    xa16r = C.ring('xa16_', 2, [128, NDC, TS], BF16)
    gbr = C.ring('gb_', 2, [128, TS], F32)
    h16 = C.sb('h16', [128, NFC, TS], BF16)
    wr = C.ring('w_', 4, [128, NDC, 256], BF16)
    wdr = C.ring('wd_', 3, [128, 4, TS], BF16)
    sgr = C.ring('sg_', 3, [128, TS], F32)
    o16r = C.ring('o16_', 4, [128, TS], BF16)
    gu = C.psring('gu', 8)
    xall_v = xall.rearrange("(dc p) t -> p dc t", p=128)
    yT_v = yT.rearrange("(dc p) t -> p dc t", p=128)
    wg_v = wg_d.rearrange("(dc p) n -> p dc n", p=128)
    wu_v = wu_d.rearrange("(dc p) n -> p dc n", p=128)
    wd_v = wd_d.rearrange("(f p) n -> p f n", p=128)
    for tl in range(NALL // TS):
        t0 = tl * TS
        xa16, xares = xa16r.next()
        for dc in range(NDC):
            P.dma('sp', xa16[:, dc, :], xall_v[:, dc, t0:t0 + TS], w=[f'{xares}_{dc}'])
        gb, gbres = gbr.next()
        P.dma('sp', gb[:, :], grow[0:1, t0:t0 + TS].partition_broadcast(128), w=[gbres])
        for fg in range(DFF // 256):
            wgt, wgres = wr.next()
            cast_load(P, wgt, wg_v[:, :, fg * 256:(fg + 1) * 256], NDC, wgres, per_chunk=True)
            wut, wures = wr.next()
            cast_load(P, wut, wu_v[:, :, fg * 256:(fg + 1) * 256], NDC, wures, per_chunk=True)
            for fc in range(2):
                ffc = fg * 2 + fc
                G, Gres = gu.next()
                U, Ures = gu.next()
                for dc in range(NDC):
                    P.matmul(G[:, :], wgt[:, dc, fc * 128:(fc + 1) * 128], xa16[:, dc, :], start=(dc == 0), stop=(dc == NDC - 1),
                             r=[f'{wgres}_{dc}', f'{xares}_{dc}'], w=[Gres])
                for dc in range(NDC):
                    P.matmul(U[:, :], wut[:, dc, fc * 128:(fc + 1) * 128], xa16[:, dc, :], start=(dc == 0), stop=(dc == NDC - 1),
                             r=[f'{wures}_{dc}', f'{xares}_{dc}'], w=[Ures])
                sg, sgres = sgr.next()
                P.activation(sg[:, :], G[:, :], AF.Silu, r=[Gres], w=[sgres])
                P.tt('pool', sg[:, :], sg[:, :], gb[:, :], ALU.mult, r=[sgres, gbres], w=[sgres])
                P.stt('dve', h16[:, ffc, :], U[:, :], 1.0, sg[:, :], ALU.mult, ALU.mult, r=[Ures, sgres], w=[f'h16_{ffc}'])
        for ocg in range(4):
            Dacc = [gu.next() for _ in range(4)]
            for fq4 in range(NFC // 4):
                wd, wdres = wdr.next()
                cast_load(P, wd, wd_v[:, fq4 * 4:(fq4 + 1) * 4, ocg * TS:(ocg + 1) * TS], 4, wdres, per_chunk=True)
                for j in range(4):
                    ffc = fq4 * 4 + j
                    for o in range(4):
                        P.matmul(Dacc[o][0][:, :], wd[:, j, o * 128:(o + 1) * 128], h16[:, ffc, :], start=(ffc == 0), stop=(ffc == NFC - 1),
                                 r=[f'{wdres}_{j}', f'h16_{ffc}'], w=[Dacc[o][1]])
            for o in range(4):
                oc = ocg * 4 + o
                o16, o16res = o16r.next()
                P.copy('act', o16[:, :], Dacc[o][0][:, :], r=[Dacc[o][1]], w=[o16res])
                P.dma('sp', yT_v[:, oc, t0:t0 + TS], o16[:, :], r=[o16res], final=True)
    return C


def build_l5c():
    C = Ctx()
    P = C.P
    yparts = C.inp('yparts', [NEXP, D, NTOK], BF16)
    xT = C.inp('xT', [D, NTOK])
    ln2g = C.inp('ln2g', [128, NDC]); ln2b = C.inp('ln2b', [128, NDC])
    outT = C.out('outT', [D, NTOK])
    K = load_consts(C)
    g2 = C.sb('g2', [128, NDC]); b2 = C.sb('b2', [128, NDC])
    P.dma('sp', g2[:, :], ln2g, w=['const_g2']); P.dma('sp', b2[:, :], ln2b, w=['const_b2'])
    z = C.sb('z', [128, NDC, TS])
    x16 = C.sb('x16d', [128, NDC, TS], BF16)
    ypr = C.ring('yp_', 4, [128, NEXP, TS], BF16)
    scr = ln_scratch(C)
    s1 = (C.ps('s1'), 's1'); s2 = (C.ps('s2'), 's2')
    xT_v = xT.rearrange("(dc p) t -> p dc t", p=128)
    outT_v = outT.rearrange("(dc p) t -> p dc t", p=128)
    yp_v = yparts.rearrange("e (dc p) t -> p e dc t", p=128)
    for g in range(NT):
        t0 = g * TS
        for oc in range(NDC):
            P.dma('sp', z[:, oc, :], xT_v[:, oc, t0:t0 + TS], w=[f'z_{oc}'])
            P.op('act', lambda e, oc=oc: e.mul(out=z[:, oc, :], in_=z[:, oc, :], mul=ALPHA_C), [f'z_{oc}'], [f'z_{oc}'])
            yp, ypres = ypr.next()
            for ex in range(NEXP):
                P.dma('sp', yp[:, ex, :], yp_v[:, ex, oc, t0:t0 + TS], w=[f'{ypres}_{ex}'])
            for ex in range(NEXP):
                P.stt('dve', z[:, oc, :], yp[:, ex, :], 1.0, z[:, oc, :], ALU.mult, ALU.add, r=[f'{ypres}_{ex}', f'z_{oc}'], w=[f'z_{oc}'])
        zall = [f'z_{oc}' for oc in range(NDC)]
        layernorm_fm(C, K, z, zall, g2, b2, 'const_g2', z, 'zo', x16, 'x16d', s1, s2, scr)
        for oc in range(NDC):
            P.dma('sp', outT_v[:, oc, t0:t0 + TS], z[:, oc, :], r=['zo'], final=True)
    return C


def l5a_inputs(inp, mixF_list, x1T_list):
    common = {'w_out': np.ascontiguousarray(inp['od_w_out'][0]),
              'ln1g': fm_vec(inp['od_ln1_g'][0], NDC), 'ln1b': fm_vec(inp['od_ln1_b'][0], NDC),
              'rw': np.ascontiguousarray(inp['od_router_w'][0]), 'rb': np.ascontiguousarray(inp['od_router_b'][0].reshape(1, NEXP))}
    return [dict(common, mixT=mixF_list[c], xT=x1T_list[c]) for c in range(8)]


def l5b_inputs(inp, r5a):
    xall = np.ascontiguousarray(np.concatenate([r5a[c]['xb16T'] for c in range(8)], axis=1))
    gall = np.concatenate([r5a[c]['gates'] for c in range(8)], axis=0)
    maps = []
    for e in range(NEXP):
        maps.append({'xall': xall, 'grow': np.ascontiguousarray(gall[:, e].reshape(1, NALL)),
                     'wg': np.ascontiguousarray(inp['od_exp_w_gate'][0, e]), 'wu': np.ascontiguousarray(inp['od_exp_w_up'][0, e]),
                     'wd': np.ascontiguousarray(inp['od_exp_w_down'][0, e])})
    return maps


def l5c_inputs(inp, r5a, r5b):
    g2, b2 = fm_vec(inp['od_ln2_g'][0], NDC), fm_vec(inp['od_ln2_b'][0], NDC)
    maps = []
    for c in range(8):
        yp = np.ascontiguousarray(np.stack([r5b[e]['yT'][:, c * NTOK:(c + 1) * NTOK] for e in range(NEXP)]))
        maps.append({'yparts': yp, 'xT': r5a[c]['xbT'], 'ln2g': g2, 'ln2b': b2})
    return maps


def assemble_output(r5c):
    out = np.empty((2, 8192, D), np.float32)
    for c in range(8):
        out[c // 4][core_positions(c)] = r5c[c]['outT'].T
    return out


def kernel(**inputs):
    inp = {k: np.asarray(v) for k, v in inputs.items()}
    r1 = build_l1().run(l1_inputs(inp))
    m2, m2d = [], []
    for c in range(8):
        b, r = c // 4, c % 4
        grp = [r1[4 * b + r2] for r2 in range(4)]
        m2.append({'qm': r1[c]['qm'], 'kmf': np.stack([g['km'] for g in grp]), 'krf': np.stack([g['kr'] for g in grp]),
                   'vmf': np.stack([g['vm'] for g in grp]), 'cmask': causal_mask_tiles(r)})
        m2d.append({'dq': r1[c]['dq'], 'dkf': np.stack([g['dk'] for g in grp]), 'dvf': np.stack([g['dv'] for g in grp]),
                    'dmask': dil_mask_tiles(r)})
    r2 = build_l2_mla().run(m2)
    r2d = build_l2_dil().run(m2d)
    del r1, m2, m2d
    mix0 = [np.ascontiguousarray(np.concatenate([r2[c]['mixT'], r2d[c]['mixD']], axis=0)) for c in range(8)]
    r3 = build_l3().run(l3_inputs(inp, mix0))
    r4 = build_l4().run(l4_inputs(r3))
    r5a = build_l5a().run(l5a_inputs(inp, [r4[c]['mixF'] for c in range(8)], [r3[c]['x1T'] for c in range(8)]))
    del r3, r4
    r5b = build_l5b().run(l5b_inputs(inp, r5a))
    r5c = build_l5c().run(l5c_inputs(inp, r5a, r5b))
    return assemble_output(r5c)
```

```python
import numpy as np
import concourse.bass as bass
import concourse.mybir as mybir
from concourse.bass_utils import run_bass_kernel_spmd

F32 = mybir.dt.float32
BF16 = mybir.dt.bfloat16
I32 = mybir.dt.int32
AF = mybir.ActivationFunctionType
ALU = mybir.AluOpType
AX = mybir.AxisListType

ENGS = ['pe', 'act', 'dve', 'pool', 'sp']
SEM_CAP = 30000
DMA_POOL = 12


class Op:
    __slots__ = ('eng', 'fn', 'reads', 'writes', 'dma', 'idx', 'pos', 'waits', 'snap',
                 'milestone', 'ms', 'dsem', 'dval', 'dprev')

    def __init__(self, eng, fn, reads, writes, dma):
        self.eng = eng
        self.fn = fn
        self.reads = reads
        self.writes = writes
        self.dma = dma
        self.milestone = False
        self.waits = ()
        self.ms = -1


class Prog:
    def __init__(self, nc, same_sync=True):
        self.nc = nc
        self.ops = []
        self.same_sync = same_sync
        self.final_dmas = []

    def op(self, eng, fn, reads=(), writes=(), dma=False):
        o = Op(eng, fn, tuple(reads), tuple(writes), dma)
        self.ops.append(o)
        return o

    def pe(self, fn, r=(), w=()):
        return self.op('pe', fn, r, w)

    def act(self, fn, r=(), w=()):
        return self.op('act', fn, r, w)

    def dve(self, fn, r=(), w=()):
        return self.op('dve', fn, r, w)

    def pool(self, fn, r=(), w=()):
        return self.op('pool', fn, r, w)

    def dma(self, q, out, in_, r=(), w=(), final=False, **kw):
        o = self.op(q, lambda e: e.dma_start(out=out, in_=in_, **kw), r, w, dma=True)
        if final:
            self.final_dmas.append(o)
        return o

    def matmul(self, out, lhsT, rhs, start=True, stop=True, r=(), w=()):
        return self.op('pe', lambda e: e.matmul(out, lhsT, rhs, start=start, stop=stop), r, w)

    def transpose(self, out, in_, ident, r=(), w=()):
        return self.op('pe', lambda e: e.transpose(out, in_, ident), r, w)

    def activation(self, out, in_, func, r=(), w=(), **kw):
        return self.op('act', lambda e: e.activation(out=out, in_=in_, func=func, **kw), r, w)

    def tt(self, eng, out, in0, in1, op, r=(), w=()):
        return self.op(eng, lambda e: e.tensor_tensor(out=out, in0=in0, in1=in1, op=op), r, w)

    def ts(self, eng, out, in0, s1, s2, op0, op1=None, r=(), w=(), **kw):
        if op1 is None:
            return self.op(eng, lambda e: e.tensor_scalar(out=out, in0=in0, scalar1=s1, scalar2=s2, op0=op0, **kw), r, w)
        return self.op(eng, lambda e: e.tensor_scalar(out=out, in0=in0, scalar1=s1, scalar2=s2, op0=op0, op1=op1, **kw), r, w)

    def stt(self, eng, out, in0, scalar, in1, op0, op1, r=(), w=()):
        return self.op(eng, lambda e: e.scalar_tensor_tensor(out=out, in0=in0, scalar=scalar, in1=in1, op0=op0, op1=op1), r, w)

    def copy(self, eng, out, in_, r=(), w=()):
        if eng == 'act':
            return self.op(eng, lambda e: e.copy(out=out, in_=in_), r, w)
        return self.op(eng, lambda e: e.tensor_copy(out=out, in_=in_), r, w)

    def memset(self, eng, ap, val, w=()):
        return self.op(eng, lambda e: e.memset(ap, val), (), w)

    def recip(self, out, in_, r=(), w=()):
        return self.op('dve', lambda e: e.reciprocal(out=out, in_=in_), r, w)

    def analyze(self):
        per = {e: [] for e in ENGS}
        for i, o in enumerate(self.ops):
            o.idx = i
            o.pos = len(per[o.eng])
            per[o.eng].append(o)
        self.per = per
        last_w = {}
        readers = {}
        known = {e: {f: -1 for f in ENGS} for e in ENGS}
        known_dma = {e: set() for e in ENGS}
        for o in self.ops:
            deps = {}
            for r in o.reads:
                w = last_w.get(r)
                if w is not None:
                    deps[w.idx] = w
            for r in o.writes:
                w = last_w.get(r)
                if w is not None:
                    deps[w.idx] = w
                rd = readers.get(r)
                if rd:
                    for x in rd.values():
                        deps[x.idx] = x
            deps.pop(o.idx, None)
            kn = known[o.eng]
            kd = known_dma[o.eng]
            waits = []
            for d in deps.values():
                if d.dma:
                    if d.idx in kd:
                        continue
                    waits.append(d)
                else:
                    if d.eng == o.eng and not o.dma:
                        if d.eng == 'pe' or not self.same_sync:
                            continue
                    if kn[d.eng] >= d.pos:
                        continue
                    waits.append(d)
            for d in waits:
                d.milestone = True
                if d.dma:
                    kd.add(d.idx)
                else:
                    if kn[d.eng] < d.pos:
                        kn[d.eng] = d.pos
                for f, p in d.snap.items():
                    if kn[f] < p:
                        kn[f] = p
            o.waits = waits
            o.snap = dict(kn)
            for r in o.writes:
                last_w[r] = o
                readers[r] = {}
            for r in o.reads:
                if r.startswith('const'):
                    continue
                rd = readers.setdefault(r, {})
                key = o.idx if o.dma else o.eng
                rd[key] = o
        for o in self.final_dmas:
            o.milestone = True

    def emit(self):
        nc = self.nc
        self.analyze()
        per = self.per
        import contextlib
        with contextlib.ExitStack() as st:
            sems = {}
            for e in ENGS:
                n = 0
                for o in per[e]:
                    if o.dma:
                        continue
                    if o.milestone:
                        o.ms = n
                        n += 1
                nsem = n // SEM_CAP + 1
                sems[e] = [st.enter_context(nc.semaphore(f"ms_{e}_{k}")) for k in range(nsem)]
            dpool = {}
            for e in ENGS:
                dm = [o for o in per[e] if o.dma]
                if not dm:
                    continue
                pool = [st.enter_context(nc.semaphore(f"dq_{e}_{k}")) for k in range(DMA_POOL)]
                vals = [0] * DMA_POOL
                k = 0
                for o in dm:
                    o.dsem = pool[k]
                    o.dprev = vals[k]
                    vals[k] += 16
                    o.dval = vals[k]
                    k = (k + 1) % DMA_POOL
            block = st.enter_context(nc.Block())

            def run(e, eng):
                for o in per[e]:
                    for d in o.waits:
                        if d.dma:
                            eng.wait_ge(d.dsem, d.dval)
                        else:
                            eng.wait_ge(sems[d.eng][d.ms // SEM_CAP], d.ms % SEM_CAP + 1)
                    if o.dma:
                        if o.dprev > 0:
                            eng.wait_ge(o.dsem, o.dprev)
                        ins = o.fn(eng)
                        ins.then_inc(o.dsem, 16)
                    else:
                        ins = o.fn(eng)
                        if o.milestone:
                            ins.then_inc(sems[e][o.ms // SEM_CAP], 1)
                if e == 'sp':
                    for o in self.final_dmas:
                        eng.wait_ge(o.dsem, o.dval)

            if per['pe']:
                block.tensor(lambda eng: run('pe', eng))
            if per['act']:
                block.scalar(lambda eng: run('act', eng))
            if per['dve']:
                block.vector(lambda eng: run('dve', eng))
            if per['pool']:
                block.gpsimd(lambda eng: run('pool', eng))
            block.sync(lambda eng: run('sp', eng))


D = 2048
NTOK = 2048
TS = 512
NT = NTOK // TS
NDC = D // 128
DFF = 5632
NFC = DFF // 128
NEXP = 8
ALPHA_C = 4.0 ** 0.25
LN_EPS = 1e-5
RMS_EPS = 1e-6
NEG = -30000.0
OFF_CQ, OFF_CKV, OFF_KR, OFF_DQ, OFF_DK, OFF_DV = 0, 448, 576, 640, 2944, 3712


class Ctx:
    def __init__(self):
        self.nc = bass.Bass("TRN2", target_bir_lowering=False)
        self.P = Prog(self.nc)
        self.in_names = []
        self.out_names = []
        self._n = 0

    def inp(self, name, shape, dt=F32):
        self.in_names.append(name)
        return self.nc.dram_tensor(name, list(shape), dt, kind="ExternalInput").ap()

    def out(self, name, shape, dt=F32):
        self.out_names.append(name)
        return self.nc.dram_tensor(name, list(shape), dt, kind="ExternalOutput").ap()

    def sb(self, name, shape, dt=F32):
        return self.nc.alloc_sbuf_tensor(name, list(shape), dt)

    def ps(self, name):
        return self.nc.alloc_psum_tensor(name, [128, 512], F32)

    def ring(self, name, n, shape, dt):
        return Ring([(self.sb(f"{name}{i}", shape, dt), f"{name}{i}") for i in range(n)])

    def psring(self, name, n):
        return Ring([(self.ps(f"{name}{i}"), f"{name}{i}") for i in range(n)])

    def run(self, in_maps):
        self.P.emit()
        res = run_bass_kernel_spmd(self.nc, in_maps, core_ids=list(range(len(in_maps))))
        if res.exec_time_ns is not None:
            print("[launch exec_time_ns]", res.exec_time_ns, flush=True)
        return res.results


class Ring:
    def __init__(self, items):
        self.items = items
        self.i = 0

    def next(self):
        it = self.items[self.i % len(self.items)]
        self.i += 1
        return it


def cast_load(P, dst, src, n, res, q='pool', maxcols=1024, per_chunk=False):
    cols = dst.shape[-1]
    for i in range(n):
        for c0 in range(0, cols, maxcols):
            c1 = min(cols, c0 + maxcols)
            P.dma(q, dst[:, i, c0:c1], src[:, i, c0:c1], w=[f"{res}_{i}" if per_chunk else res])


def load_consts(C, need_rot=False):
    P = C.P
    k = {}
    k['ones32'] = C.sb('ones32', [128, 128], F32)
    k['ones16'] = C.sb('ones16', [128, 128], BF16)
    P.memset('dve', k['ones32'][:, :], 1.0, w=['const_ones32'])
    P.memset('dve', k['ones16'][:, :], 1.0, w=['const_ones16'])
    return k


def layernorm_fm(C, K, z, zres, g_sb, b_sb, gres, xo, xo_res, x16, x16_res, ps_s1, ps_s2, scr):
    P = C.P
    (s1, s1r), (s2, s2r) = ps_s1, ps_s2
    sq = scr['sq']
    zl = list(zres) if isinstance(zres, (list, tuple)) else [zres]
    for oc in range(NDC):
        P.matmul(s1[:, :], K['ones32'][:, :], z[:, oc, :], start=(oc == 0), stop=(oc == NDC - 1),
                 r=zl + ['const_ones32'], w=[s1r])
    for oc in range(NDC):
        t, tr = sq.next()
        P.activation(t[:, :], z[:, oc, :], AF.Square, r=zl, w=[tr])
        P.matmul(s2[:, :], K['ones32'][:, :], t[:, :], start=(oc == 0), stop=(oc == NDC - 1),
                 r=[tr, 'const_ones32'], w=[s2r])
    mean, m2, rstd = scr['mean'], scr['m2'], scr['rstd']
    P.ts('dve', mean[:, :], s1[:, :], 1.0 / D, None, ALU.mult, r=[s1r], w=['ln_mean'])
    P.stt('dve', m2[:, :], mean[:, :], 1.0, mean[:, :], ALU.mult, ALU.mult, r=['ln_mean'], w=['ln_m2'])
    P.stt('dve', m2[:, :], s2[:, :], 1.0 / D, m2[:, :], ALU.mult, ALU.subtract, r=[s2r, 'ln_m2'], w=['ln_m2'])
    P.ts('dve', m2[:, :], m2[:, :], LN_EPS, None, ALU.add, r=['ln_m2'], w=['ln_m2'])
    P.activation(rstd[:, :], m2[:, :], AF.Sqrt, r=['ln_m2'], w=['ln_rstd'])
    P.recip(rstd[:, :], rstd[:, :], r=['ln_rstd'], w=['ln_rstd'])
    for oc in range(NDC):
        t, tr = sq.next()
        P.stt('dve', t[:, :], z[:, oc, :], 1.0, mean[:, :], ALU.mult, ALU.subtract, r=zl + ['ln_mean'], w=[tr])
        P.stt('dve', t[:, :], t[:, :], 1.0, rstd[:, :], ALU.mult, ALU.mult, r=[tr, 'ln_rstd'], w=[tr])
        P.ts('dve', xo[:, oc, :], t[:, :], g_sb[:, oc:oc + 1], b_sb[:, oc:oc + 1], ALU.mult, ALU.add,
             r=[tr, gres], w=[xo_res] + (zl if xo is z else []))
        P.copy('act', x16[:, oc, :], xo[:, oc, :], r=[xo_res], w=[x16_res(oc) if callable(x16_res) else x16_res])


def ln_scratch(C):
    return {'sq': C.ring('lnsq', 3, [128, 512], F32), 'mean': C.sb('ln_mean', [128, 512]),
            'm2': C.sb('ln_m2', [128, 512]), 'rstd': C.sb('ln_rstd', [128, 512])}


def rope_fm(C, n, acc, accr, cs_sb, col0, Rm, rings, out_ap, sfx):
    P = C.P
    t16, t16r = rings['t16'].next()
    rot, rotr = rings['rot'].next()
    a32, a32r = rings['a32'].next()
    b32, b32r = rings['b32'].next()
    o16, o16r = rings['o16'].next()
    P.copy('act', t16[0:n, :], acc[0:n, :], r=[accr], w=[t16r])
    P.matmul(rot[0:n, :], Rm[0:n, 0:n], t16[0:n, :], r=[t16r, 'const_R' + sfx], w=[rotr])
    P.copy('act', a32[0:n, :], acc[0:n, :], r=[accr], w=[a32r])
    P.copy('act', b32[0:n, :], rot[0:n, :], r=[rotr], w=[b32r])
    P.tt('pool', a32[0:n, :], a32[0:n, :], cs_sb[0:n, 0, col0:col0 + TS], ALU.mult, r=[a32r, 'const_cs' + sfx], w=[a32r])
    P.tt('pool', b32[0:n, :], b32[0:n, :], cs_sb[0:n, 1, col0:col0 + TS], ALU.mult, r=[b32r, 'const_cs' + sfx], w=[b32r])
    P.tt('pool', o16[0:n, :], a32[0:n, :], b32[0:n, :], ALU.add, r=[a32r, b32r], w=[o16r])
    P.dma('sp', out_ap, o16[0:n, :], r=[o16r], final=True)


def build_l1():
    C = Ctx()
    P = C.P
    xT = C.inp('xT', [D, NTOK])
    w_in = C.inp('w_in', [D, 4480])
    qn = C.inp('qn', [128, 4])
    w_qb = C.inp('w_qb', [512, 1920])
    kvn = C.inp('kvn', [128, 1])
    w_kvb = C.inp('w_kvb', [128, 2560])
    cs64 = C.inp('cs64', [64, 2, NTOK])
    cs128 = C.inp('cs128', [128, 2, NTOK])
    r64 = C.inp('r64', [64, 64])
    r128 = C.inp('r128', [128, 128])
    qm = C.out('qm', [10, 192, NTOK], BF16)
    km = C.out('km', [10, 128, NTOK], BF16)
    kr = C.out('kr', [64, NTOK], BF16)
    vm = C.out('vm', [10, 128, 16, 128], BF16)
    dq = C.out('dq', [18, 128, NTOK], BF16)
    dk = C.out('dk', [6, 128, NTOK], BF16)
    dv = C.out('dv', [6, 128, 16, 128], BF16)
    K = load_consts(C)
    qn_sb = C.sb('qn_sb', [128, 4])
    kvn_sb = C.sb('kvn_sb', [128, 1])
    cs64_sb = C.sb('cs64_sb', [64, 2, NTOK])
    cs128_sb = C.sb('cs128_sb', [128, 2, NTOK])
    R64 = C.sb('R64', [64, 64], BF16)
    R128 = C.sb('R128', [128, 128], BF16)
    wqb16 = C.sb('wqb16', [128, 4, 1920], BF16)
    wk16 = C.sb('wk16', [128, 10, 128], BF16)
    wv16 = C.sb('wv16', [128, 10, 128], BF16)
    P.dma('sp', qn_sb[:, :], qn, w=['const_qn'])
    P.dma('sp', kvn_sb[:, :], kvn, w=['const_kvn'])
    cast_load(P, cs64_sb, cs64, 2, 'const_cs64', q='sp', maxcols=2048)
    cast_load(P, cs128_sb, cs128, 2, 'const_cs128', q='sp', maxcols=2048)
    P.dma('pool', R64[:, :], r64, w=['const_R64'])
    P.dma('pool', R128[:, :], r128, w=['const_R128'])
    cast_load(P, wqb16, w_qb.rearrange("(kc p) n -> p kc n", p=128), 4, 'const_wqb', maxcols=480)
    wkv_v = w_kvb.rearrange("r (h two c) -> r two h c", two=2, c=128)
    cast_load(P, wk16, wkv_v[:, 0, :, :], 10, 'const_wk')
    cast_load(P, wv16, wkv_v[:, 1, :, :], 10, 'const_wv')

    x16r = C.ring('x16_', 2, [128, NDC, TS], BF16)
    wgr = C.ring('wg_', 2, [128, NDC, 640], BF16)
    accs = C.psring('acc', 3)
    rings = {'t16': C.ring('t16_', 2, [128, TS], BF16), 'rot': C.psring('rot', 2),
             'a32': C.ring('a32_', 2, [128, TS], F32), 'b32': C.ring('b32_', 2, [128, TS], F32),
             'o16': C.ring('o16_', 4, [128, TS], BF16)}
    ssq = (C.ps('ssq'), 'ssq')
    sskv = (C.ps('sskv'), 'sskv')
    sqr = C.ring('sq_', 2, [128, TS], F32)
    cq32 = C.sb('cq32', [128, 4, TS])
    cqn16 = C.sb('cqn16', [128, 4, TS], BF16)
    ckv32 = C.sb('ckv32', [128, TS])
    ckvn16 = C.sb('ckvn16', [128, TS], BF16)
    rstdq = C.sb('rstdq', [128, TS])
    rstdkv = C.sb('rstdkv', [128, TS])
    vst = C.ring('vst_', 2, [128, 10, 128], BF16)
    xT_v = xT.rearrange("(dc p) t -> p dc t", p=128)
    w_in_v = w_in.rearrange("(dc p) n -> p dc n", p=128)

    def proj(acc, accr, x16, x16res, wg, wgres, c0, n):
        for dc in range(NDC):
            P.matmul(acc[0:n, :], wg[:, dc, c0:c0 + n], x16[:, dc, :], start=(dc == 0), stop=(dc == NDC - 1),
                     r=[x16res, wgres], w=[accr])

    def rms(ss, ssr, n_feat, rstd, rstdres):
        P.ts('dve', rstd[:, :], ss[:, :], 1.0 / n_feat, RMS_EPS, ALU.mult, ALU.add, r=[ssr], w=[rstdres])
        P.activation(rstd[:, :], rstd[:, :], AF.Sqrt, r=[rstdres], w=[rstdres])
        P.recip(rstd[:, :], rstd[:, :], r=[rstdres], w=[rstdres])

    for g in range(NT):
        t0 = g * TS
        x16, x16res = x16r.next()
        cast_load(P, x16, xT_v[:, :, t0:t0 + TS], NDC, x16res)
        wg, wgres = wgr.next()
        cast_load(P, wg[:, :, 0:640], w_in_v[:, :, 0:640], NDC, wgres)
        cq_sizes = [128, 128, 128, 64]
        for cc, n in enumerate(cq_sizes):
            acc, accr = accs.next()
            proj(acc, accr, x16, x16res, wg, wgres, cc * 128, n)
            P.copy('act', cq32[0:n, cc, :], acc[0:n, :], r=[accr], w=['cq32'])
            sq, sqres = sqr.next()
            P.activation(sq[0:n, :], acc[0:n, :], AF.Square, r=[accr], w=[sqres])
            P.matmul(ssq[0][:, :], K['ones32'][0:n, :], sq[0:n, :], start=(cc == 0), stop=(cc == 3),
                     r=[sqres, 'const_ones32'], w=['ssq'])
        rms(ssq[0], 'ssq', 448.0, rstdq, 'rstdq')
        for cc, n in enumerate(cq_sizes):
            P.stt('dve', cqn16[0:n, cc, :], cq32[0:n, cc, :], qn_sb[0:n, cc:cc + 1], rstdq[0:n, :], ALU.mult, ALU.mult,
                  r=['cq32', 'rstdq', 'const_qn'], w=['cqn16'])
        acc, accr = accs.next()
        proj(acc, accr, x16, x16res, wg, wgres, OFF_CKV, 128)
        P.copy('act', ckv32[:, :], acc[:, :], r=[accr], w=['ckv32'])
        sq, sqres = sqr.next()
        P.activation(sq[:, :], acc[:, :], AF.Square, r=[accr], w=[sqres])
        P.matmul(sskv[0][:, :], K['ones32'][:, :], sq[:, :], r=[sqres, 'const_ones32'], w=['sskv'])
        rms(sskv[0], 'sskv', 128.0, rstdkv, 'rstdkv')
        P.stt('dve', ckvn16[:, :], ckv32[:, :], kvn_sb[:, 0:1], rstdkv[:, :], ALU.mult, ALU.mult,
              r=['ckv32', 'rstdkv', 'const_kvn'], w=['ckvn16'])
        acc, accr = accs.next()
        proj(acc, accr, x16, x16res, wg, wgres, OFF_KR, 64)
        rope_fm(C, 64, acc, accr, cs64_sb, t0, R64, rings, kr[:, t0:t0 + TS], '64')
        for h in range(10):
            acc, accr = accs.next()
            for kc, kn in enumerate(cq_sizes):
                P.matmul(acc[:, :], wqb16[0:kn, kc, h * 192:h * 192 + 128], cqn16[0:kn, kc, :], start=(kc == 0), stop=(kc == 3),
                         r=['cqn16', 'const_wqb'], w=[accr])
            o16, o16r = rings['o16'].next()
            P.copy('act', o16[:, :], acc[:, :], r=[accr], w=[o16r])
            P.dma('sp', qm[h, 0:128, t0:t0 + TS], o16[:, :], r=[o16r], final=True)
            acc, accr = accs.next()
            for kc, kn in enumerate(cq_sizes):
                P.matmul(acc[0:64, :], wqb16[0:kn, kc, h * 192 + 128:h * 192 + 192], cqn16[0:kn, kc, :], start=(kc == 0),
                         stop=(kc == 3), r=['cqn16', 'const_wqb'], w=[accr])
            rope_fm(C, 64, acc, accr, cs64_sb, t0, R64, rings, qm[h, 128:192, t0:t0 + TS], '64')
        for h in range(10):
            acc, accr = accs.next()
            P.matmul(acc[:, :], wk16[:, h, :], ckvn16[:, :], r=['ckvn16', 'const_wk'], w=[accr])
            o16, o16r = rings['o16'].next()
            P.copy('act', o16[:, :], acc[:, :], r=[accr], w=[o16r])
            P.dma('sp', km[h, :, t0:t0 + TS], o16[:, :], r=[o16r], final=True)
        for tb in range(4):
            vs, vsr = vst.next()
            for (h0, h1) in [(0, 4), (4, 8), (8, 10)]:
                acc, accr = accs.next()
                nn = (h1 - h0) * 128
                P.matmul(acc[:, 0:nn], ckvn16[:, tb * 128:(tb + 1) * 128], wv16[:, h0:h1, :], r=['ckvn16', 'const_wv'], w=[accr])
                P.copy('act', vs[:, h0:h1, :], acc[:, 0:nn].rearrange("p (h c) -> p h c", c=128), r=[accr], w=[vsr])
            P.dma('sp', vm[:, :, g * 4 + tb, :].rearrange("h p c -> p h c"), vs[:, :, :], r=[vsr], final=True)
        for gi in range(6):
            wg, wgres = wgr.next()
            c_lo = OFF_DQ + gi * 512
            cast_load(P, wg[:, :, 0:512], w_in_v[:, :, c_lo:c_lo + 512], NDC, wgres)
            for j in range(4):
                ci = gi * 4 + j
                acc, accr = accs.next()
                proj(acc, accr, x16, x16res, wg, wgres, j * 128, 128)
                dst = dq[ci, :, t0:t0 + TS] if ci < 18 else dk[ci - 18, :, t0:t0 + TS]
                rope_fm(C, 128, acc, accr, cs128_sb, t0, R128, rings, dst, '128')
        for (c_lo, ncol, h0) in [(OFF_DV, 512, 0), (OFF_DV + 512, 256, 4)]:
            wg, wgres = wgr.next()
            cast_load(P, wg[:, :, 0:ncol], w_in_v[:, :, c_lo:c_lo + ncol], NDC, wgres)
            nh = ncol // 128
            for tb in range(4):
                acc, accr = accs.next()
                for dc in range(NDC):
                    P.matmul(acc[:, 0:ncol], x16[:, dc, tb * 128:(tb + 1) * 128], wg[:, dc, 0:ncol], start=(dc == 0),
                             stop=(dc == NDC - 1), r=[x16res, wgres], w=[accr])
                vs, vsr = vst.next()
                P.copy('act', vs[:, 0:nh, :], acc[:, 0:ncol].rearrange("p (h c) -> p h c", c=128), r=[accr], w=[vsr])
                P.dma('sp', dv[h0:h0 + nh, :, g * 4 + tb, :].rearrange("h p c -> p h c"), vs[:, 0:nh, :], r=[vsr], final=True)
    return C


def core_positions(c):
    r = c % 4
    t = np.arange(NTOK)
    return 512 * (4 * (t // 512) + r) + (t % 512)


def rope_table_fm(pos, dim):
    half = dim // 2
    inv_freq = (1.0 / (np.float32(10000.0) ** (np.arange(0, dim, 2, dtype=np.float32) / np.float32(dim)))).astype(np.float32)
    ang = pos.astype(np.float32)[None, :] * inv_freq[:, None]
    cos = np.cos(ang).astype(np.float32)
    sin = np.sin(ang).astype(np.float32)
    out = np.empty((dim, 2, pos.shape[0]), np.float32)
    out[:half, 0] = cos
    out[half:, 0] = cos
    out[:half, 1] = sin
    out[half:, 1] = sin
    return out


def rot_lhsT(dim):
    half = dim // 2
    m = np.zeros((dim, dim), np.float32)
    for j in range(half):
        m[j + half, j] = -1.0
        m[j, j + half] = 1.0
    return m


def fm_vec(v, n_chunks):
    o = np.zeros((n_chunks * 128,), np.float32)
    o[:v.shape[0]] = v
    return np.ascontiguousarray(o.reshape(n_chunks, 128).T)


def l1_inputs(inp):
    maps = []
    wqb = np.zeros((512, 1920), np.float32)
    wqb[:448] = inp['ev_w_q_b'][0]
    common = {
        'w_in': np.ascontiguousarray(inp['ev_w_in'][0]), 'qn': fm_vec(inp['ev_q_norm'][0], 4), 'w_qb': wqb,
        'kvn': fm_vec(inp['ev_kv_norm'][0], 1), 'w_kvb': np.ascontiguousarray(inp['ev_w_kv_b'][0]),
        'r64': rot_lhsT(64), 'r128': rot_lhsT(128),
    }
    for c in range(8):
        pos = core_positions(c)
        m = dict(common)
        m['xT'] = np.ascontiguousarray(inp['x'][c // 4][pos].T)
        m['cs64'] = rope_table_fm(pos, 64)
        m['cs128'] = rope_table_fm(pos, 128)
        maps.append(m)
    return maps


def build_l2_mla():
    C = Ctx()
    P = C.P
    qm = C.inp('qm', [10, 192, NTOK], BF16)
    kmf = C.inp('kmf', [4, 10, 128, NTOK], BF16)
    krf = C.inp('krf', [4, 64, NTOK], BF16)
    vmf = C.inp('vmf', [4, 10, 128, 16, 128], BF16)
    cmask = C.inp('cmask', [128, 16, TS], BF16)
    mixT = C.out('mixT', [10, 128, NTOK], BF16)
    K = load_consts(C)
    scale = 192.0 ** -0.5
    cm = C.sb('cm', [128, 16, TS], BF16)
    cast_load(P, cm, cmask, 16, 'const_cm', q='sp', maxcols=TS)
    krT = C.sb('krT', [64, 4, NTOK], BF16)
    cast_load(P, krT, krf.rearrange("r p t -> p r t"), 4, 'const_kr', q='sp', maxcols=NTOK)
    kTr = C.ring('kT_', 2, [128, 4, NTOK], BF16)
    vr = C.ring('v_', 2, [128, 4, 16 * 128], BF16)
    qnr = C.ring('qn_', 2, [128, NTOK], BF16)
    qrr = C.ring('qr_', 2, [64, NTOK], BF16)
    Sr = C.psring('S', 3)
    Or = C.psring('O', 2)
    Dr = C.psring('Dn', 2)
    pTr = C.ring('pT_', 3, [128, TS], BF16)
    tmpr = C.ring('tmp_', 2, [128, TS], F32)
    rec = C.sb('rec', [128, TS])
    o32 = C.sb('o32', [128, TS])
    o16r = C.ring('ao16_', 2, [128, TS], BF16)
    for h in range(10):
        kT, kTres = kTr.next()
        v, vres = vr.next()
        qn, qnres = qnr.next()
        qr, qrres = qrr.next()
        for r2 in range(4):
            P.dma('sp', kT[:, r2, :], kmf[r2, h, :, :], w=[kTres + f'_{r2}'])
            P.dma('sp', v[:, r2, :], vmf[r2, h, :, :, :].rearrange("p b c -> p (b c)"), w=[vres + f'_{r2}'])
        P.dma('sp', qn[:, :], qm[h, 0:128, :], w=[qnres])
        P.dma('sp', qr[:, :], qm[h, 128:192, :], w=[qrres])
        for g in range(NT):
            O, Ores = Or.next()
            Dn, Dres = Dr.next()
            nblk = 16 * (g + 1)
            idx = 0
            for g2 in range(g + 1):
                for r2 in range(4):
                    for kb in range(4):
                        S, Sres = Sr.next()
                        pT, pTres = pTr.next()
                        c0 = g2 * TS + kb * 128
                        P.matmul(S[:, :], kT[:, r2, c0:c0 + 128], qn[:, g * TS:(g + 1) * TS], start=True, stop=False,
                                 r=[kTres + f'_{r2}', qnres], w=[Sres])
                        P.matmul(S[:, :], krT[0:64, r2, c0:c0 + 128], qr[0:64, g * TS:(g + 1) * TS], start=False, stop=True,
                                 r=['const_kr', qrres], w=[Sres])
                        if g2 == g:
                            tmp, tmpres = tmpr.next()
                            P.stt('dve', tmp[:, :], S[:, :], scale, cm[:, r2 * 4 + kb, :], ALU.mult, ALU.add,
                                  r=[Sres, 'const_cm'], w=[tmpres])
                            P.activation(pT[:, :], tmp[:, :], AF.Exp, r=[tmpres], w=[pTres])
                        else:
                            P.activation(pT[:, :], S[:, :], AF.Exp, r=[Sres], w=[pTres], scale=scale)
                        blk = g2 * 4 + kb
                        P.matmul(O[:, :], v[:, r2, blk * 128:(blk + 1) * 128], pT[:, :], start=(idx == 0), stop=(idx == nblk - 1),
                                 r=[vres + f'_{r2}', pTres], w=[Ores])
                        P.matmul(Dn[:, :], K['ones16'][:, :], pT[:, :], start=(idx == 0), stop=(idx == nblk - 1),
                                 r=['const_ones16', pTres], w=[Dres])
                        idx += 1
            P.recip(rec[:, :], Dn[:, :], r=[Dres], w=['rec'])
            P.copy('act', o32[:, :], O[:, :], r=[Ores], w=['o32'])
            o16, o16res = o16r.next()
            P.tt('pool', o16[:, :], o32[:, :], rec[:, :], ALU.mult, r=['o32', 'rec'], w=[o16res])
            P.dma('sp', mixT[h, :, g * TS:(g + 1) * TS], o16[:, :], r=[o16res], final=True)
    return C


def causal_mask_tiles(r):
    m = np.zeros((128, 16, TS), np.float32)
    ki = np.arange(128)[:, None]
    qi = np.arange(TS)[None, :]
    for r2 in range(4):
        for kb in range(4):
            if r2 > r:
                m[:, r2 * 4 + kb, :] = NEG
            elif r2 == r:
                m[:, r2 * 4 + kb, :] = np.where(kb * 128 + ki <= qi, 0.0, NEG)
    import ml_dtypes
    return m.astype(ml_dtypes.bfloat16)


DIL_PATTERNS = ((128, 1), (512, 4), (2048, 16))


def _dil_slots(grp):
    if grp < 2:
        return [(3, 1), (0, 0), (1, 0), (2, 0), (3, 0)]
    return [(r2, 1) for r2 in range(4)] + [(r2, 0) for r2 in range(4)]


def _dil_mask_bool(r, grp, r2, dg, kb):
    W, d = DIL_PATTERNS[grp]
    ki = np.arange(128)[:, None]
    qi = np.arange(TS)[None, :]
    delta = 512 * (4 * dg + r - r2) + qi - (128 * kb + ki)
    return (delta >= 0) & (delta <= W) & (delta % d == 0)


def dil_block_list():
    blocks = []
    for grp in range(3):
        for (r2, dg) in _dil_slots(grp):
            for kb in range(4):
                if any(_dil_mask_bool(r, grp, r2, dg, kb).any() for r in range(4)):
                    blocks.append((grp, r2, dg, kb))
    return blocks


def dil_mask_tiles(r):
    import ml_dtypes
    bl = dil_block_list()
    m = np.full((128, len(bl), TS), NEG, np.float32)
    for i, (grp, r2, dg, kb) in enumerate(bl):
        m[:, i, :] = np.where(_dil_mask_bool(r, grp, r2, dg, kb), 0.0, NEG)
    return m.astype(ml_dtypes.bfloat16)


def build_l2_dil():
    C = Ctx()
    P = C.P
    bl = dil_block_list()
    nb = len(bl)
    dq = C.inp('dq', [18, 128, NTOK], BF16)
    dkf = C.inp('dkf', [4, 6, 128, NTOK], BF16)
    dvf = C.inp('dvf', [4, 6, 128, 16, 128], BF16)
    dmask = C.inp('dmask', [128, nb, TS], BF16)
    mixT = C.out('mixD', [6, 128, NTOK], BF16)
    K = load_consts(C)
    scale = 128.0 ** -0.5
    dm = C.sb('dm', [128, nb, TS], BF16)
    for i in range(nb):
        P.dma('sp', dm[:, i, :], dmask[:, i, :], w=[f'const_dm{i}'])
    kT = C.sb('dkT', [128, 4, NTOK], BF16)
    v = C.sb('dv', [128, 4, 16 * 128], BF16)
    q3 = C.sb('dq3', [128, 3, NTOK], BF16)
    Sr = C.psring('S', 3)
    Or = C.psring('O', 2)
    Dr = C.psring('Dn', 2)
    pTr = C.ring('pT_', 3, [128, TS], BF16)
    tmpr = C.ring('tmp_', 3, [128, TS], F32)
    rec = C.sb('rec', [128, TS])
    o32 = C.sb('o32', [128, TS])
    o16r = C.ring('ao16_', 2, [128, TS], BF16)
    for hd in range(6):
        for r2 in range(4):
            P.dma('sp', kT[:, r2, :], dkf[r2, hd, :, :], w=[f'dkT_{r2}'])
            P.dma('sp', v[:, r2, :], dvf[r2, hd, :, :, :].rearrange("p b c -> p (b c)"), w=[f'dv_{r2}'])
        for grp in range(3):
            P.dma('sp', q3[:, grp, :], dq[grp * 6 + hd, :, :], w=[f'dq3_{grp}'])
        for g in range(NT):
            todo = [(i, b) for i, b in enumerate(bl) if g - b[2] >= 0]
            O, Ores = Or.next()
            Dn, Dres = Dr.next()
            for idx, (i, (grp, r2, dg, kb)) in enumerate(todo):
                g2 = g - dg
                S, Sres = Sr.next()
                pT, pTres = pTr.next()
                tmp, tmpres = tmpr.next()
                c0 = g2 * TS + kb * 128
                P.matmul(S[:, :], kT[:, r2, c0:c0 + 128], q3[:, grp, g * TS:(g + 1) * TS], r=[f'dkT_{r2}', f'dq3_{grp}'], w=[Sres])
                P.stt('dve', tmp[:, :], S[:, :], scale, dm[:, i, :], ALU.mult, ALU.add, r=[Sres, f'const_dm{i}'], w=[tmpres])
                P.activation(pT[:, :], tmp[:, :], AF.Exp, r=[tmpres], w=[pTres])
                blk = g2 * 4 + kb
                first, last = idx == 0, idx == len(todo) - 1
                P.matmul(O[:, :], v[:, r2, blk * 128:(blk + 1) * 128], pT[:, :], start=first, stop=last, r=[f'dv_{r2}', pTres], w=[Ores])
                P.matmul(Dn[:, :], K['ones16'][:, :], pT[:, :], start=first, stop=last, r=['const_ones16', pTres], w=[Dres])
            P.recip(rec[:, :], Dn[:, :], r=[Dres], w=['rec'])
            P.copy('act', o32[:, :], O[:, :], r=[Ores], w=['o32'])
            o16, o16res = o16r.next()
            P.tt('pool', o16[:, :], o32[:, :], rec[:, :], ALU.mult, r=['o32', 'rec'], w=[o16res])
            P.dma('sp', mixT[hd, :, g * TS:(g + 1) * TS], o16[:, :], r=[o16res], final=True)
    return C


def build_l3(debug_xa=False):
    C = Ctx()
    P = C.P
    mixT = C.inp('mixT', [16, 128, NTOK], BF16)
    xT = C.inp('xT', [D, NTOK])
    w_out = C.inp('w_out', [D, D])
    ln1g = C.inp('ln1g', [128, NDC]); ln1b = C.inp('ln1b', [128, NDC])
    ln2g = C.inp('ln2g', [128, NDC]); ln2b = C.inp('ln2b', [128, NDC])
    wg_d = C.inp('wg', [D, DFF]); wu_d = C.inp('wu', [D, DFF]); wd_d = C.inp('wd', [DFF, D])
    w_qkv = C.inp('w_qkv', [D, 6144])
    w_f = C.inp('w_f', [D, 16]); b_f = C.inp('b_f', [16, 1])
    x1T = C.out('x1T', [D, NTOK])
    fq = C.out('fq', [16, 128, NTOK], BF16)
    fk = C.out('fk', [16, 128, NTOK], BF16)
    fv = C.out('fv', [16, 128, 16, 128], BF16)
    logf = C.out('logf', [16, NTOK])
    xaT = C.out('xaT', [D, NTOK]) if debug_xa else None
    K = load_consts(C)
    g1 = C.sb('g1', [128, NDC]); b1 = C.sb('b1', [128, NDC]); g2 = C.sb('g2', [128, NDC]); b2 = C.sb('b2', [128, NDC])
    P.dma('sp', g1[:, :], ln1g, w=['const_g1']); P.dma('sp', b1[:, :], ln1b, w=['const_b1'])
    P.dma('sp', g2[:, :], ln2g, w=['const_g2']); P.dma('sp', b2[:, :], ln2b, w=['const_b2'])
    wf32 = C.sb('wf32', [128, NDC, 16])
    P.dma('sp', wf32[:, :, :], w_f.rearrange("(dc p) n -> p dc n", p=128), w=['const_wf'])
    nbf = C.sb('nbf', [16, 1])
    P.dma('sp', nbf[:, :], b_f, w=['const_nbf'])
    P.op('act', lambda e: e.mul(out=nbf[:, :], in_=nbf[:, :], mul=-1.0), ['const_nbf'], ['const_nbf2'])

    mbuf = C.sb('mbuf', [128, NDC, TS], BF16)
    z = C.sb('z', [128, NDC, TS])
    xa16 = C.sb('xa16', [128, NDC, TS], BF16)
    h16 = C.sb('h16', [128, NFC, TS], BF16)
    wr = C.ring('w_', 4, [128, NDC, 256], BF16)
    wdr = C.ring('wd_', 3, [128, 4, TS], BF16)
    xcr = C.ring('xc_', 3, [128, TS], F32)
    scr = ln_scratch(C)
    sgr = C.ring('sg_', 2, [128, TS], F32)
    o16r = C.ring('o16_', 4, [128, TS], BF16)
    vsr_ = C.ring('vs_', 2, [128, 4, 128], BF16)
    lf = C.ring('lf_', 2, [16, TS], F32)
    accs = C.psring('acc', 2)
    gu = C.psring('gu', 4)
    s1 = (C.ps('s1'), 's1'); s2 = (C.ps('s2'), 's2')
    mixT_v = mixT.rearrange("c p t -> p c t")
    xT_v = xT.rearrange("(dc p) t -> p dc t", p=128)
    x1T_v = x1T.rearrange("(dc p) t -> p dc t", p=128)

    def wload(src_v, c_lo):
        w, wres = wr.next()
        cast_load(P, w, src_v[:, :, c_lo:c_lo + 256], NDC, wres, per_chunk=True)
        return w, wres

    for g in range(NT):
        t0 = g * TS
        for c in range(NDC):
            P.dma('sp', mbuf[:, c, :], mixT_v[:, c, t0:t0 + TS], w=[f'mbuf_{c}'])
        w_out_v = w_out.rearrange("(ic p) n -> p ic n", p=128)
        for og in range(8):
            w, wres = wload(w_out_v, og * 256)
            for o in range(2):
                oc = og * 2 + o
                acc, accr = accs.next()
                for ic in range(NDC):
                    P.matmul(acc[:, :], w[:, ic, o * 128:(o + 1) * 128], mbuf[:, ic, :], start=(ic == 0), stop=(ic == NDC - 1),
                             r=[f'{wres}_{ic}', f'mbuf_{ic}'], w=[accr])
                xc, xcres = xcr.next()
                P.dma('sp', xc[:, :], xT_v[:, oc, t0:t0 + TS], w=[xcres])
                P.op('act', lambda e, xc=xc: e.mul(out=xc[:, :], in_=xc[:, :], mul=ALPHA_C), [xcres], [xcres])
                P.stt('dve', z[:, oc, :], acc[:, :], 1.0, xc[:, :], ALU.mult, ALU.add, r=[accr, xcres], w=['z'])
        layernorm_fm(C, K, z, 'z', g1, b1, 'const_g1', z, 'z', xa16, 'xa16', s1, s2, scr)
        if debug_xa:
            for oc in range(NDC):
                P.dma('sp', xaT.rearrange("(dc p) t -> p dc t", p=128)[:, oc, t0:t0 + TS], z[:, oc, :], r=['z'], final=True)
        for oc in range(NDC):
            P.op('act', lambda e, oc=oc: e.mul(out=z[:, oc, :], in_=z[:, oc, :], mul=ALPHA_C), ['z'], ['z'])
        wg_v = wg_d.rearrange("(dc p) n -> p dc n", p=128)
        wu_v = wu_d.rearrange("(dc p) n -> p dc n", p=128)
        for fg in range(DFF // 256):
            wgt, wgres = wload(wg_v, fg * 256)
            wut, wures = wload(wu_v, fg * 256)
            for fc in range(2):
                ffc = fg * 2 + fc
                G, Gres = gu.next()
                U, Ures = gu.next()
                for dc in range(NDC):
                    P.matmul(G[:, :], wgt[:, dc, fc * 128:(fc + 1) * 128], xa16[:, dc, :], start=(dc == 0), stop=(dc == NDC - 1),
                             r=[f'{wgres}_{dc}', 'xa16'], w=[Gres])
                for dc in range(NDC):
                    P.matmul(U[:, :], wut[:, dc, fc * 128:(fc + 1) * 128], xa16[:, dc, :], start=(dc == 0), stop=(dc == NDC - 1),
                             r=[f'{wures}_{dc}', 'xa16'], w=[Ures])
                sg, sgres = sgr.next()
                P.activation(sg[:, :], G[:, :], AF.Silu, r=[Gres], w=[sgres])
                P.stt('dve', h16[:, ffc, :], U[:, :], 1.0, sg[:, :], ALU.mult, ALU.mult, r=[Ures, sgres], w=[f'h16_{ffc}'])
        wd_v = wd_d.rearrange("(f p) n -> p f n", p=128)
        for ocg in range(4):
            Dacc = [gu.next() for _ in range(4)]
            for fq4 in range(NFC // 4):
                wd, wdres = wdr.next()
                cast_load(P, wd, wd_v[:, fq4 * 4:(fq4 + 1) * 4, ocg * TS:(ocg + 1) * TS], 4, wdres, per_chunk=True)
                for j in range(4):
                    ffc = fq4 * 4 + j
                    for o in range(4):
                        P.matmul(Dacc[o][0][:, :], wd[:, j, o * 128:(o + 1) * 128], h16[:, ffc, :], start=(ffc == 0), stop=(ffc == NFC - 1),
                                 r=[f'{wdres}_{j}', f'h16_{ffc}'], w=[Dacc[o][1]])
            for o in range(4):
                oc = ocg * 4 + o
                P.stt('dve', z[:, oc, :], Dacc[o][0][:, :], 1.0, z[:, oc, :], ALU.mult, ALU.add, r=[Dacc[o][1], 'z'], w=['z'])
        layernorm_fm(C, K, z, 'z', g2, b2, 'const_g2', z, 'z', mbuf, lambda oc: f'mbuf_{oc}', s1, s2, scr)
        for oc in range(NDC):
            P.dma('sp', x1T_v[:, oc, t0:t0 + TS], z[:, oc, :], r=['z'], final=True)
        w_qkv_v = w_qkv.rearrange("(dc p) n -> p dc n", p=128)
        for qg in range(16):
            w, wres = wload(w_qkv_v, qg * 256)
            for j in range(2):
                ch = qg * 2 + j
                acc, accr = accs.next()
                for dc in range(NDC):
                    P.matmul(acc[:, :], w[:, dc, j * 128:(j + 1) * 128], mbuf[:, dc, :], start=(dc == 0), stop=(dc == NDC - 1),
                             r=[f'{wres}_{dc}', f'mbuf_{dc}'], w=[accr])
                o16, o16res = o16r.next()
                P.copy('act', o16[:, :], acc[:, :], r=[accr], w=[o16res])
                dst = fq[ch, :, t0:t0 + TS] if ch < 16 else fk[ch - 16, :, t0:t0 + TS]
                P.dma('sp', dst, o16[:, :], r=[o16res], final=True)
        for vg in range(8):
            w, wres = wload(w_qkv_v, 4096 + vg * 256)
            for tb in range(4):
                acc, accr = accs.next()
                for dc in range(NDC):
                    P.matmul(acc[:, 0:256], mbuf[:, dc, tb * 128:(tb + 1) * 128], w[:, dc, :], start=(dc == 0), stop=(dc == NDC - 1),
                             r=[f'{wres}_{dc}', f'mbuf_{dc}'], w=[accr])
                vs, vsres = vsr_.next()
                P.copy('act', vs[:, 0:2, :], acc[:, 0:256].rearrange("p (h c) -> p h c", c=128), r=[accr], w=[vsres])
                P.dma('sp', fv[vg * 2:vg * 2 + 2, :, g * 4 + tb, :].rearrange("h p c -> p h c"), vs[:, 0:2, :], r=[vsres], final=True)
        acc, accr = accs.next()
        for dc in range(NDC):
            P.matmul(acc[0:16, :], wf32[:, dc, :], z[:, dc, :], start=(dc == 0), stop=(dc == NDC - 1), r=['const_wf', 'z'], w=[accr])
        l, lres = lf.next()
        P.activation(l[:, :], acc[0:16, :], AF.Exp, r=[accr, 'const_nbf2'], w=[lres], scale=-1.0, bias=nbf[:, 0:1])
        P.activation(l[:, :], l[:, :], AF.Ln, r=[lres], w=[lres], bias=1.0)
        P.op('act', lambda e, l=l: e.mul(out=l[:, :], in_=l[:, :], mul=-1.0), [lres], [lres])
        P.dma('sp', logf[:, t0:t0 + TS], l[:, :], r=[lres], final=True)
    return C


def l3_inputs(inp, mixT_list):
    common = {
        'w_out': np.ascontiguousarray(inp['ev_w_out'][0]),
        'ln1g': fm_vec(inp['ev_ln1_g'][0], NDC), 'ln1b': fm_vec(inp['ev_ln1_b'][0], NDC),
        'ln2g': fm_vec(inp['ev_ln2_g'][0], NDC), 'ln2b': fm_vec(inp['ev_ln2_b'][0], NDC),
        'wg': np.ascontiguousarray(inp['ev_ffn_w_gate'][0]), 'wu': np.ascontiguousarray(inp['ev_ffn_w_up'][0]),
        'wd': np.ascontiguousarray(inp['ev_ffn_w_down'][0]), 'w_qkv': np.ascontiguousarray(inp['od_w_qkv'][0]),
        'w_f': np.ascontiguousarray(inp['od_w_f'][0]), 'b_f': np.ascontiguousarray(inp['od_b_f'][0].reshape(16, 1)),
    }
    maps = []
    for c in range(8):
        m = dict(common)
        m['mixT'] = mixT_list[c]
        m['xT'] = np.ascontiguousarray(inp['x'][c // 4][core_positions(c)].T)
        maps.append(m)
    return maps


def build_l4():
    C = Ctx()
    P = C.P
    fq = C.inp('fq', [16, 128, NTOK], BF16)
    fkf = C.inp('fkf', [4, 16, 128, NTOK], BF16)
    fvf = C.inp('fvf', [4, 16, 128, 16, 128], BF16)
    logff = C.inp('logff', [4, 16, NTOK])
    cmask = C.inp('cmask', [128, 16, TS], BF16)
    onehot = C.inp('onehot', [16, 4])
    sel_d = C.inp('sel', [16, 16, 128])
    id_d = C.inp('ident16', [16, 16])
    mixF = C.out('mixF', [16, 128, NTOK], BF16)
    K = load_consts(C)
    scale = 128.0 ** -0.5
    cm = C.sb('cm_sb', [128, 16, TS], BF16)
    cast_load(P, cm, cmask, 16, 'const_cm', q='sp', maxcols=TS, per_chunk=True)
    lf_sb = C.sb('lf_sb', [16, 4, NTOK])
    cT = C.sb('cT', [16, 4, NTOK])
    for r2 in range(4):
        P.dma('sp', lf_sb[:, r2, :], logff[r2, :, :], w=[f'lf_{r2}'])
    oh = C.sb('oh_sb', [16, 4]); P.dma('sp', oh[:, :], onehot, w=['const_oh'])
    sel = C.sb('sel_sb', [16, 16, 128]); P.dma('sp', sel[:, :, :], sel_d, w=['const_sel'])
    id16 = C.sb('id16', [16, 16]); P.dma('sp', id16[:, :], id_d, w=['const_id'])
    ones_s = C.sb('ones_s', [16, TS]); P.memset('dve', ones_s[:, :], 1.0, w=['const_ones_s'])
    prev = None
    for j in range(16):
        g2, r2 = j // 4, j % 4
        seg = lf_sb[:, r2, g2 * TS:(g2 + 1) * TS]
        out = cT[:, r2, g2 * TS:(g2 + 1) * TS]
        init = 0.0 if prev is None else prev
        rd = [f'lf_{r2}', 'const_ones_s'] + ([] if prev is None else [f'cT_{j - 1}'])
        P.op('dve', lambda e, out=out, seg=seg, init=init: e.tensor_tensor_scan(out=out, data0=ones_s[:, :], data1=seg, initial=init,
                                                                                 op0=ALU.mult, op1=ALU.add), rd, [f'cT_{j}'])
        prev = cT[:, r2, (g2 + 1) * TS - 1:(g2 + 1) * TS]
    allc = [f'cT_{j}' for j in range(16)]
    c_own = C.sb('c_own', [16, NT, TS])
    for g in range(NT):
        P.ts('dve', c_own[:, g, :], cT[:, 0, g * TS:(g + 1) * TS], oh[:, 0:1], None, ALU.mult, r=allc + ['const_oh'], w=[f'cown_{g}'])
        for r2 in range(1, 4):
            P.stt('dve', c_own[:, g, :], cT[:, r2, g * TS:(g + 1) * TS], oh[:, r2:r2 + 1], c_own[:, g, :], ALU.mult, ALU.add,
                  r=allc + ['const_oh', f'cown_{g}'], w=[f'cown_{g}'])
    c_tok = C.sb('c_tok', [128, 64, 16])
    ctp = C.psring('ctp', 2)
    for half in range(2):
        ps, psr = ctp.next()
        for i in range(32):
            blk = half * 32 + i
            r2, lb = blk // 16, blk % 16
            P.matmul(ps[:, i * 16:(i + 1) * 16], cT[:, r2, lb * 128:(lb + 1) * 128], id16[:, :], r=allc + ['const_id'], w=[psr])
        P.copy('act', c_tok[:, half * 32:(half + 1) * 32, :], ps[:, :].rearrange("p (b h) -> p b h", h=16), r=[psr], w=['c_tok'])
    kT = C.sb('kT', [128, 4, NTOK], BF16)
    v = C.sb('v', [128, 4, 16 * 128], BF16)
    qr_ = C.ring('q_', 2, [128, NTOK], BF16)
    Sr = C.psring('S', 2)
    Or = C.psring('O', 2)
    Dr = C.psring('Dn', 2)
    pTr = C.ring('pT_', 3, [128, TS], BF16)
    tmpr = C.ring('tmp_', 3, [128, TS], F32)
    cqr = C.sb('cqr', [128, TS])
    cref = C.sb('cref', [128, 1])
    bias_all = C.sb('bias_all', [128, 64])
    rec = C.sb('rec', [128, TS])
    o32 = C.sb('o32', [128, TS])
    o16r = C.ring('ao16_', 2, [128, TS], BF16)
    for h in range(16):
        q, qres = qr_.next()
        for r2 in range(4):
            P.dma('sp', kT[:, r2, :], fkf[r2, h, :, :], w=[f'kT_{r2}'])
            P.dma('sp', v[:, r2, :], fvf[r2, h, :, :, :].rearrange("p b c -> p (b c)"), w=[f'v_{r2}'])
        P.dma('sp', q[:, :], fq[h, :, :], w=[qres])
        for g in range(NT):
            cqb, cqbr = ctp.next()
            P.matmul(cqb[:, :], sel[:, h, :], c_own[:, g, :], r=['const_sel', f'cown_{g}'], w=[cqbr])
            P.copy('act', cref[:, :], cqb[:, 0:1], r=[cqbr], w=['cref'])
            P.ts('dve', cqr[:, :], cqb[:, :], cref[:, 0:1], None, ALU.subtract, r=[cqbr, 'cref'], w=['cqr'])
            P.ts('dve', bias_all[:, :], c_tok[:, :, h], cref[:, 0:1], -1.0, ALU.subtract, ALU.mult, r=['c_tok', 'cref'], w=['bias_all'])
            O, Ores = Or.next()
            Dn, Dres = Dr.next()
            nblk = 16 * (g + 1)
            idx = 0
            for g2 in range(g + 1):
                for r2 in range(4):
                    for kb in range(4):
                        S, Sres = Sr.next()
                        pT, pTres = pTr.next()
                        tmp, tmpres = tmpr.next()
                        c0 = g2 * TS + kb * 128
                        lb = g2 * 4 + kb
                        P.matmul(S[:, :], kT[:, r2, c0:c0 + 128], q[:, g * TS:(g + 1) * TS], r=[f'kT_{r2}', qres], w=[Sres])
                        P.stt('dve', tmp[:, :], S[:, :], scale, cqr[:, :], ALU.mult, ALU.add, r=[Sres, 'cqr'], w=[tmpres])
                        if g2 == g:
                            P.stt('dve', tmp[:, :], tmp[:, :], 1.0, cm[:, r2 * 4 + kb, :], ALU.mult, ALU.add,
                                  r=[tmpres, f'const_cm_{r2 * 4 + kb}'], w=[tmpres])
                        bcol = r2 * 16 + lb
                        P.activation(pT[:, :], tmp[:, :], AF.Exp, r=[tmpres, 'bias_all'], w=[pTres], bias=bias_all[:, bcol:bcol + 1])
                        P.matmul(O[:, :], v[:, r2, lb * 128:(lb + 1) * 128], pT[:, :], start=(idx == 0), stop=(idx == nblk - 1),
                                 r=[f'v_{r2}', pTres], w=[Ores])
                        P.matmul(Dn[:, :], K['ones16'][:, :], pT[:, :], start=(idx == 0), stop=(idx == nblk - 1),
                                 r=['const_ones16', pTres], w=[Dres])
                        idx += 1
            P.recip(rec[:, :], Dn[:, :], r=[Dres], w=['rec'])
            P.copy('act', o32[:, :], O[:, :], r=[Ores], w=['o32'])
            o16, o16res = o16r.next()
            P.tt('pool', o16[:, :], o32[:, :], rec[:, :], ALU.mult, r=['o32', 'rec'], w=[o16res])
            P.dma('sp', mixF[h, :, g * TS:(g + 1) * TS], o16[:, :], r=[o16res], final=True)
    return C


def l4_inputs(r3):
    sel = np.zeros((16, 16, 128), np.float32)
    for h in range(16):
        sel[h, h, :] = 1.0
    maps = []
    for c in range(8):
        b, r = c // 4, c % 4
        grp = [r3[4 * b + r2] for r2 in range(4)]
        oh = np.zeros((16, 4), np.float32)
        oh[:, r] = 1.0
        maps.append({'fq': r3[c]['fq'], 'fkf': np.stack([g['fk'] for g in grp]), 'fvf': np.stack([g['fv'] for g in grp]),
                     'logff': np.stack([g['logf'] for g in grp]), 'cmask': causal_mask_tiles(r), 'onehot': oh, 'sel': sel,
                     'ident16': np.eye(16, dtype=np.float32)})
    return maps


def build_l5a():
    C = Ctx()
    P = C.P
    mixT = C.inp('mixT', [16, 128, NTOK], BF16)
    xT = C.inp('xT', [D, NTOK])
    w_out = C.inp('w_out', [D, D])
    ln1g = C.inp('ln1g', [128, NDC]); ln1b = C.inp('ln1b', [128, NDC])
    rw = C.inp('rw', [D, NEXP]); rb = C.inp('rb', [1, NEXP])
    xbT = C.out('xbT', [D, NTOK])
    xb16T = C.out('xb16T', [D, NTOK], BF16)
    gates = C.out('gates', [NTOK, NEXP])
    K = load_consts(C)
    g1 = C.sb('g1', [128, NDC]); b1 = C.sb('b1', [128, NDC])
    P.dma('sp', g1[:, :], ln1g, w=['const_g1']); P.dma('sp', b1[:, :], ln1b, w=['const_b1'])
    rw32 = C.sb('rw32', [128, NDC, NEXP])
    P.dma('sp', rw32[:, :, :], rw.rearrange("(dc p) n -> p dc n", p=128), w=['const_rw'])
    rb32 = C.sb('rb32', [1, NEXP]); P.dma('sp', rb32[:, :], rb, w=['const_rb'])
    mbuf = C.sb('mbuf', [128, NDC, TS], BF16)
    xb16 = C.sb('xb16', [128, NDC, TS], BF16)
    z = C.sb('z', [128, NDC, TS])
    wr = C.ring('w_', 4, [128, NDC, 256], BF16)
    xcr = C.ring('xc_', 3, [128, TS], F32)
    scr = ln_scratch(C)
    accs = C.psring('acc', 2)
    s1 = (C.ps('s1'), 's1'); s2 = (C.ps('s2'), 's2')
    lgp = C.psring('lgp', 2)
    sm = {n: C.ring(n + '_', 2, [128, NEXP], F32) for n in ('l', 'mk', 'l2', 'sel', 'e', 'gt')}
    s1c = {n: C.ring(n + '_', 2, [128, 1], F32) for n in ('m1', 'm2', 'nm1', 'den')}
    mixT_v = mixT.rearrange("c p t -> p c t")
    xT_v = xT.rearrange("(dc p) t -> p dc t", p=128)
    xbT_v = xbT.rearrange("(dc p) t -> p dc t", p=128)
    xb16T_v = xb16T.rearrange("(dc p) t -> p dc t", p=128)
    w_out_v = w_out.rearrange("(ic p) n -> p ic n", p=128)
    for g in range(NT):
        t0 = g * TS
        for c in range(NDC):
            P.dma('sp', mbuf[:, c, :], mixT_v[:, c, t0:t0 + TS], w=[f'mbuf_{c}'])
        for og in range(8):
            w, wres = wr.next()
            cast_load(P, w, w_out_v[:, :, og * 256:(og + 1) * 256], NDC, wres, per_chunk=True)
            for o in range(2):
                oc = og * 2 + o
                acc, accr = accs.next()
                for ic in range(NDC):
                    P.matmul(acc[:, :], w[:, ic, o * 128:(o + 1) * 128], mbuf[:, ic, :], start=(ic == 0), stop=(ic == NDC - 1),
                             r=[f'{wres}_{ic}', f'mbuf_{ic}'], w=[accr])
                xc, xcres = xcr.next()
                P.dma('sp', xc[:, :], xT_v[:, oc, t0:t0 + TS], w=[xcres])
                P.op('act', lambda e, xc=xc: e.mul(out=xc[:, :], in_=xc[:, :], mul=ALPHA_C), [xcres], [xcres])
                P.stt('dve', z[:, oc, :], acc[:, :], 1.0, xc[:, :], ALU.mult, ALU.add, r=[accr, xcres], w=['z'])
        layernorm_fm(C, K, z, 'z', g1, b1, 'const_g1', z, 'z', xb16, 'xb16', s1, s2, scr)
        for oc in range(NDC):
            P.dma('sp', xbT_v[:, oc, t0:t0 + TS], z[:, oc, :], r=['z'], final=True)
            P.dma('sp', xb16T_v[:, oc, t0:t0 + TS], xb16[:, oc, :], r=['xb16'], final=True)
        for tb in range(4):
            lg, lgr = lgp.next()
            for dc in range(NDC):
                P.matmul(lg[:, 0:NEXP], z[:, dc, tb * 128:(tb + 1) * 128], rw32[:, dc, :], start=(dc == 0), stop=False,
                         r=['z', 'const_rw'], w=[lgr])
            P.matmul(lg[:, 0:NEXP], K['ones32'][0:1, :], rb32[0:1, :], start=False, stop=True, r=['const_ones32', 'const_rb'], w=[lgr])
            l, lr_ = sm['l'].next(); mk, mkr = sm['mk'].next(); l2, l2r = sm['l2'].next()
            sl, slr = sm['sel'].next(); ee, eer = sm['e'].next(); gt, gtr = sm['gt'].next()
            m1, m1r = s1c['m1'].next(); m2, m2r = s1c['m2'].next(); nm1, nm1r = s1c['nm1'].next(); den, denr = s1c['den'].next()
            P.copy('act', l[:, :], lg[:, 0:NEXP], r=[lgr], w=[lr_])
            P.op('dve', lambda e, m1=m1, l=l: e.tensor_reduce(out=m1[:, :], in_=l[:, :], axis=AX.X, op=ALU.max), [lr_], [m1r])
            P.ts('dve', mk[:, :], l[:, :], m1[:, 0:1], None, ALU.is_equal, r=[lr_, m1r], w=[mkr])
            P.stt('dve', l2[:, :], mk[:, :], -1.0e30, l[:, :], ALU.mult, ALU.add, r=[mkr, lr_], w=[l2r])
            P.op('dve', lambda e, m2=m2, l2=l2: e.tensor_reduce(out=m2[:, :], in_=l2[:, :], axis=AX.X, op=ALU.max), [l2r], [m2r])
            P.ts('dve', sl[:, :], l[:, :], m2[:, 0:1], None, ALU.is_ge, r=[lr_, m2r], w=[slr])
            P.ts('dve', nm1[:, :], m1[:, :], -1.0, None, ALU.mult, r=[m1r], w=[nm1r])
            P.activation(ee[:, :], l[:, :], AF.Exp, r=[lr_, nm1r], w=[eer], bias=nm1[:, 0:1])
            P.stt('dve', ee[:, :], ee[:, :], 1.0, sl[:, :], ALU.mult, ALU.mult, r=[eer, slr], w=[eer])
            P.op('dve', lambda e, den=den, ee=ee: e.tensor_reduce(out=den[:, :], in_=ee[:, :], axis=AX.X, op=ALU.add), [eer], [denr])
            P.recip(den[:, :], den[:, :], r=[denr], w=[denr])
            P.ts('dve', gt[:, :], ee[:, :], den[:, 0:1], None, ALU.mult, r=[eer, denr], w=[gtr])
            P.dma('sp', gates[t0 + tb * 128:t0 + (tb + 1) * 128, :], gt[:, :], r=[gtr], final=True)
    return C


NALL = 8 * NTOK


def build_l5b():
    C = Ctx()
    P = C.P
    xall = C.inp('xall', [D, NALL], BF16)
    grow = C.inp('grow', [1, NALL])
    wg_d = C.inp('wg', [D, DFF]); wu_d = C.inp('wu', [D, DFF]); wd_d = C.inp('wd', [DFF, D])
    yT = C.out('yT', [D, NALL], BF16)
    NP = 2
    xa16 = [C.sb(f'xa16_{t}', [128, NDC, TS], BF16) for t in range(NP)]
    gbs = [C.sb(f'gb_{t}', [128, TS], F32) for t in range(NP)]
    h16 = [C.sb(f'h16_{t}', [128, NFC, TS], BF16) for t in range(NP)]
    wr = C.ring('w_', 4, [128, NDC, 256], BF16)
    wdr = C.ring('wd_', 3, [128, 4, TS], BF16)
    sgr = C.ring('sg_', 3, [128, TS], F32)
    o16r = C.ring('o16_', 4, [128, TS], BF16)
    gu = C.psring('gu', 8)
    xall_v = xall.rearrange("(dc p) t -> p dc t", p=128)
    yT_v = yT.rearrange("(dc p) t -> p dc t", p=128)
    wg_v = wg_d.rearrange("(dc p) n -> p dc n", p=128)
    wu_v = wu_d.rearrange("(dc p) n -> p dc n", p=128)
    wd_v = wd_d.rearrange("(f p) n -> p f n", p=128)
    for pr in range(NALL // (TS * NP)):
        t0s = [(pr * NP + t) * TS for t in range(NP)]
        for t in range(NP):
            for dc in range(NDC):
                P.dma('sp', xa16[t][:, dc, :], xall_v[:, dc, t0s[t]:t0s[t] + TS], w=[f'xa{t}_{dc}'])
            P.dma('sp', gbs[t][:, :], grow[0:1, t0s[t]:t0s[t] + TS].partition_broadcast(128), w=[f'gb{t}'])
        for fg in range(DFF // 256):
            wgt, wgres = wr.next()
            cast_load(P, wgt, wg_v[:, :, fg * 256:(fg + 1) * 256], NDC, wgres, per_chunk=True)
            wut, wures = wr.next()
            cast_load(P, wut, wu_v[:, :, fg * 256:(fg + 1) * 256], NDC, wures, per_chunk=True)
            for fc in range(2):
                ffc = fg * 2 + fc
                for t in range(NP):
                    G, Gres = gu.next()
                    U, Ures = gu.next()
                    for dc in range(NDC):
                        P.matmul(G[:, :], wgt[:, dc, fc * 128:(fc + 1) * 128], xa16[t][:, dc, :], start=(dc == 0), stop=(dc == NDC - 1),
                                 r=[f'{wgres}_{dc}', f'xa{t}_{dc}'], w=[Gres])
                    for dc in range(NDC):
                        P.matmul(U[:, :], wut[:, dc, fc * 128:(fc + 1) * 128], xa16[t][:, dc, :], start=(dc == 0), stop=(dc == NDC - 1),
                                 r=[f'{wures}_{dc}', f'xa{t}_{dc}'], w=[Ures])
                    sg, sgres = sgr.next()
                    P.activation(sg[:, :], G[:, :], AF.Silu, r=[Gres], w=[sgres])
                    P.tt('pool', sg[:, :], sg[:, :], gbs[t][:, :], ALU.mult, r=[sgres, f'gb{t}'], w=[sgres])
                    P.stt('dve', h16[t][:, ffc, :], U[:, :], 1.0, sg[:, :], ALU.mult, ALU.mult, r=[Ures, sgres], w=[f'h16_{t}_{ffc}'])
        for ocg in range(4):
            Dacc = [[gu.next() for _ in range(4)] for t in range(NP)]
            for fq4 in range(NFC // 4):
                wd, wdres = wdr.next()
                cast_load(P, wd, wd_v[:, fq4 * 4:(fq4 + 1) * 4, ocg * TS:(ocg + 1) * TS], 4, wdres, per_chunk=True)
                for j in range(4):
                    ffc = fq4 * 4 + j
                    for t in range(NP):
                        for o in range(4):
                            P.matmul(Dacc[t][o][0][:, :], wd[:, j, o * 128:(o + 1) * 128], h16[t][:, ffc, :], start=(ffc == 0),
                                     stop=(ffc == NFC - 1), r=[f'{wdres}_{j}', f'h16_{t}_{ffc}'], w=[Dacc[t][o][1]])
            for t in range(NP):
                for o in range(4):
                    oc = ocg * 4 + o
                    o16, o16res = o16r.next()
                    P.copy('act', o16[:, :], Dacc[t][o][0][:, :], r=[Dacc[t][o][1]], w=[o16res])
                    P.dma('sp', yT_v[:, oc, t0s[t]:t0s[t] + TS], o16[:, :], r=[o16res], final=True)
    return C


def build_l5c():
    C = Ctx()
    P = C.P
    yparts = C.inp('yparts', [NEXP, D, NTOK], BF16)
    xT = C.inp('xT', [D, NTOK])
    ln2g = C.inp('ln2g', [128, NDC]); ln2b = C.inp('ln2b', [128, NDC])
    outT = C.out('outT', [D, NTOK])
    K = load_consts(C)
    g2 = C.sb('g2', [128, NDC]); b2 = C.sb('b2', [128, NDC])
    P.dma('sp', g2[:, :], ln2g, w=['const_g2']); P.dma('sp', b2[:, :], ln2b, w=['const_b2'])
    z = C.sb('z', [128, NDC, TS])
    x16 = C.sb('x16d', [128, NDC, TS], BF16)
    ypr = C.ring('yp_', 4, [128, NEXP, TS], BF16)
    scr = ln_scratch(C)
    s1 = (C.ps('s1'), 's1'); s2 = (C.ps('s2'), 's2')
    xT_v = xT.rearrange("(dc p) t -> p dc t", p=128)
    outT_v = outT.rearrange("(dc p) t -> p dc t", p=128)
    yp_v = yparts.rearrange("e (dc p) t -> p e dc t", p=128)
    for g in range(NT):
        t0 = g * TS
        for oc in range(NDC):
            P.dma('sp', z[:, oc, :], xT_v[:, oc, t0:t0 + TS], w=[f'z_{oc}'])
            P.op('act', lambda e, oc=oc: e.mul(out=z[:, oc, :], in_=z[:, oc, :], mul=ALPHA_C), [f'z_{oc}'], [f'z_{oc}'])
            yp, ypres = ypr.next()
            for ex in range(NEXP):
                P.dma('sp', yp[:, ex, :], yp_v[:, ex, oc, t0:t0 + TS], w=[f'{ypres}_{ex}'])
            for ex in range(NEXP):
                P.stt('dve', z[:, oc, :], yp[:, ex, :], 1.0, z[:, oc, :], ALU.mult, ALU.add, r=[f'{ypres}_{ex}', f'z_{oc}'], w=[f'z_{oc}'])
        zall = [f'z_{oc}' for oc in range(NDC)]
        layernorm_fm(C, K, z, zall, g2, b2, 'const_g2', z, 'zo', x16, 'x16d', s1, s2, scr)
        for oc in range(NDC):
            P.dma('sp', outT_v[:, oc, t0:t0 + TS], z[:, oc, :], r=['zo'], final=True)
    return C


def l5a_inputs(inp, mixF_list, x1T_list):
    common = {'w_out': np.ascontiguousarray(inp['od_w_out'][0]),
              'ln1g': fm_vec(inp['od_ln1_g'][0], NDC), 'ln1b': fm_vec(inp['od_ln1_b'][0], NDC),
              'rw': np.ascontiguousarray(inp['od_router_w'][0]), 'rb': np.ascontiguousarray(inp['od_router_b'][0].reshape(1, NEXP))}
    return [dict(common, mixT=mixF_list[c], xT=x1T_list[c]) for c in range(8)]


def l5b_inputs(inp, r5a):
    xall = np.ascontiguousarray(np.concatenate([r5a[c]['xb16T'] for c in range(8)], axis=1))
    gall = np.concatenate([r5a[c]['gates'] for c in range(8)], axis=0)
    maps = []
    for e in range(NEXP):
        maps.append({'xall': xall, 'grow': np.ascontiguousarray(gall[:, e].reshape(1, NALL)),
                     'wg': np.ascontiguousarray(inp['od_exp_w_gate'][0, e]), 'wu': np.ascontiguousarray(inp['od_exp_w_up'][0, e]),
                     'wd': np.ascontiguousarray(inp['od_exp_w_down'][0, e])})
    return maps


def l5c_inputs(inp, r5a, r5b):
    g2, b2 = fm_vec(inp['od_ln2_g'][0], NDC), fm_vec(inp['od_ln2_b'][0], NDC)
    maps = []
    for c in range(8):
        yp = np.ascontiguousarray(np.stack([r5b[e]['yT'][:, c * NTOK:(c + 1) * NTOK] for e in range(NEXP)]))
        maps.append({'yparts': yp, 'xT': r5a[c]['xbT'], 'ln2g': g2, 'ln2b': b2})
    return maps


def assemble_output(r5c):
    out = np.empty((2, 8192, D), np.float32)
    for c in range(8):
        out[c // 4][core_positions(c)] = r5c[c]['outT'].T
    return out


def kernel(**inputs):
    inp = {k: np.asarray(v) for k, v in inputs.items()}
    r1 = build_l1().run(l1_inputs(inp))
    m2, m2d = [], []
    for c in range(8):
        b, r = c // 4, c % 4
        grp = [r1[4 * b + r2] for r2 in range(4)]
        m2.append({'qm': r1[c]['qm'], 'kmf': np.stack([g['km'] for g in grp]), 'krf': np.stack([g['kr'] for g in grp]),
                   'vmf': np.stack([g['vm'] for g in grp]), 'cmask': causal_mask_tiles(r)})
        m2d.append({'dq': r1[c]['dq'], 'dkf': np.stack([g['dk'] for g in grp]), 'dvf': np.stack([g['dv'] for g in grp]),
                    'dmask': dil_mask_tiles(r)})
    r2 = build_l2_mla().run(m2)
    r2d = build_l2_dil().run(m2d)
    del r1, m2, m2d
    mix0 = [np.ascontiguousarray(np.concatenate([r2[c]['mixT'], r2d[c]['mixD']], axis=0)) for c in range(8)]
    r3 = build_l3().run(l3_inputs(inp, mix0))
    r4 = build_l4().run(l4_inputs(r3))
    r5a = build_l5a().run(l5a_inputs(inp, [r4[c]['mixF'] for c in range(8)], [r3[c]['x1T'] for c in range(8)]))
    del r3, r4
    r5b = build_l5b().run(l5b_inputs(inp, r5a))
    r5c = build_l5c().run(l5c_inputs(inp, r5a, r5b))
    return assemble_output(r5c)
```

```python
import numpy as np
import concourse.bass as bass
import concourse.mybir as mybir
from concourse.bass_utils import run_bass_kernel_spmd

F32 = mybir.dt.float32
BF16 = mybir.dt.bfloat16
I32 = mybir.dt.int32
AF = mybir.ActivationFunctionType
ALU = mybir.AluOpType
AX = mybir.AxisListType

ENGS = ['pe', 'act', 'dve', 'pool', 'sp']
SEM_CAP = 30000
DMA_POOL = 12


class Op:
    __slots__ = ('eng', 'fn', 'reads', 'writes', 'dma', 'idx', 'pos', 'waits', 'snap',
                 'milestone', 'ms', 'dsem', 'dval', 'dprev')

    def __init__(self, eng, fn, reads, writes, dma):
        self.eng = eng
        self.fn = fn
        self.reads = reads
        self.writes = writes
        self.dma = dma
        self.milestone = False
        self.waits = ()
        self.ms = -1


class Prog:
    def __init__(self, nc, same_sync=True):
        self.nc = nc
        self.ops = []
        self.same_sync = same_sync
        self.final_dmas = []

    def op(self, eng, fn, reads=(), writes=(), dma=False):
        o = Op(eng, fn, tuple(reads), tuple(writes), dma)
        self.ops.append(o)
        return o

    def pe(self, fn, r=(), w=()):
        return self.op('pe', fn, r, w)

    def act(self, fn, r=(), w=()):
        return self.op('act', fn, r, w)

    def dve(self, fn, r=(), w=()):
        return self.op('dve', fn, r, w)

    def pool(self, fn, r=(), w=()):
        return self.op('pool', fn, r, w)

    def dma(self, q, out, in_, r=(), w=(), final=False, **kw):
        o = self.op(q, lambda e: e.dma_start(out=out, in_=in_, **kw), r, w, dma=True)
        if final:
            self.final_dmas.append(o)
        return o

    def matmul(self, out, lhsT, rhs, start=True, stop=True, r=(), w=()):
        return self.op('pe', lambda e: e.matmul(out, lhsT, rhs, start=start, stop=stop), r, w)

    def transpose(self, out, in_, ident, r=(), w=()):
        return self.op('pe', lambda e: e.transpose(out, in_, ident), r, w)

    def activation(self, out, in_, func, r=(), w=(), **kw):
        return self.op('act', lambda e: e.activation(out=out, in_=in_, func=func, **kw), r, w)

    def tt(self, eng, out, in0, in1, op, r=(), w=()):
        return self.op(eng, lambda e: e.tensor_tensor(out=out, in0=in0, in1=in1, op=op), r, w)

    def ts(self, eng, out, in0, s1, s2, op0, op1=None, r=(), w=(), **kw):
        if op1 is None:
            return self.op(eng, lambda e: e.tensor_scalar(out=out, in0=in0, scalar1=s1, scalar2=s2, op0=op0, **kw), r, w)
        return self.op(eng, lambda e: e.tensor_scalar(out=out, in0=in0, scalar1=s1, scalar2=s2, op0=op0, op1=op1, **kw), r, w)

    def stt(self, eng, out, in0, scalar, in1, op0, op1, r=(), w=()):
        return self.op(eng, lambda e: e.scalar_tensor_tensor(out=out, in0=in0, scalar=scalar, in1=in1, op0=op0, op1=op1), r, w)

    def copy(self, eng, out, in_, r=(), w=()):
        if eng == 'act':
            return self.op(eng, lambda e: e.copy(out=out, in_=in_), r, w)
        return self.op(eng, lambda e: e.tensor_copy(out=out, in_=in_), r, w)

    def memset(self, eng, ap, val, w=()):
        return self.op(eng, lambda e: e.memset(ap, val), (), w)

    def recip(self, out, in_, r=(), w=()):
        return self.op('dve', lambda e: e.reciprocal(out=out, in_=in_), r, w)

    def analyze(self):
        per = {e: [] for e in ENGS}
        for i, o in enumerate(self.ops):
            o.idx = i
            o.pos = len(per[o.eng])
            per[o.eng].append(o)
        self.per = per
        last_w = {}
        readers = {}
        known = {e: {f: -1 for f in ENGS} for e in ENGS}
        known_dma = {e: set() for e in ENGS}
        for o in self.ops:
            deps = {}
            for r in o.reads:
                w = last_w.get(r)
                if w is not None:
                    deps[w.idx] = w
            for r in o.writes:
                w = last_w.get(r)
                if w is not None:
                    deps[w.idx] = w
                rd = readers.get(r)
                if rd:
                    for x in rd.values():
                        deps[x.idx] = x
            deps.pop(o.idx, None)
            kn = known[o.eng]
            kd = known_dma[o.eng]
            waits = []
            for d in deps.values():
                if d.dma:
                    if d.idx in kd:
                        continue
                    waits.append(d)
                else:
                    if d.eng == o.eng and not o.dma:
                        if d.eng == 'pe' or not self.same_sync:
                            continue
                    if kn[d.eng] >= d.pos:
                        continue
                    waits.append(d)
            for d in waits:
                d.milestone = True
                if d.dma:
                    kd.add(d.idx)
                else:
                    if kn[d.eng] < d.pos:
                        kn[d.eng] = d.pos
                for f, p in d.snap.items():
                    if kn[f] < p:
                        kn[f] = p
            o.waits = waits
            o.snap = dict(kn)
            for r in o.writes:
                last_w[r] = o
                readers[r] = {}
            for r in o.reads:
                if r.startswith('const'):
                    continue
                rd = readers.setdefault(r, {})
                key = o.idx if o.dma else o.eng
                rd[key] = o
        for o in self.final_dmas:
            o.milestone = True

    def emit(self):
        nc = self.nc
        self.analyze()
        per = self.per
        import contextlib
        with contextlib.ExitStack() as st:
            sems = {}
            for e in ENGS:
                n = 0
                for o in per[e]:
                    if o.dma:
                        continue
                    if o.milestone:
                        o.ms = n
                        n += 1
                nsem = n // SEM_CAP + 1
                sems[e] = [st.enter_context(nc.semaphore(f"ms_{e}_{k}")) for k in range(nsem)]
            dpool = {}
            for e in ENGS:
                dm = [o for o in per[e] if o.dma]
                if not dm:
                    continue
                pool = [st.enter_context(nc.semaphore(f"dq_{e}_{k}")) for k in range(DMA_POOL)]
                vals = [0] * DMA_POOL
                k = 0
                for o in dm:
                    o.dsem = pool[k]
                    o.dprev = vals[k]
                    vals[k] += 16
                    o.dval = vals[k]
                    k = (k + 1) % DMA_POOL
            block = st.enter_context(nc.Block())

            def run(e, eng):
                for o in per[e]:
                    for d in o.waits:
                        if d.dma:
                            eng.wait_ge(d.dsem, d.dval)
                        else:
                            eng.wait_ge(sems[d.eng][d.ms // SEM_CAP], d.ms % SEM_CAP + 1)
                    if o.dma:
                        if o.dprev > 0:
                            eng.wait_ge(o.dsem, o.dprev)
                        ins = o.fn(eng)
                        ins.then_inc(o.dsem, 16)
                    else:
                        ins = o.fn(eng)
                        if o.milestone:
                            ins.then_inc(sems[e][o.ms // SEM_CAP], 1)
                if e == 'sp':
                    for o in self.final_dmas:
                        eng.wait_ge(o.dsem, o.dval)

            if per['pe']:
                block.tensor(lambda eng: run('pe', eng))
            if per['act']:
                block.scalar(lambda eng: run('act', eng))
            if per['dve']:
                block.vector(lambda eng: run('dve', eng))
            if per['pool']:
                block.gpsimd(lambda eng: run('pool', eng))
            block.sync(lambda eng: run('sp', eng))


D = 2048
NTOK = 2048
TS = 512
NT = NTOK // TS
NDC = D // 128
DFF = 5632
NFC = DFF // 128
NEXP = 8
ALPHA_C = 4.0 ** 0.25
LN_EPS = 1e-5
RMS_EPS = 1e-6
NEG = -30000.0
OFF_CQ, OFF_CKV, OFF_KR, OFF_DQ, OFF_DK, OFF_DV = 0, 448, 576, 640, 2944, 3712


class Ctx:
    def __init__(self):
        self.nc = bass.Bass("TRN2", target_bir_lowering=False)
        self.P = Prog(self.nc)
        self.in_names = []
        self.out_names = []
        self._n = 0

    def inp(self, name, shape, dt=F32):
        self.in_names.append(name)
        return self.nc.dram_tensor(name, list(shape), dt, kind="ExternalInput").ap()

    def out(self, name, shape, dt=F32):
        self.out_names.append(name)
        return self.nc.dram_tensor(name, list(shape), dt, kind="ExternalOutput").ap()

    def sb(self, name, shape, dt=F32):
        return self.nc.alloc_sbuf_tensor(name, list(shape), dt)

    def ps(self, name):
        return self.nc.alloc_psum_tensor(name, [128, 512], F32)

    def ring(self, name, n, shape, dt):
        return Ring([(self.sb(f"{name}{i}", shape, dt), f"{name}{i}") for i in range(n)])

    def psring(self, name, n):
        return Ring([(self.ps(f"{name}{i}"), f"{name}{i}") for i in range(n)])

    def run(self, in_maps):
        self.P.emit()
        res = run_bass_kernel_spmd(self.nc, in_maps, core_ids=list(range(len(in_maps))))
        if res.exec_time_ns is not None:
            print("[launch exec_time_ns]", res.exec_time_ns, flush=True)
        return res.results


class Ring:
    def __init__(self, items):
        self.items = items
        self.i = 0

    def next(self):
        it = self.items[self.i % len(self.items)]
        self.i += 1
        return it


def cast_load(P, dst, src, n, res, q='pool', maxcols=1024, per_chunk=False):
    cols = dst.shape[-1]
    for i in range(n):
        for c0 in range(0, cols, maxcols):
            c1 = min(cols, c0 + maxcols)
            P.dma(q, dst[:, i, c0:c1], src[:, i, c0:c1], w=[f"{res}_{i}" if per_chunk else res])


def load_consts(C, need_rot=False):
    P = C.P
    k = {}
    k['ones32'] = C.sb('ones32', [128, 128], F32)
    k['ones16'] = C.sb('ones16', [128, 128], BF16)
    P.memset('dve', k['ones32'][:, :], 1.0, w=['const_ones32'])
    P.memset('dve', k['ones16'][:, :], 1.0, w=['const_ones16'])
    return k


def layernorm_fm(C, K, z, zres, g_sb, b_sb, gres, xo, xo_res, x16, x16_res, ps_s1, ps_s2, scr):
    P = C.P
    (s1, s1r), (s2, s2r) = ps_s1, ps_s2
    sq = scr['sq']
    zl = list(zres) if isinstance(zres, (list, tuple)) else [zres]
    for oc in range(NDC):
        P.matmul(s1[:, :], K['ones32'][:, :], z[:, oc, :], start=(oc == 0), stop=(oc == NDC - 1),
                 r=zl + ['const_ones32'], w=[s1r])
    for oc in range(NDC):
        t, tr = sq.next()
        P.activation(t[:, :], z[:, oc, :], AF.Square, r=zl, w=[tr])
        P.matmul(s2[:, :], K['ones32'][:, :], t[:, :], start=(oc == 0), stop=(oc == NDC - 1),
                 r=[tr, 'const_ones32'], w=[s2r])
    mean, m2, rstd = scr['mean'], scr['m2'], scr['rstd']
    P.ts('dve', mean[:, :], s1[:, :], 1.0 / D, None, ALU.mult, r=[s1r], w=['ln_mean'])
    P.stt('dve', m2[:, :], mean[:, :], 1.0, mean[:, :], ALU.mult, ALU.mult, r=['ln_mean'], w=['ln_m2'])
    P.stt('dve', m2[:, :], s2[:, :], 1.0 / D, m2[:, :], ALU.mult, ALU.subtract, r=[s2r, 'ln_m2'], w=['ln_m2'])
    P.ts('dve', m2[:, :], m2[:, :], LN_EPS, None, ALU.add, r=['ln_m2'], w=['ln_m2'])
    P.activation(rstd[:, :], m2[:, :], AF.Sqrt, r=['ln_m2'], w=['ln_rstd'])
    P.recip(rstd[:, :], rstd[:, :], r=['ln_rstd'], w=['ln_rstd'])
    for oc in range(NDC):
        t, tr = sq.next()
        P.stt('dve', t[:, :], z[:, oc, :], 1.0, mean[:, :], ALU.mult, ALU.subtract, r=zl + ['ln_mean'], w=[tr])
        P.stt('dve', t[:, :], t[:, :], 1.0, rstd[:, :], ALU.mult, ALU.mult, r=[tr, 'ln_rstd'], w=[tr])
        P.ts('dve', xo[:, oc, :], t[:, :], g_sb[:, oc:oc + 1], b_sb[:, oc:oc + 1], ALU.mult, ALU.add,
             r=[tr, gres], w=[xo_res] + (zl if xo is z else []))
        P.copy('act', x16[:, oc, :], xo[:, oc, :], r=[xo_res], w=[x16_res(oc) if callable(x16_res) else x16_res])


def ln_scratch(C):
    return {'sq': C.ring('lnsq', 3, [128, 512], F32), 'mean': C.sb('ln_mean', [128, 512]),
            'm2': C.sb('ln_m2', [128, 512]), 'rstd': C.sb('ln_rstd', [128, 512])}


def rope_fm(C, n, acc, accr, cs_sb, col0, Rm, rings, out_ap, sfx):
    P = C.P
    t16, t16r = rings['t16'].next()
    rot, rotr = rings['rot'].next()
    a32, a32r = rings['a32'].next()
    b32, b32r = rings['b32'].next()
    o16, o16r = rings['o16'].next()
    P.copy('act', t16[0:n, :], acc[0:n, :], r=[accr], w=[t16r])
    P.matmul(rot[0:n, :], Rm[0:n, 0:n], t16[0:n, :], r=[t16r, 'const_R' + sfx], w=[rotr])
    P.copy('act', a32[0:n, :], acc[0:n, :], r=[accr], w=[a32r])
    P.copy('act', b32[0:n, :], rot[0:n, :], r=[rotr], w=[b32r])
    P.tt('pool', a32[0:n, :], a32[0:n, :], cs_sb[0:n, 0, col0:col0 + TS], ALU.mult, r=[a32r, 'const_cs' + sfx], w=[a32r])
    P.tt('pool', b32[0:n, :], b32[0:n, :], cs_sb[0:n, 1, col0:col0 + TS], ALU.mult, r=[b32r, 'const_cs' + sfx], w=[b32r])
    P.tt('pool', o16[0:n, :], a32[0:n, :], b32[0:n, :], ALU.add, r=[a32r, b32r], w=[o16r])
    P.dma('sp', out_ap, o16[0:n, :], r=[o16r], final=True)


def build_l1():
    C = Ctx()
    P = C.P
    xT = C.inp('xT', [D, NTOK])
    w_in = C.inp('w_in', [D, 4480])
    qn = C.inp('qn', [128, 4])
    w_qb = C.inp('w_qb', [512, 1920])
    kvn = C.inp('kvn', [128, 1])
    w_kvb = C.inp('w_kvb', [128, 2560])
    cs64 = C.inp('cs64', [64, 2, NTOK])
    cs128 = C.inp('cs128', [128, 2, NTOK])
    r64 = C.inp('r64', [64, 64])
    r128 = C.inp('r128', [128, 128])
    qm = C.out('qm', [10, 192, NTOK], BF16)
    km = C.out('km', [10, 128, NTOK], BF16)
    kr = C.out('kr', [64, NTOK], BF16)
    vm = C.out('vm', [10, 128, 16, 128], BF16)
    dq = C.out('dq', [18, 128, NTOK], BF16)
    dk = C.out('dk', [6, 128, NTOK], BF16)
    dv = C.out('dv', [6, 128, 16, 128], BF16)
    K = load_consts(C)
    qn_sb = C.sb('qn_sb', [128, 4])
    kvn_sb = C.sb('kvn_sb', [128, 1])
    cs64_sb = C.sb('cs64_sb', [64, 2, NTOK])
    cs128_sb = C.sb('cs128_sb', [128, 2, NTOK])
    R64 = C.sb('R64', [64, 64], BF16)
    R128 = C.sb('R128', [128, 128], BF16)
    wqb16 = C.sb('wqb16', [128, 4, 1920], BF16)
    wk16 = C.sb('wk16', [128, 10, 128], BF16)
    wv16 = C.sb('wv16', [128, 10, 128], BF16)
    P.dma('sp', qn_sb[:, :], qn, w=['const_qn'])
    P.dma('sp', kvn_sb[:, :], kvn, w=['const_kvn'])
    cast_load(P, cs64_sb, cs64, 2, 'const_cs64', q='sp', maxcols=2048)
    cast_load(P, cs128_sb, cs128, 2, 'const_cs128', q='sp', maxcols=2048)
    P.dma('pool', R64[:, :], r64, w=['const_R64'])
    P.dma('pool', R128[:, :], r128, w=['const_R128'])
    cast_load(P, wqb16, w_qb.rearrange("(kc p) n -> p kc n", p=128), 4, 'const_wqb', maxcols=480)
    wkv_v = w_kvb.rearrange("r (h two c) -> r two h c", two=2, c=128)
    cast_load(P, wk16, wkv_v[:, 0, :, :], 10, 'const_wk')
    cast_load(P, wv16, wkv_v[:, 1, :, :], 10, 'const_wv')

    x16r = C.ring('x16_', 2, [128, NDC, TS], BF16)
    wgr = C.ring('wg_', 2, [128, NDC, 640], BF16)
    accs = C.psring('acc', 3)
    rings = {'t16': C.ring('t16_', 2, [128, TS], BF16), 'rot': C.psring('rot', 2),
             'a32': C.ring('a32_', 2, [128, TS], F32), 'b32': C.ring('b32_', 2, [128, TS], F32),
             'o16': C.ring('o16_', 4, [128, TS], BF16)}
    ssq = (C.ps('ssq'), 'ssq')
    sskv = (C.ps('sskv'), 'sskv')
    sqr = C.ring('sq_', 2, [128, TS], F32)
    cq32 = C.sb('cq32', [128, 4, TS])
    cqn16 = C.sb('cqn16', [128, 4, TS], BF16)
    ckv32 = C.sb('ckv32', [128, TS])
    ckvn16 = C.sb('ckvn16', [128, TS], BF16)
    rstdq = C.sb('rstdq', [128, TS])
    rstdkv = C.sb('rstdkv', [128, TS])
    vst = C.ring('vst_', 2, [128, 10, 128], BF16)
    xT_v = xT.rearrange("(dc p) t -> p dc t", p=128)
    w_in_v = w_in.rearrange("(dc p) n -> p dc n", p=128)

    def proj(acc, accr, x16, x16res, wg, wgres, c0, n):
        for dc in range(NDC):
            P.matmul(acc[0:n, :], wg[:, dc, c0:c0 + n], x16[:, dc, :], start=(dc == 0), stop=(dc == NDC - 1),
                     r=[x16res, wgres], w=[accr])

    def rms(ss, ssr, n_feat, rstd, rstdres):
        P.ts('dve', rstd[:, :], ss[:, :], 1.0 / n_feat, RMS_EPS, ALU.mult, ALU.add, r=[ssr], w=[rstdres])
        P.activation(rstd[:, :], rstd[:, :], AF.Sqrt, r=[rstdres], w=[rstdres])
        P.recip(rstd[:, :], rstd[:, :], r=[rstdres], w=[rstdres])

    for g in range(NT):
        t0 = g * TS
        x16, x16res = x16r.next()
        cast_load(P, x16, xT_v[:, :, t0:t0 + TS], NDC, x16res)
        wg, wgres = wgr.next()
        cast_load(P, wg[:, :, 0:640], w_in_v[:, :, 0:640], NDC, wgres)
        cq_sizes = [128, 128, 128, 64]
        for cc, n in enumerate(cq_sizes):
            acc, accr = accs.next()
            proj(acc, accr, x16, x16res, wg, wgres, cc * 128, n)
            P.copy('act', cq32[0:n, cc, :], acc[0:n, :], r=[accr], w=['cq32'])
            sq, sqres = sqr.next()
            P.activation(sq[0:n, :], acc[0:n, :], AF.Square, r=[accr], w=[sqres])
            P.matmul(ssq[0][:, :], K['ones32'][0:n, :], sq[0:n, :], start=(cc == 0), stop=(cc == 3),
                     r=[sqres, 'const_ones32'], w=['ssq'])
        rms(ssq[0], 'ssq', 448.0, rstdq, 'rstdq')
        for cc, n in enumerate(cq_sizes):
            P.stt('dve', cqn16[0:n, cc, :], cq32[0:n, cc, :], qn_sb[0:n, cc:cc + 1], rstdq[0:n, :], ALU.mult, ALU.mult,
                  r=['cq32', 'rstdq', 'const_qn'], w=['cqn16'])
        acc, accr = accs.next()
        proj(acc, accr, x16, x16res, wg, wgres, OFF_CKV, 128)
        P.copy('act', ckv32[:, :], acc[:, :], r=[accr], w=['ckv32'])
        sq, sqres = sqr.next()
        P.activation(sq[:, :], acc[:, :], AF.Square, r=[accr], w=[sqres])
        P.matmul(sskv[0][:, :], K['ones32'][:, :], sq[:, :], r=[sqres, 'const_ones32'], w=['sskv'])
        rms(sskv[0], 'sskv', 128.0, rstdkv, 'rstdkv')
        P.stt('dve', ckvn16[:, :], ckv32[:, :], kvn_sb[:, 0:1], rstdkv[:, :], ALU.mult, ALU.mult,
              r=['ckv32', 'rstdkv', 'const_kvn'], w=['ckvn16'])
        acc, accr = accs.next()
        proj(acc, accr, x16, x16res, wg, wgres, OFF_KR, 64)
        rope_fm(C, 64, acc, accr, cs64_sb, t0, R64, rings, kr[:, t0:t0 + TS], '64')
        for h in range(10):
            acc, accr = accs.next()
            for kc, kn in enumerate(cq_sizes):
                P.matmul(acc[:, :], wqb16[0:kn, kc, h * 192:h * 192 + 128], cqn16[0:kn, kc, :], start=(kc == 0), stop=(kc == 3),
                         r=['cqn16', 'const_wqb'], w=[accr])
            o16, o16r = rings['o16'].next()
            P.copy('act', o16[:, :], acc[:, :], r=[accr], w=[o16r])
            P.dma('sp', qm[h, 0:128, t0:t0 + TS], o16[:, :], r=[o16r], final=True)
            acc, accr = accs.next()
            for kc, kn in enumerate(cq_sizes):
                P.matmul(acc[0:64, :], wqb16[0:kn, kc, h * 192 + 128:h * 192 + 192], cqn16[0:kn, kc, :], start=(kc == 0),
                         stop=(kc == 3), r=['cqn16', 'const_wqb'], w=[accr])
            rope_fm(C, 64, acc, accr, cs64_sb, t0, R64, rings, qm[h, 128:192, t0:t0 + TS], '64')
        for h in range(10):
            acc, accr = accs.next()
            P.matmul(acc[:, :], wk16[:, h, :], ckvn16[:, :], r=['ckvn16', 'const_wk'], w=[accr])
            o16, o16r = rings['o16'].next()
            P.copy('act', o16[:, :], acc[:, :], r=[accr], w=[o16r])
            P.dma('sp', km[h, :, t0:t0 + TS], o16[:, :], r=[o16r], final=True)
        for tb in range(4):
            vs, vsr = vst.next()
            for (h0, h1) in [(0, 4), (4, 8), (8, 10)]:
                acc, accr = accs.next()
                nn = (h1 - h0) * 128
                P.matmul(acc[:, 0:nn], ckvn16[:, tb * 128:(tb + 1) * 128], wv16[:, h0:h1, :], r=['ckvn16', 'const_wv'], w=[accr])
                P.copy('act', vs[:, h0:h1, :], acc[:, 0:nn].rearrange("p (h c) -> p h c", c=128), r=[accr], w=[vsr])
            P.dma('sp', vm[:, :, g * 4 + tb, :].rearrange("h p c -> p h c"), vs[:, :, :], r=[vsr], final=True)
        for gi in range(6):
            wg, wgres = wgr.next()
            c_lo = OFF_DQ + gi * 512
            cast_load(P, wg[:, :, 0:512], w_in_v[:, :, c_lo:c_lo + 512], NDC, wgres)
            for j in range(4):
                ci = gi * 4 + j
                acc, accr = accs.next()
                proj(acc, accr, x16, x16res, wg, wgres, j * 128, 128)
                dst = dq[ci, :, t0:t0 + TS] if ci < 18 else dk[ci - 18, :, t0:t0 + TS]
                rope_fm(C, 128, acc, accr, cs128_sb, t0, R128, rings, dst, '128')
        for (c_lo, ncol, h0) in [(OFF_DV, 512, 0), (OFF_DV + 512, 256, 4)]:
            wg, wgres = wgr.next()
            cast_load(P, wg[:, :, 0:ncol], w_in_v[:, :, c_lo:c_lo + ncol], NDC, wgres)
            nh = ncol // 128
            for tb in range(4):
                acc, accr = accs.next()
                for dc in range(NDC):
                    P.matmul(acc[:, 0:ncol], x16[:, dc, tb * 128:(tb + 1) * 128], wg[:, dc, 0:ncol], start=(dc == 0),
                             stop=(dc == NDC - 1), r=[x16res, wgres], w=[accr])
                vs, vsr = vst.next()
                P.copy('act', vs[:, 0:nh, :], acc[:, 0:ncol].rearrange("p (h c) -> p h c", c=128), r=[accr], w=[vsr])
                P.dma('sp', dv[h0:h0 + nh, :, g * 4 + tb, :].rearrange("h p c -> p h c"), vs[:, 0:nh, :], r=[vsr], final=True)
    return C


def core_positions(c):
    r = c % 4
    t = np.arange(NTOK)
    return 512 * (4 * (t // 512) + r) + (t % 512)


def rope_table_fm(pos, dim):
    half = dim // 2
    inv_freq = (1.0 / (np.float32(10000.0) ** (np.arange(0, dim, 2, dtype=np.float32) / np.float32(dim)))).astype(np.float32)
    ang = pos.astype(np.float32)[None, :] * inv_freq[:, None]
    cos = np.cos(ang).astype(np.float32)
    sin = np.sin(ang).astype(np.float32)
    out = np.empty((dim, 2, pos.shape[0]), np.float32)
    out[:half, 0] = cos
    out[half:, 0] = cos
    out[:half, 1] = sin
    out[half:, 1] = sin
    return out


def rot_lhsT(dim):
    half = dim // 2
    m = np.zeros((dim, dim), np.float32)
    for j in range(half):
        m[j + half, j] = -1.0
        m[j, j + half] = 1.0
    return m


def fm_vec(v, n_chunks):
    o = np.zeros((n_chunks * 128,), np.float32)
    o[:v.shape[0]] = v
    return np.ascontiguousarray(o.reshape(n_chunks, 128).T)


def l1_inputs(inp):
    maps = []
    wqb = np.zeros((512, 1920), np.float32)
    wqb[:448] = inp['ev_w_q_b'][0]
    common = {
        'w_in': np.ascontiguousarray(inp['ev_w_in'][0]), 'qn': fm_vec(inp['ev_q_norm'][0], 4), 'w_qb': wqb,
        'kvn': fm_vec(inp['ev_kv_norm'][0], 1), 'w_kvb': np.ascontiguousarray(inp['ev_w_kv_b'][0]),
        'r64': rot_lhsT(64), 'r128': rot_lhsT(128),
    }
    for c in range(8):
        pos = core_positions(c)
        m = dict(common)
        m['xT'] = np.ascontiguousarray(inp['x'][c // 4][pos].T)
        m['cs64'] = rope_table_fm(pos, 64)
        m['cs128'] = rope_table_fm(pos, 128)
        maps.append(m)
    return maps


def attn_pipeline(blocks, s_stage, pv_stage, L=2):
    n = len(blocks)
    st = [None] * n
    for i in range(n + L):
        if i < n:
            st[i] = s_stage(i, blocks[i])
        j = i - L
        if j >= 0:
            pv_stage(j, blocks[j], st[j], j == 0, j == n - 1)


def build_l2_mla():
    C = Ctx()
    P = C.P
    qm = C.inp('qm', [10, 192, NTOK], BF16)
    kmf = C.inp('kmf', [4, 10, 128, NTOK], BF16)
    krf = C.inp('krf', [4, 64, NTOK], BF16)
    vmf = C.inp('vmf', [4, 10, 128, 16, 128], BF16)
    cmask = C.inp('cmask', [128, 16, TS], BF16)
    mixT = C.out('mixT', [10, 128, NTOK], BF16)
    K = load_consts(C)
    scale = 192.0 ** -0.5
    cm = C.sb('cm', [128, 16, TS], BF16)
    cast_load(P, cm, cmask, 16, 'const_cm', q='sp', maxcols=TS)
    krT = C.sb('krT', [64, 4, NTOK], BF16)
    cast_load(P, krT, krf.rearrange("r p t -> p r t"), 4, 'const_kr', q='sp', maxcols=NTOK)
    kTr = C.ring('kT_', 2, [128, 4, NTOK], BF16)
    vr = C.ring('v_', 2, [128, 4, 16 * 128], BF16)
    qnr = C.ring('qn_', 2, [128, NTOK], BF16)
    qrr = C.ring('qr_', 2, [64, NTOK], BF16)
    Sr = C.psring('S', 3)
    Or = C.psring('O', 2)
    Dr = C.psring('Dn', 2)
    pTr = C.ring('pT_', 5, [128, TS], BF16)
    tmpr = C.ring('tmp_', 3, [128, TS], F32)
    rec = C.sb('rec', [128, TS])
    o32 = C.sb('o32', [128, TS])
    o16r = C.ring('ao16_', 2, [128, TS], BF16)
    for h in range(10):
        kT, kTres = kTr.next()
        v, vres = vr.next()
        qn, qnres = qnr.next()
        qr, qrres = qrr.next()
        for r2 in range(4):
            P.dma('sp', kT[:, r2, :], kmf[r2, h, :, :], w=[kTres + f'_{r2}'])
            P.dma('sp', v[:, r2, :], vmf[r2, h, :, :, :].rearrange("p b c -> p (b c)"), w=[vres + f'_{r2}'])
        P.dma('sp', qn[:, :], qm[h, 0:128, :], w=[qnres])
        P.dma('sp', qr[:, :], qm[h, 128:192, :], w=[qrres])
        for g in range(NT):
            O, Ores = Or.next()
            Dn, Dres = Dr.next()
            blocks = [(g2, r2, kb) for g2 in range(g + 1) for r2 in range(4) for kb in range(4)]

            def s_stage(i, b, g=g, kT=kT, kTres=kTres, qn=qn, qnres=qnres, qr=qr, qrres=qrres):
                g2, r2, kb = b
                S, Sres = Sr.next()
                pT, pTres = pTr.next()
                c0 = g2 * TS + kb * 128
                P.matmul(S[:, :], kT[:, r2, c0:c0 + 128], qn[:, g * TS:(g + 1) * TS], start=True, stop=False,
                         r=[kTres + f'_{r2}', qnres], w=[Sres])
                P.matmul(S[:, :], krT[0:64, r2, c0:c0 + 128], qr[0:64, g * TS:(g + 1) * TS], start=False, stop=True,
                         r=['const_kr', qrres], w=[Sres])
                if g2 == g:
                    tmp, tmpres = tmpr.next()
                    P.stt('dve', tmp[:, :], S[:, :], scale, cm[:, r2 * 4 + kb, :], ALU.mult, ALU.add,
                          r=[Sres, 'const_cm'], w=[tmpres])
                    P.activation(pT[:, :], tmp[:, :], AF.Exp, r=[tmpres], w=[pTres])
                else:
                    P.activation(pT[:, :], S[:, :], AF.Exp, r=[Sres], w=[pTres], scale=scale)
                return pT, pTres

            def pv_stage(j, b, stt_, first, last, v=v, vres=vres, O=O, Ores=Ores, Dn=Dn, Dres=Dres):
                g2, r2, kb = b
                pT, pTres = stt_
                blk = g2 * 4 + kb
                P.matmul(O[:, :], v[:, r2, blk * 128:(blk + 1) * 128], pT[:, :], start=first, stop=last,
                         r=[vres + f'_{r2}', pTres], w=[Ores])
                P.matmul(Dn[:, :], K['ones16'][:, :], pT[:, :], start=first, stop=last, r=['const_ones16', pTres], w=[Dres])

            attn_pipeline(blocks, s_stage, pv_stage)
            P.recip(rec[:, :], Dn[:, :], r=[Dres], w=['rec'])
            P.copy('act', o32[:, :], O[:, :], r=[Ores], w=['o32'])
            o16, o16res = o16r.next()
            P.tt('pool', o16[:, :], o32[:, :], rec[:, :], ALU.mult, r=['o32', 'rec'], w=[o16res])
            P.dma('sp', mixT[h, :, g * TS:(g + 1) * TS], o16[:, :], r=[o16res], final=True)
    return C


def causal_mask_tiles(r):
    m = np.zeros((128, 16, TS), np.float32)
    ki = np.arange(128)[:, None]
    qi = np.arange(TS)[None, :]
    for r2 in range(4):
        for kb in range(4):
            if r2 > r:
                m[:, r2 * 4 + kb, :] = NEG
            elif r2 == r:
                m[:, r2 * 4 + kb, :] = np.where(kb * 128 + ki <= qi, 0.0, NEG)
    import ml_dtypes
    return m.astype(ml_dtypes.bfloat16)


DIL_PATTERNS = ((128, 1), (512, 4), (2048, 16))


def _dil_slots(grp):
    if grp < 2:
        return [(3, 1), (0, 0), (1, 0), (2, 0), (3, 0)]
    return [(r2, 1) for r2 in range(4)] + [(r2, 0) for r2 in range(4)]


def _dil_mask_bool(r, grp, r2, dg, kb):
    W, d = DIL_PATTERNS[grp]
    ki = np.arange(128)[:, None]
    qi = np.arange(TS)[None, :]
    delta = 512 * (4 * dg + r - r2) + qi - (128 * kb + ki)
    return (delta >= 0) & (delta <= W) & (delta % d == 0)


def dil_block_list():
    blocks = []
    for grp in range(3):
        for (r2, dg) in _dil_slots(grp):
            for kb in range(4):
                if any(_dil_mask_bool(r, grp, r2, dg, kb).any() for r in range(4)):
                    blocks.append((grp, r2, dg, kb))
    return blocks


def dil_mask_tiles(r):
    import ml_dtypes
    bl = dil_block_list()
    m = np.full((128, len(bl), TS), NEG, np.float32)
    for i, (grp, r2, dg, kb) in enumerate(bl):
        m[:, i, :] = np.where(_dil_mask_bool(r, grp, r2, dg, kb), 0.0, NEG)
    return m.astype(ml_dtypes.bfloat16)


def build_l2_dil():
    C = Ctx()
    P = C.P
    bl = dil_block_list()
    nb = len(bl)
    dq = C.inp('dq', [18, 128, NTOK], BF16)
    dkf = C.inp('dkf', [4, 6, 128, NTOK], BF16)
    dvf = C.inp('dvf', [4, 6, 128, 16, 128], BF16)
    dmask = C.inp('dmask', [128, nb, TS], BF16)
    mixT = C.out('mixD', [6, 128, NTOK], BF16)
    K = load_consts(C)
    scale = 128.0 ** -0.5
    dm = C.sb('dm', [128, nb, TS], BF16)
    for i in range(nb):
        P.dma('sp', dm[:, i, :], dmask[:, i, :], w=[f'const_dm{i}'])
    kT = C.sb('dkT', [128, 4, NTOK], BF16)
    v = C.sb('dv', [128, 4, 16 * 128], BF16)
    q3 = C.sb('dq3', [128, 3, NTOK], BF16)
    Sr = C.psring('S', 3)
    Or = C.psring('O', 2)
    Dr = C.psring('Dn', 2)
    pTr = C.ring('pT_', 5, [128, TS], BF16)
    tmpr = C.ring('tmp_', 3, [128, TS], F32)
    rec = C.sb('rec', [128, TS])
    o32 = C.sb('o32', [128, TS])
    o16r = C.ring('ao16_', 2, [128, TS], BF16)
    for hd in range(6):
        for r2 in range(4):
            P.dma('sp', kT[:, r2, :], dkf[r2, hd, :, :], w=[f'dkT_{r2}'])
            P.dma('sp', v[:, r2, :], dvf[r2, hd, :, :, :].rearrange("p b c -> p (b c)"), w=[f'dv_{r2}'])
        for grp in range(3):
            P.dma('sp', q3[:, grp, :], dq[grp * 6 + hd, :, :], w=[f'dq3_{grp}'])
        for g in range(NT):
            todo = [(i, b) for i, b in enumerate(bl) if g - b[2] >= 0]
            O, Ores = Or.next()
            Dn, Dres = Dr.next()
            def s_stage(_i, tb, g=g):
                i, (grp, r2, dg, kb) = tb
                g2 = g - dg
                S, Sres = Sr.next()
                pT, pTres = pTr.next()
                tmp, tmpres = tmpr.next()
                c0 = g2 * TS + kb * 128
                P.matmul(S[:, :], kT[:, r2, c0:c0 + 128], q3[:, grp, g * TS:(g + 1) * TS], r=[f'dkT_{r2}', f'dq3_{grp}'], w=[Sres])
                P.stt('dve', tmp[:, :], S[:, :], scale, dm[:, i, :], ALU.mult, ALU.add, r=[Sres, f'const_dm{i}'], w=[tmpres])
                P.activation(pT[:, :], tmp[:, :], AF.Exp, r=[tmpres], w=[pTres])
                return pT, pTres

            def pv_stage(_j, tb, stt_, first, last, g=g, O=O, Ores=Ores, Dn=Dn, Dres=Dres):
                i, (grp, r2, dg, kb) = tb
                pT, pTres = stt_
                blk = (g - dg) * 4 + kb
                P.matmul(O[:, :], v[:, r2, blk * 128:(blk + 1) * 128], pT[:, :], start=first, stop=last, r=[f'dv_{r2}', pTres], w=[Ores])
                P.matmul(Dn[:, :], K['ones16'][:, :], pT[:, :], start=first, stop=last, r=['const_ones16', pTres], w=[Dres])

            attn_pipeline(todo, s_stage, pv_stage)
            P.recip(rec[:, :], Dn[:, :], r=[Dres], w=['rec'])
            P.copy('act', o32[:, :], O[:, :], r=[Ores], w=['o32'])
            o16, o16res = o16r.next()
            P.tt('pool', o16[:, :], o32[:, :], rec[:, :], ALU.mult, r=['o32', 'rec'], w=[o16res])
            P.dma('sp', mixT[hd, :, g * TS:(g + 1) * TS], o16[:, :], r=[o16res], final=True)
    return C


def build_l3(debug_xa=False):
    C = Ctx()
    P = C.P
    mixT = C.inp('mixT', [16, 128, NTOK], BF16)
    xT = C.inp('xT', [D, NTOK])
    w_out = C.inp('w_out', [D, D])
    ln1g = C.inp('ln1g', [128, NDC]); ln1b = C.inp('ln1b', [128, NDC])
    ln2g = C.inp('ln2g', [128, NDC]); ln2b = C.inp('ln2b', [128, NDC])
    wg_d = C.inp('wg', [D, DFF]); wu_d = C.inp('wu', [D, DFF]); wd_d = C.inp('wd', [DFF, D])
    w_qkv = C.inp('w_qkv', [D, 6144])
    w_f = C.inp('w_f', [D, 16]); b_f = C.inp('b_f', [16, 1])
    x1T = C.out('x1T', [D, NTOK])
    fq = C.out('fq', [16, 128, NTOK], BF16)
    fk = C.out('fk', [16, 128, NTOK], BF16)
    fv = C.out('fv', [16, 128, 16, 128], BF16)
    logf = C.out('logf', [16, NTOK])
    xaT = C.out('xaT', [D, NTOK]) if debug_xa else None
    K = load_consts(C)
    g1 = C.sb('g1', [128, NDC]); b1 = C.sb('b1', [128, NDC]); g2 = C.sb('g2', [128, NDC]); b2 = C.sb('b2', [128, NDC])
    P.dma('sp', g1[:, :], ln1g, w=['const_g1']); P.dma('sp', b1[:, :], ln1b, w=['const_b1'])
    P.dma('sp', g2[:, :], ln2g, w=['const_g2']); P.dma('sp', b2[:, :], ln2b, w=['const_b2'])
    wf32 = C.sb('wf32', [128, NDC, 16])
    P.dma('sp', wf32[:, :, :], w_f.rearrange("(dc p) n -> p dc n", p=128), w=['const_wf'])
    nbf = C.sb('nbf', [16, 1])
    P.dma('sp', nbf[:, :], b_f, w=['const_nbf'])
    P.op('act', lambda e: e.mul(out=nbf[:, :], in_=nbf[:, :], mul=-1.0), ['const_nbf'], ['const_nbf2'])

    mbuf = C.sb('mbuf', [128, NDC, TS], BF16)
    z = C.sb('z', [128, NDC, TS])
    xa16 = C.sb('xa16', [128, NDC, TS], BF16)
    h16 = C.sb('h16', [128, NFC, TS], BF16)
    wr = C.ring('w_', 4, [128, NDC, 256], BF16)
    wdr = C.ring('wd_', 3, [128, 4, TS], BF16)
    xcr = C.ring('xc_', 3, [128, TS], F32)
    scr = ln_scratch(C)
    sgr = C.ring('sg_', 2, [128, TS], F32)
    o16r = C.ring('o16_', 4, [128, TS], BF16)
    vsr_ = C.ring('vs_', 2, [128, 4, 128], BF16)
    lf = C.ring('lf_', 2, [16, TS], F32)
    accs = C.psring('acc', 2)
    gu = C.psring('gu', 4)
    s1 = (C.ps('s1'), 's1'); s2 = (C.ps('s2'), 's2')
    mixT_v = mixT.rearrange("c p t -> p c t")
    xT_v = xT.rearrange("(dc p) t -> p dc t", p=128)
    x1T_v = x1T.rearrange("(dc p) t -> p dc t", p=128)

    def wload(src_v, c_lo):
        w, wres = wr.next()
        cast_load(P, w, src_v[:, :, c_lo:c_lo + 256], NDC, wres, per_chunk=True)
        return w, wres

    for g in range(NT):
        t0 = g * TS
        for c in range(NDC):
            P.dma('sp', mbuf[:, c, :], mixT_v[:, c, t0:t0 + TS], w=[f'mbuf_{c}'])
        w_out_v = w_out.rearrange("(ic p) n -> p ic n", p=128)
        for og in range(8):
            w, wres = wload(w_out_v, og * 256)
            for o in range(2):
                oc = og * 2 + o
                acc, accr = accs.next()
                for ic in range(NDC):
                    P.matmul(acc[:, :], w[:, ic, o * 128:(o + 1) * 128], mbuf[:, ic, :], start=(ic == 0), stop=(ic == NDC - 1),
                             r=[f'{wres}_{ic}', f'mbuf_{ic}'], w=[accr])
                xc, xcres = xcr.next()
                P.dma('sp', xc[:, :], xT_v[:, oc, t0:t0 + TS], w=[xcres])
                P.op('act', lambda e, xc=xc: e.mul(out=xc[:, :], in_=xc[:, :], mul=ALPHA_C), [xcres], [xcres])
                P.stt('dve', z[:, oc, :], acc[:, :], 1.0, xc[:, :], ALU.mult, ALU.add, r=[accr, xcres], w=['z'])
        layernorm_fm(C, K, z, 'z', g1, b1, 'const_g1', z, 'z', xa16, 'xa16', s1, s2, scr)
        if debug_xa:
            for oc in range(NDC):
                P.dma('sp', xaT.rearrange("(dc p) t -> p dc t", p=128)[:, oc, t0:t0 + TS], z[:, oc, :], r=['z'], final=True)
        for oc in range(NDC):
            P.op('act', lambda e, oc=oc: e.mul(out=z[:, oc, :], in_=z[:, oc, :], mul=ALPHA_C), ['z'], ['z'])
        wg_v = wg_d.rearrange("(dc p) n -> p dc n", p=128)
        wu_v = wu_d.rearrange("(dc p) n -> p dc n", p=128)
        for fg in range(DFF // 256):
            wgt, wgres = wload(wg_v, fg * 256)
            wut, wures = wload(wu_v, fg * 256)
            for fc in range(2):
                ffc = fg * 2 + fc
                G, Gres = gu.next()
                U, Ures = gu.next()
                for dc in range(NDC):
                    P.matmul(G[:, :], wgt[:, dc, fc * 128:(fc + 1) * 128], xa16[:, dc, :], start=(dc == 0), stop=(dc == NDC - 1),
                             r=[f'{wgres}_{dc}', 'xa16'], w=[Gres])
                for dc in range(NDC):
                    P.matmul(U[:, :], wut[:, dc, fc * 128:(fc + 1) * 128], xa16[:, dc, :], start=(dc == 0), stop=(dc == NDC - 1),
                             r=[f'{wures}_{dc}', 'xa16'], w=[Ures])
                sg, sgres = sgr.next()
                P.activation(sg[:, :], G[:, :], AF.Silu, r=[Gres], w=[sgres])
                P.stt('dve', h16[:, ffc, :], U[:, :], 1.0, sg[:, :], ALU.mult, ALU.mult, r=[Ures, sgres], w=[f'h16_{ffc}'])
        wd_v = wd_d.rearrange("(f p) n -> p f n", p=128)
        for ocg in range(4):
            Dacc = [gu.next() for _ in range(4)]
            for fq4 in range(NFC // 4):
                wd, wdres = wdr.next()
                cast_load(P, wd, wd_v[:, fq4 * 4:(fq4 + 1) * 4, ocg * TS:(ocg + 1) * TS], 4, wdres, per_chunk=True)
                for j in range(4):
                    ffc = fq4 * 4 + j
                    for o in range(4):
                        P.matmul(Dacc[o][0][:, :], wd[:, j, o * 128:(o + 1) * 128], h16[:, ffc, :], start=(ffc == 0), stop=(ffc == NFC - 1),
                                 r=[f'{wdres}_{j}', f'h16_{ffc}'], w=[Dacc[o][1]])
            for o in range(4):
                oc = ocg * 4 + o
                P.stt('dve', z[:, oc, :], Dacc[o][0][:, :], 1.0, z[:, oc, :], ALU.mult, ALU.add, r=[Dacc[o][1], 'z'], w=['z'])
        layernorm_fm(C, K, z, 'z', g2, b2, 'const_g2', z, 'z', mbuf, lambda oc: f'mbuf_{oc}', s1, s2, scr)
        for oc in range(NDC):
            P.dma('sp', x1T_v[:, oc, t0:t0 + TS], z[:, oc, :], r=['z'], final=True)
        w_qkv_v = w_qkv.rearrange("(dc p) n -> p dc n", p=128)
        for qg in range(16):
            w, wres = wload(w_qkv_v, qg * 256)
            for j in range(2):
                ch = qg * 2 + j
                acc, accr = accs.next()
                for dc in range(NDC):
                    P.matmul(acc[:, :], w[:, dc, j * 128:(j + 1) * 128], mbuf[:, dc, :], start=(dc == 0), stop=(dc == NDC - 1),
                             r=[f'{wres}_{dc}', f'mbuf_{dc}'], w=[accr])
                o16, o16res = o16r.next()
                P.copy('act', o16[:, :], acc[:, :], r=[accr], w=[o16res])
                dst = fq[ch, :, t0:t0 + TS] if ch < 16 else fk[ch - 16, :, t0:t0 + TS]
                P.dma('sp', dst, o16[:, :], r=[o16res], final=True)
        for vg in range(8):
            w, wres = wload(w_qkv_v, 4096 + vg * 256)
            for tb in range(4):
                acc, accr = accs.next()
                for dc in range(NDC):
                    P.matmul(acc[:, 0:256], mbuf[:, dc, tb * 128:(tb + 1) * 128], w[:, dc, :], start=(dc == 0), stop=(dc == NDC - 1),
                             r=[f'{wres}_{dc}', f'mbuf_{dc}'], w=[accr])
                vs, vsres = vsr_.next()
                P.copy('act', vs[:, 0:2, :], acc[:, 0:256].rearrange("p (h c) -> p h c", c=128), r=[accr], w=[vsres])
                P.dma('sp', fv[vg * 2:vg * 2 + 2, :, g * 4 + tb, :].rearrange("h p c -> p h c"), vs[:, 0:2, :], r=[vsres], final=True)
        acc, accr = accs.next()
        for dc in range(NDC):
            P.matmul(acc[0:16, :], wf32[:, dc, :], z[:, dc, :], start=(dc == 0), stop=(dc == NDC - 1), r=['const_wf', 'z'], w=[accr])
        l, lres = lf.next()
        P.activation(l[:, :], acc[0:16, :], AF.Exp, r=[accr, 'const_nbf2'], w=[lres], scale=-1.0, bias=nbf[:, 0:1])
        P.activation(l[:, :], l[:, :], AF.Ln, r=[lres], w=[lres], bias=1.0)
        P.op('act', lambda e, l=l: e.mul(out=l[:, :], in_=l[:, :], mul=-1.0), [lres], [lres])
        P.dma('sp', logf[:, t0:t0 + TS], l[:, :], r=[lres], final=True)
    return C


def l3_inputs(inp, mixT_list):
    common = {
        'w_out': np.ascontiguousarray(inp['ev_w_out'][0]),
        'ln1g': fm_vec(inp['ev_ln1_g'][0], NDC), 'ln1b': fm_vec(inp['ev_ln1_b'][0], NDC),
        'ln2g': fm_vec(inp['ev_ln2_g'][0], NDC), 'ln2b': fm_vec(inp['ev_ln2_b'][0], NDC),
        'wg': np.ascontiguousarray(inp['ev_ffn_w_gate'][0]), 'wu': np.ascontiguousarray(inp['ev_ffn_w_up'][0]),
        'wd': np.ascontiguousarray(inp['ev_ffn_w_down'][0]), 'w_qkv': np.ascontiguousarray(inp['od_w_qkv'][0]),
        'w_f': np.ascontiguousarray(inp['od_w_f'][0]), 'b_f': np.ascontiguousarray(inp['od_b_f'][0].reshape(16, 1)),
    }
    maps = []
    for c in range(8):
        m = dict(common)
        m['mixT'] = mixT_list[c]
        m['xT'] = np.ascontiguousarray(inp['x'][c // 4][core_positions(c)].T)
        maps.append(m)
    return maps


def build_l4():
    C = Ctx()
    P = C.P
    fq = C.inp('fq', [16, 128, NTOK], BF16)
    fkf = C.inp('fkf', [4, 16, 128, NTOK], BF16)
    fvf = C.inp('fvf', [4, 16, 128, 16, 128], BF16)
    logff = C.inp('logff', [4, 16, NTOK])
    cmask = C.inp('cmask', [128, 16, TS], BF16)
    onehot = C.inp('onehot', [16, 4])
    sel_d = C.inp('sel', [16, 16, 128])
    id_d = C.inp('ident16', [16, 16])
    mixF = C.out('mixF', [16, 128, NTOK], BF16)
    K = load_consts(C)
    scale = 128.0 ** -0.5
    cm = C.sb('cm_sb', [128, 16, TS], BF16)
    cast_load(P, cm, cmask, 16, 'const_cm', q='sp', maxcols=TS, per_chunk=True)
    lf_sb = C.sb('lf_sb', [16, 4, NTOK])
    cT = C.sb('cT', [16, 4, NTOK])
    for r2 in range(4):
        P.dma('sp', lf_sb[:, r2, :], logff[r2, :, :], w=[f'lf_{r2}'])
    oh = C.sb('oh_sb', [16, 4]); P.dma('sp', oh[:, :], onehot, w=['const_oh'])
    sel = C.sb('sel_sb', [16, 16, 128]); P.dma('sp', sel[:, :, :], sel_d, w=['const_sel'])
    id16 = C.sb('id16', [16, 16]); P.dma('sp', id16[:, :], id_d, w=['const_id'])
    ones_s = C.sb('ones_s', [16, TS]); P.memset('dve', ones_s[:, :], 1.0, w=['const_ones_s'])
    prev = None
    for j in range(16):
        g2, r2 = j // 4, j % 4
        seg = lf_sb[:, r2, g2 * TS:(g2 + 1) * TS]
        out = cT[:, r2, g2 * TS:(g2 + 1) * TS]
        init = 0.0 if prev is None else prev
        rd = [f'lf_{r2}', 'const_ones_s'] + ([] if prev is None else [f'cT_{j - 1}'])
        P.op('dve', lambda e, out=out, seg=seg, init=init: e.tensor_tensor_scan(out=out, data0=ones_s[:, :], data1=seg, initial=init,
                                                                                 op0=ALU.mult, op1=ALU.add), rd, [f'cT_{j}'])
        prev = cT[:, r2, (g2 + 1) * TS - 1:(g2 + 1) * TS]
    allc = [f'cT_{j}' for j in range(16)]
    c_own = C.sb('c_own', [16, NT, TS])
    for g in range(NT):
        P.ts('dve', c_own[:, g, :], cT[:, 0, g * TS:(g + 1) * TS], oh[:, 0:1], None, ALU.mult, r=allc + ['const_oh'], w=[f'cown_{g}'])
        for r2 in range(1, 4):
            P.stt('dve', c_own[:, g, :], cT[:, r2, g * TS:(g + 1) * TS], oh[:, r2:r2 + 1], c_own[:, g, :], ALU.mult, ALU.add,
                  r=allc + ['const_oh', f'cown_{g}'], w=[f'cown_{g}'])
    c_tok = C.sb('c_tok', [128, 64, 16])
    ctp = C.psring('ctp', 1)
    for half in range(2):
        ps, psr = ctp.next()
        for i in range(32):
            blk = half * 32 + i
            r2, lb = blk // 16, blk % 16
            P.matmul(ps[:, i * 16:(i + 1) * 16], cT[:, r2, lb * 128:(lb + 1) * 128], id16[:, :], r=allc + ['const_id'], w=[psr])
        P.copy('act', c_tok[:, half * 32:(half + 1) * 32, :], ps[:, :].rearrange("p (b h) -> p b h", h=16), r=[psr], w=['c_tok'])
    kT = C.sb('kT', [128, 4, NTOK], BF16)
    v = C.sb('v', [128, 4, 16 * 128], BF16)
    qr_ = C.ring('q_', 2, [128, NTOK], BF16)
    Sr = C.psring('S', 3)
    Or = C.psring('O', 2)
    Dr = C.psring('Dn', 2)
    pTr = C.ring('pT_', 5, [128, TS], BF16)
    tmpr = C.ring('tmp_', 4, [128, TS], F32)
    cqr = C.sb('cqr', [128, TS])
    cref = C.sb('cref', [128, 1])
    bias_all = C.sb('bias_all', [128, 64])
    rec = C.sb('rec', [128, TS])
    o32 = C.sb('o32', [128, TS])
    o16r = C.ring('ao16_', 2, [128, TS], BF16)
    for h in range(16):
        q, qres = qr_.next()
        for r2 in range(4):
            P.dma('sp', kT[:, r2, :], fkf[r2, h, :, :], w=[f'kT_{r2}'])
            P.dma('sp', v[:, r2, :], fvf[r2, h, :, :, :].rearrange("p b c -> p (b c)"), w=[f'v_{r2}'])
        P.dma('sp', q[:, :], fq[h, :, :], w=[qres])
        for g in range(NT):
            cqb, cqbr = ctp.next()
            P.matmul(cqb[:, :], sel[:, h, :], c_own[:, g, :], r=['const_sel', f'cown_{g}'], w=[cqbr])
            P.copy('act', cref[:, :], cqb[:, 0:1], r=[cqbr], w=['cref'])
            P.ts('dve', cqr[:, :], cqb[:, :], cref[:, 0:1], None, ALU.subtract, r=[cqbr, 'cref'], w=['cqr'])
            P.ts('dve', bias_all[:, :], c_tok[:, :, h], cref[:, 0:1], -1.0, ALU.subtract, ALU.mult, r=['c_tok', 'cref'], w=['bias_all'])
            O, Ores = Or.next()
            Dn, Dres = Dr.next()
            blocks = [(g2, r2, kb) for g2 in range(g + 1) for r2 in range(4) for kb in range(4)]

            def s_stage(i, b, g=g, q=q, qres=qres):
                g2, r2, kb = b
                S, Sres = Sr.next()
                pT, pTres = pTr.next()
                tmp, tmpres = tmpr.next()
                c0 = g2 * TS + kb * 128
                lb = g2 * 4 + kb
                P.matmul(S[:, :], kT[:, r2, c0:c0 + 128], q[:, g * TS:(g + 1) * TS], r=[f'kT_{r2}', qres], w=[Sres])
                P.stt('dve', tmp[:, :], S[:, :], scale, cqr[:, :], ALU.mult, ALU.add, r=[Sres, 'cqr'], w=[tmpres])
                if g2 == g:
                    P.stt('dve', tmp[:, :], tmp[:, :], 1.0, cm[:, r2 * 4 + kb, :], ALU.mult, ALU.add,
                          r=[tmpres, f'const_cm_{r2 * 4 + kb}'], w=[tmpres])
                bcol = r2 * 16 + lb
                P.activation(pT[:, :], tmp[:, :], AF.Exp, r=[tmpres, 'bias_all'], w=[pTres], bias=bias_all[:, bcol:bcol + 1])
                return pT, pTres

            def pv_stage(j, b, stt_, first, last, O=O, Ores=Ores, Dn=Dn, Dres=Dres):
                g2, r2, kb = b
                pT, pTres = stt_
                lb = g2 * 4 + kb
                P.matmul(O[:, :], v[:, r2, lb * 128:(lb + 1) * 128], pT[:, :], start=first, stop=last, r=[f'v_{r2}', pTres], w=[Ores])
                P.matmul(Dn[:, :], K['ones16'][:, :], pT[:, :], start=first, stop=last, r=['const_ones16', pTres], w=[Dres])

            attn_pipeline(blocks, s_stage, pv_stage)
            P.recip(rec[:, :], Dn[:, :], r=[Dres], w=['rec'])
            P.copy('act', o32[:, :], O[:, :], r=[Ores], w=['o32'])
            o16, o16res = o16r.next()
            P.tt('pool', o16[:, :], o32[:, :], rec[:, :], ALU.mult, r=['o32', 'rec'], w=[o16res])
            P.dma('sp', mixF[h, :, g * TS:(g + 1) * TS], o16[:, :], r=[o16res], final=True)
    return C


def l4_inputs(r3):
    sel = np.zeros((16, 16, 128), np.float32)
    for h in range(16):
        sel[h, h, :] = 1.0
    maps = []
    for c in range(8):
        b, r = c // 4, c % 4
        grp = [r3[4 * b + r2] for r2 in range(4)]
        oh = np.zeros((16, 4), np.float32)
        oh[:, r] = 1.0
        maps.append({'fq': r3[c]['fq'], 'fkf': np.stack([g['fk'] for g in grp]), 'fvf': np.stack([g['fv'] for g in grp]),
                     'logff': np.stack([g['logf'] for g in grp]), 'cmask': causal_mask_tiles(r), 'onehot': oh, 'sel': sel,
                     'ident16': np.eye(16, dtype=np.float32)})
    return maps


def build_l5a():
    C = Ctx()
    P = C.P
    mixT = C.inp('mixT', [16, 128, NTOK], BF16)
    xT = C.inp('xT', [D, NTOK])
    w_out = C.inp('w_out', [D, D])
    ln1g = C.inp('ln1g', [128, NDC]); ln1b = C.inp('ln1b', [128, NDC])
    rw = C.inp('rw', [D, NEXP]); rb = C.inp('rb', [1, NEXP])
    xbT = C.out('xbT', [D, NTOK])
    xb16T = C.out('xb16T', [D, NTOK], BF16)
    gates = C.out('gates', [NTOK, NEXP])
    K = load_consts(C)
    g1 = C.sb('g1', [128, NDC]); b1 = C.sb('b1', [128, NDC])
    P.dma('sp', g1[:, :], ln1g, w=['const_g1']); P.dma('sp', b1[:, :], ln1b, w=['const_b1'])
    rw32 = C.sb('rw32', [128, NDC, NEXP])
    P.dma('sp', rw32[:, :, :], rw.rearrange("(dc p) n -> p dc n", p=128), w=['const_rw'])
    rb32 = C.sb('rb32', [1, NEXP]); P.dma('sp', rb32[:, :], rb, w=['const_rb'])
    mbuf = C.sb('mbuf', [128, NDC, TS], BF16)
    xb16 = C.sb('xb16', [128, NDC, TS], BF16)
    z = C.sb('z', [128, NDC, TS])
    wr = C.ring('w_', 4, [128, NDC, 256], BF16)
    xcr = C.ring('xc_', 3, [128, TS], F32)
    scr = ln_scratch(C)
    accs = C.psring('acc', 2)
    s1 = (C.ps('s1'), 's1'); s2 = (C.ps('s2'), 's2')
    lgp = C.psring('lgp', 2)
    sm = {n: C.ring(n + '_', 2, [128, NEXP], F32) for n in ('l', 'mk', 'l2', 'sel', 'e', 'gt')}
    s1c = {n: C.ring(n + '_', 2, [128, 1], F32) for n in ('m1', 'm2', 'nm1', 'den')}
    mixT_v = mixT.rearrange("c p t -> p c t")
    xT_v = xT.rearrange("(dc p) t -> p dc t", p=128)
    xbT_v = xbT.rearrange("(dc p) t -> p dc t", p=128)
    xb16T_v = xb16T.rearrange("(dc p) t -> p dc t", p=128)
    w_out_v = w_out.rearrange("(ic p) n -> p ic n", p=128)
    for g in range(NT):
        t0 = g * TS
        for c in range(NDC):
            P.dma('sp', mbuf[:, c, :], mixT_v[:, c, t0:t0 + TS], w=[f'mbuf_{c}'])
        for og in range(8):
            w, wres = wr.next()
            cast_load(P, w, w_out_v[:, :, og * 256:(og + 1) * 256], NDC, wres, per_chunk=True)
            for o in range(2):
                oc = og * 2 + o
                acc, accr = accs.next()
                for ic in range(NDC):
                    P.matmul(acc[:, :], w[:, ic, o * 128:(o + 1) * 128], mbuf[:, ic, :], start=(ic == 0), stop=(ic == NDC - 1),
                             r=[f'{wres}_{ic}', f'mbuf_{ic}'], w=[accr])
                xc, xcres = xcr.next()
                P.dma('sp', xc[:, :], xT_v[:, oc, t0:t0 + TS], w=[xcres])
                P.op('act', lambda e, xc=xc: e.mul(out=xc[:, :], in_=xc[:, :], mul=ALPHA_C), [xcres], [xcres])
                P.stt('dve', z[:, oc, :], acc[:, :], 1.0, xc[:, :], ALU.mult, ALU.add, r=[accr, xcres], w=['z'])
        layernorm_fm(C, K, z, 'z', g1, b1, 'const_g1', z, 'z', xb16, 'xb16', s1, s2, scr)
        for oc in range(NDC):
            P.dma('sp', xbT_v[:, oc, t0:t0 + TS], z[:, oc, :], r=['z'], final=True)
            P.dma('sp', xb16T_v[:, oc, t0:t0 + TS], xb16[:, oc, :], r=['xb16'], final=True)
        for tb in range(4):
            lg, lgr = lgp.next()
            for dc in range(NDC):
                P.matmul(lg[:, 0:NEXP], z[:, dc, tb * 128:(tb + 1) * 128], rw32[:, dc, :], start=(dc == 0), stop=False,
                         r=['z', 'const_rw'], w=[lgr])
            P.matmul(lg[:, 0:NEXP], K['ones32'][0:1, :], rb32[0:1, :], start=False, stop=True, r=['const_ones32', 'const_rb'], w=[lgr])
            l, lr_ = sm['l'].next(); mk, mkr = sm['mk'].next(); l2, l2r = sm['l2'].next()
            sl, slr = sm['sel'].next(); ee, eer = sm['e'].next(); gt, gtr = sm['gt'].next()
            m1, m1r = s1c['m1'].next(); m2, m2r = s1c['m2'].next(); nm1, nm1r = s1c['nm1'].next(); den, denr = s1c['den'].next()
            P.copy('act', l[:, :], lg[:, 0:NEXP], r=[lgr], w=[lr_])
            P.op('dve', lambda e, m1=m1, l=l: e.tensor_reduce(out=m1[:, :], in_=l[:, :], axis=AX.X, op=ALU.max), [lr_], [m1r])
            P.ts('dve', mk[:, :], l[:, :], m1[:, 0:1], None, ALU.is_equal, r=[lr_, m1r], w=[mkr])
            P.stt('dve', l2[:, :], mk[:, :], -1.0e30, l[:, :], ALU.mult, ALU.add, r=[mkr, lr_], w=[l2r])
            P.op('dve', lambda e, m2=m2, l2=l2: e.tensor_reduce(out=m2[:, :], in_=l2[:, :], axis=AX.X, op=ALU.max), [l2r], [m2r])
            P.ts('dve', sl[:, :], l[:, :], m2[:, 0:1], None, ALU.is_ge, r=[lr_, m2r], w=[slr])
            P.ts('dve', nm1[:, :], m1[:, :], -1.0, None, ALU.mult, r=[m1r], w=[nm1r])
            P.activation(ee[:, :], l[:, :], AF.Exp, r=[lr_, nm1r], w=[eer], bias=nm1[:, 0:1])
            P.stt('dve', ee[:, :], ee[:, :], 1.0, sl[:, :], ALU.mult, ALU.mult, r=[eer, slr], w=[eer])
            P.op('dve', lambda e, den=den, ee=ee: e.tensor_reduce(out=den[:, :], in_=ee[:, :], axis=AX.X, op=ALU.add), [eer], [denr])
            P.recip(den[:, :], den[:, :], r=[denr], w=[denr])
            P.ts('dve', gt[:, :], ee[:, :], den[:, 0:1], None, ALU.mult, r=[eer, denr], w=[gtr])
            P.dma('sp', gates[t0 + tb * 128:t0 + (tb + 1) * 128, :], gt[:, :], r=[gtr], final=True)
    return C


NALL = 8 * NTOK


def build_l5b():
    C = Ctx()
    P = C.P
    xall = C.inp('xall', [D, NALL], BF16)
    grow = C.inp('grow', [1, NALL])
    wg_d = C.inp('wg', [D, DFF]); wu_d = C.inp('wu', [D, DFF]); wd_d = C.inp('wd', [DFF, D])
    yT = C.out('yT', [D, NALL], BF16)
    NP = 2
    xa16 = [C.sb(f'xa16_{t}', [128, NDC, TS], BF16) for t in range(NP)]
    gbs = [C.sb(f'gb_{t}', [128, TS], F32) for t in range(NP)]
    h16 = [C.sb(f'h16_{t}', [128, NFC, TS], BF16) for t in range(NP)]
    wr = C.ring('w_', 4, [128, NDC, 256], BF16)
    wdr = C.ring('wd_', 3, [128, 4, TS], BF16)
    sgr = C.ring('sg_', 3, [128, TS], F32)
    o16r = C.ring('o16_', 4, [128, TS], BF16)
    gu = C.psring('gu', 8)
    xall_v = xall.rearrange("(dc p) t -> p dc t", p=128)
    yT_v = yT.rearrange("(dc p) t -> p dc t", p=128)
    wg_v = wg_d.rearrange("(dc p) n -> p dc n", p=128)
    wu_v = wu_d.rearrange("(dc p) n -> p dc n", p=128)
    wd_v = wd_d.rearrange("(f p) n -> p f n", p=128)
    for pr in range(NALL // (TS * NP)):
        t0s = [(pr * NP + t) * TS for t in range(NP)]
        for t in range(NP):
            for dc in range(NDC):
                P.dma('sp', xa16[t][:, dc, :], xall_v[:, dc, t0s[t]:t0s[t] + TS], w=[f'xa{t}_{dc}'])
            P.dma('sp', gbs[t][:, :], grow[0:1, t0s[t]:t0s[t] + TS].partition_broadcast(128), w=[f'gb{t}'])
        for fg in range(DFF // 256):
            wgt, wgres = wr.next()
            cast_load(P, wgt, wg_v[:, :, fg * 256:(fg + 1) * 256], NDC, wgres, per_chunk=True)
            wut, wures = wr.next()
            cast_load(P, wut, wu_v[:, :, fg * 256:(fg + 1) * 256], NDC, wures, per_chunk=True)
            for fc in range(2):
                ffc = fg * 2 + fc
                for t in range(NP):
                    G, Gres = gu.next()
                    U, Ures = gu.next()
                    for dc in range(NDC):
                        P.matmul(G[:, :], wgt[:, dc, fc * 128:(fc + 1) * 128], xa16[t][:, dc, :], start=(dc == 0), stop=(dc == NDC - 1),
                                 r=[f'{wgres}_{dc}', f'xa{t}_{dc}'], w=[Gres])
                    for dc in range(NDC):
                        P.matmul(U[:, :], wut[:, dc, fc * 128:(fc + 1) * 128], xa16[t][:, dc, :], start=(dc == 0), stop=(dc == NDC - 1),
                                 r=[f'{wures}_{dc}', f'xa{t}_{dc}'], w=[Ures])
                    sg, sgres = sgr.next()
                    P.activation(sg[:, :], G[:, :], AF.Silu, r=[Gres], w=[sgres])
                    P.tt('pool', sg[:, :], sg[:, :], gbs[t][:, :], ALU.mult, r=[sgres, f'gb{t}'], w=[sgres])
                    P.stt('dve', h16[t][:, ffc, :], U[:, :], 1.0, sg[:, :], ALU.mult, ALU.mult, r=[Ures, sgres], w=[f'h16_{t}_{ffc}'])
        for ocg in range(4):
            Dacc = [[gu.next() for _ in range(4)] for t in range(NP)]
            for fq4 in range(NFC // 4):
                wd, wdres = wdr.next()
                cast_load(P, wd, wd_v[:, fq4 * 4:(fq4 + 1) * 4, ocg * TS:(ocg + 1) * TS], 4, wdres, per_chunk=True)
                for j in range(4):
                    ffc = fq4 * 4 + j
                    for t in range(NP):
                        for o in range(4):
                            P.matmul(Dacc[t][o][0][:, :], wd[:, j, o * 128:(o + 1) * 128], h16[t][:, ffc, :], start=(ffc == 0),
                                     stop=(ffc == NFC - 1), r=[f'{wdres}_{j}', f'h16_{t}_{ffc}'], w=[Dacc[t][o][1]])
            for t in range(NP):
                for o in range(4):
                    oc = ocg * 4 + o
                    o16, o16res = o16r.next()
                    P.copy('act', o16[:, :], Dacc[t][o][0][:, :], r=[Dacc[t][o][1]], w=[o16res])
                    P.dma('sp', yT_v[:, oc, t0s[t]:t0s[t] + TS], o16[:, :], r=[o16res], final=True)
    return C


def build_l5c():
    C = Ctx()
    P = C.P
    yparts = C.inp('yparts', [NEXP, D, NTOK], BF16)
    xT = C.inp('xT', [D, NTOK])
    ln2g = C.inp('ln2g', [128, NDC]); ln2b = C.inp('ln2b', [128, NDC])
    outT = C.out('outT', [D, NTOK])
    K = load_consts(C)
    g2 = C.sb('g2', [128, NDC]); b2 = C.sb('b2', [128, NDC])
    P.dma('sp', g2[:, :], ln2g, w=['const_g2']); P.dma('sp', b2[:, :], ln2b, w=['const_b2'])
    z = C.sb('z', [128, NDC, TS])
    x16 = C.sb('x16d', [128, NDC, TS], BF16)
    ypr = C.ring('yp_', 4, [128, NEXP, TS], BF16)
    scr = ln_scratch(C)
    s1 = (C.ps('s1'), 's1'); s2 = (C.ps('s2'), 's2')
    xT_v = xT.rearrange("(dc p) t -> p dc t", p=128)
    outT_v = outT.rearrange("(dc p) t -> p dc t", p=128)
    yp_v = yparts.rearrange("e (dc p) t -> p e dc t", p=128)
    for g in range(NT):
        t0 = g * TS
        for oc in range(NDC):
            P.dma('sp', z[:, oc, :], xT_v[:, oc, t0:t0 + TS], w=[f'z_{oc}'])
            P.op('act', lambda e, oc=oc: e.mul(out=z[:, oc, :], in_=z[:, oc, :], mul=ALPHA_C), [f'z_{oc}'], [f'z_{oc}'])
            yp, ypres = ypr.next()
            for ex in range(NEXP):
                P.dma('sp', yp[:, ex, :], yp_v[:, ex, oc, t0:t0 + TS], w=[f'{ypres}_{ex}'])
            for ex in range(NEXP):
                P.stt('dve', z[:, oc, :], yp[:, ex, :], 1.0, z[:, oc, :], ALU.mult, ALU.add, r=[f'{ypres}_{ex}', f'z_{oc}'], w=[f'z_{oc}'])
        zall = [f'z_{oc}' for oc in range(NDC)]
        layernorm_fm(C, K, z, zall, g2, b2, 'const_g2', z, 'zo', x16, 'x16d', s1, s2, scr)
        for oc in range(NDC):
            P.dma('sp', outT_v[:, oc, t0:t0 + TS], z[:, oc, :], r=['zo'], final=True)
    return C


def l5a_inputs(inp, mixF_list, x1T_list):
    common = {'w_out': np.ascontiguousarray(inp['od_w_out'][0]),
              'ln1g': fm_vec(inp['od_ln1_g'][0], NDC), 'ln1b': fm_vec(inp['od_ln1_b'][0], NDC),
              'rw': np.ascontiguousarray(inp['od_router_w'][0]), 'rb': np.ascontiguousarray(inp['od_router_b'][0].reshape(1, NEXP))}
    return [dict(common, mixT=mixF_list[c], xT=x1T_list[c]) for c in range(8)]


def l5b_inputs(inp, r5a):
    xall = np.ascontiguousarray(np.concatenate([r5a[c]['xb16T'] for c in range(8)], axis=1))
    gall = np.concatenate([r5a[c]['gates'] for c in range(8)], axis=0)
    maps = []
    for e in range(NEXP):
        maps.append({'xall': xall, 'grow': np.ascontiguousarray(gall[:, e].reshape(1, NALL)),
                     'wg': np.ascontiguousarray(inp['od_exp_w_gate'][0, e]), 'wu': np.ascontiguousarray(inp['od_exp_w_up'][0, e]),
                     'wd': np.ascontiguousarray(inp['od_exp_w_down'][0, e])})
    return maps


def l5c_inputs(inp, r5a, r5b):
    g2, b2 = fm_vec(inp['od_ln2_g'][0], NDC), fm_vec(inp['od_ln2_b'][0], NDC)
    maps = []
    for c in range(8):
        yp = np.ascontiguousarray(np.stack([r5b[e]['yT'][:, c * NTOK:(c + 1) * NTOK] for e in range(NEXP)]))
        maps.append({'yparts': yp, 'xT': r5a[c]['xbT'], 'ln2g': g2, 'ln2b': b2})
    return maps


def assemble_output(r5c):
    out = np.empty((2, 8192, D), np.float32)
    for c in range(8):
        out[c // 4][core_positions(c)] = r5c[c]['outT'].T
    return out


def kernel(**inputs):
    inp = {k: np.asarray(v) for k, v in inputs.items()}
    r1 = build_l1().run(l1_inputs(inp))
    m2, m2d = [], []
    for c in range(8):
        b, r = c // 4, c % 4
        grp = [r1[4 * b + r2] for r2 in range(4)]
        m2.append({'qm': r1[c]['qm'], 'kmf': np.stack([g['km'] for g in grp]), 'krf': np.stack([g['kr'] for g in grp]),
                   'vmf': np.stack([g['vm'] for g in grp]), 'cmask': causal_mask_tiles(r)})
        m2d.append({'dq': r1[c]['dq'], 'dkf': np.stack([g['dk'] for g in grp]), 'dvf': np.stack([g['dv'] for g in grp]),
                    'dmask': dil_mask_tiles(r)})
    r2 = build_l2_mla().run(m2)
    r2d = build_l2_dil().run(m2d)
    del r1, m2, m2d
    mix0 = [np.ascontiguousarray(np.concatenate([r2[c]['mixT'], r2d[c]['mixD']], axis=0)) for c in range(8)]
    r3 = build_l3().run(l3_inputs(inp, mix0))
    r4 = build_l4().run(l4_inputs(r3))
    r5a = build_l5a().run(l5a_inputs(inp, [r4[c]['mixF'] for c in range(8)], [r3[c]['x1T'] for c in range(8)]))
    del r3, r4
    r5b = build_l5b().run(l5b_inputs(inp, r5a))
    r5c = build_l5c().run(l5c_inputs(inp, r5a, r5b))
    return assemble_output(r5c)
```
